# Optimizing a Trainium2 kernel written in Bass

```python
import jax, jax.numpy as jnp
from jax import lax
import numpy as np

D_MODEL = 1024
BATCH = 8
SEQ = 2048
DEPTH = 2

CHUNK = 64
MIX_W = D_MODEL
MLSTM_HEADS = 4
MLSTM_DH = 3 * D_MODEL // 32
MLSTM_W = MLSTM_HEADS * MLSTM_DH
LRU_BLOCKS = 6
LRU_BW = D_MODEL // 16
LRU_W = LRU_BLOCKS * LRU_BW
LRU_CONV = 4
RG_C = 8.0
ATT_HEADS = 4
ATT_DH = D_MODEL // 16
ATT_W = ATT_HEADS * ATT_DH
IDX_HEADS = 4
IDX_DIM = 64
TOPK_MAX = 256
Q_BLOCK = CHUNK
D_FF = 11 * D_MODEL // 4
FFN_CONV = 3
EPS = 1e-6

IN_SPLITS = (MLSTM_W, MLSTM_W, MLSTM_W, MLSTM_W, MLSTM_HEADS, MLSTM_HEADS,
             LRU_W, LRU_W,
             ATT_W, ATT_W, ATT_W, IDX_HEADS * IDX_DIM, IDX_DIM, IDX_HEADS)
D_IN = sum(IN_SPLITS)

kernel_name = "hybrid_mlstm_rglru_dsa_block"


def rmsnorm(x, g):
    xf = x.astype(jnp.float32)
    y = xf * lax.rsqrt(jnp.mean(xf * xf, axis=-1, keepdims=True) + EPS)
    return (y * g.astype(jnp.float32)).astype(x.dtype)


def causal_depthwise_conv(x, w, b):
    width, c = w.shape
    xp = jnp.pad(x, ((0, 0), (width - 1, 0), (0, 0)))
    y = lax.conv_general_dilated(xp, w[:, None, :].astype(x.dtype), window_strides=(1,),
                                 padding='VALID', dimension_numbers=('NWC', 'WIO', 'NWC'),
                                 feature_group_count=c)
    return y + b.astype(x.dtype)


def split_cols(p):
    idx, acc = [], 0
    for s in IN_SPLITS[:-1]:
        acc += s
        idx.append(acc)
    return jnp.split(p, idx, axis=-1)


def mlstm_chunkwise(q, k, v, ig, fg):
    B, S, H, D = q.shape
    L = CHUNK
    nc = S // L
    f32 = jnp.float32
    q = q.astype(f32)
    k = k.astype(f32) * (D ** -0.5)
    v = v.astype(f32)
    log_f = jax.nn.log_sigmoid(fg.astype(f32))
    ig = ig.astype(f32)

    def to_chunks(a):
        return a.reshape(B, nc, L, H, D).transpose(1, 0, 3, 2, 4)

    def gate_chunks(a):
        return a.reshape(B, nc, L, H).transpose(1, 0, 3, 2)

    qs, ks, vs = to_chunks(q), to_chunks(k), to_chunks(v)
    i_s = gate_chunks(ig)
    b_s = jnp.cumsum(gate_chunks(log_f), axis=-1)
    tril = jnp.tril(jnp.ones((L, L), dtype=bool))

    def step(carry, inp):
        C, n, m = carry
        qc, kc, vc, ic, bc = inp
        d_log = bc[..., :, None] - bc[..., None, :] + ic[..., None, :]
        d_log = jnp.where(tril, d_log, -jnp.inf)
        inter = bc + m[..., None]
        m_t = jnp.maximum(inter, jnp.max(d_log, axis=-1))
        w_intra = jnp.exp(d_log - m_t[..., None])
        w_inter = jnp.exp(inter - m_t)
        s = jnp.einsum('bhtd,bhsd->bhts', qc, kc) * w_intra
        num = (w_inter[..., None] * jnp.einsum('bhtd,bhde->bhte', qc, C)
               + jnp.einsum('bhts,bhse->bhte', s, vc))
        den = w_inter * jnp.einsum('bhtd,bhd->bht', qc, n) + jnp.sum(s, axis=-1)
        h = num / jnp.maximum(jnp.abs(den), jnp.exp(-m_t))[..., None]
        b_last = bc[..., -1]
        g = b_last[..., None] - bc + ic
        m_new = jnp.maximum(b_last + m, jnp.max(g, axis=-1))
        wg = jnp.exp(g - m_new[..., None])
        decay = jnp.exp(b_last + m - m_new)
        C_new = decay[..., None, None] * C + jnp.einsum('bhs,bhsd,bhse->bhde', wg, kc, vc)
        n_new = decay[..., None] * n + jnp.einsum('bhs,bhsd->bhd', wg, kc)
        return (C_new, n_new, m_new), h

    init = (jnp.zeros((B, H, D, D), f32), jnp.zeros((B, H, D), f32), jnp.zeros((B, H), f32))
    _, hs = lax.scan(step, init, (qs, ks, vs, i_s, b_s))
    return hs.transpose(1, 0, 3, 2, 4).reshape(B, S, H, D)


def rglru(x, w_a, b_a, w_x, b_x, lam):
    B, S, C = x.shape
    f32 = jnp.float32
    xf = x.astype(f32)
    xb = xf.reshape(B, S, LRU_BLOCKS, LRU_BW)
    r = jax.nn.sigmoid(jnp.einsum('bsnc,ncd->bsnd', xb, w_a.astype(f32)).reshape(B, S, C)
                       + b_a.astype(f32))
    i = jax.nn.sigmoid(jnp.einsum('bsnc,ncd->bsnd', xb, w_x.astype(f32)).reshape(B, S, C)
                       + b_x.astype(f32))
    log_a = -RG_C * r * jax.nn.softplus(-lam.astype(f32))
    a = jnp.exp(log_a)
    u = jnp.sqrt(-jnp.expm1(2.0 * log_a)) * (i * xf)

    def combine(e1, e2):
        a1, b1 = e1
        a2, b2 = e2
        return a1 * a2, a2 * b1 + b2

    _, h = lax.associative_scan(combine, (a, u), axis=1)
    return h.astype(x.dtype)


def dsa_attention(q, k, v, iq, ik, iw):
    B, S, H, Dh = q.shape
    n_sel = min(TOPK_MAX, S // 4)
    nblk = S // Q_BLOCK
    key_chunk = jnp.arange(S) // CHUNK
    bidx = jnp.arange(B)[:, None, None]
    idx_scale = (IDX_HEADS ** -0.5) * (IDX_DIM ** -0.5)
    att_scale = Dh ** -0.5
    ikf = ik.astype(jnp.float32)

    def to_blocks(a):
        return a.reshape((B, nblk, Q_BLOCK) + a.shape[2:]).swapaxes(0, 1)

    def block(args):
        blk, qb, iqb, iwb = args
        t = blk * Q_BLOCK + jnp.arange(Q_BLOCK)
        admissible = key_chunk[None, :] <= (t // CHUNK)[:, None]
        rel = jax.nn.relu(jnp.einsum('bqhd,bsd->bqhs', iqb.astype(jnp.float32), ikf))
        score = jnp.einsum('bqh,bqhs->bqs', iwb.astype(jnp.float32), rel) * idx_scale
        score = jnp.where(admissible[None], score, -jnp.inf)
        top_val, top_idx = lax.top_k(score, n_sel)
        valid = jnp.isfinite(top_val)
        ks = k[bidx, top_idx]
        vs = v[bidx, top_idx]
        logits = jnp.einsum('bqhd,bqkhd->bqhk', qb, ks).astype(jnp.float32) * att_scale
        logits = jnp.where(valid[:, :, None, :], logits, -jnp.inf)
        p = jax.nn.softmax(logits, axis=-1)
        return jnp.einsum('bqhk,bqkhd->bqhd', p.astype(vs.dtype), vs)

    out = lax.map(block, (jnp.arange(nblk), to_blocks(q), to_blocks(iq), to_blocks(iw)))
    return out.swapaxes(0, 1).reshape(B, S, H, Dh)


def hybrid_mixer(xn, w_in, b_igate, b_fgate, mlstm_norm, lru_conv_w, lru_conv_b,
                 lru_w_a, lru_b_a, lru_w_x, lru_b_x, lru_lambda, w_out):
    B, S, _ = xn.shape
    p = xn @ w_in
    mq, mk, mv, mo, mi, mf, lx, lg, aq, ak, av, iq, ik, iw = split_cols(p)
    shp = (B, S, MLSTM_HEADS, MLSTM_DH)
    hA = mlstm_chunkwise(mq.reshape(shp), mk.reshape(shp), mv.reshape(shp),
                         mi + b_igate, mf + b_fgate).astype(xn.dtype)
    hA = rmsnorm(hA, mlstm_norm.reshape(MLSTM_HEADS, MLSTM_DH)).reshape(B, S, MLSTM_W)
    hA = hA * jax.nn.sigmoid(mo)
    xc = causal_depthwise_conv(lx, lru_conv_w, lru_conv_b)
    hB = rglru(xc, lru_w_a, lru_b_a, lru_w_x, lru_b_x, lru_lambda) * jax.nn.gelu(lg, approximate=True)
    ashp = (B, S, ATT_HEADS, ATT_DH)
    hC = dsa_attention(aq.reshape(ashp), ak.reshape(ashp), av.reshape(ashp),
                       iq.reshape(B, S, IDX_HEADS, IDX_DIM), ik, iw).reshape(B, S, ATT_W)
    return jnp.concatenate([hA, hB, hC], axis=-1) @ w_out


def conv_ffn(xn, ffn_up, ffn_conv_w, ffn_conv_b, ffn_down):
    u = causal_depthwise_conv(xn @ ffn_up, ffn_conv_w, ffn_conv_b)
    gate, up = jnp.split(u, 2, axis=-1)
    return (jax.nn.gelu(gate, approximate=True) * up) @ ffn_down


def setup_inputs(seed: int = 0) -> dict:
    key = jax.random.key(seed)
    ks = jax.random.split(key, 24)
    f32 = jnp.float32
    nrm = lambda k, shape, s: jax.random.normal(k, shape, f32) * s
    gain = lambda k, shape: 1.0 + 0.05 * jax.random.normal(k, shape, f32)
    u = jax.random.uniform(ks[20], (DEPTH, LRU_W), f32, minval=0.9, maxval=0.999)
    s = u ** (1.0 / RG_C)
    lru_lambda = jnp.log(s) - jnp.log1p(-s)
    return {
        "x": nrm(ks[0], (BATCH, SEQ, D_MODEL), 1.0),
        "norm_mix_pre": gain(ks[1], (DEPTH, D_MODEL)),
        "norm_mix_post": gain(ks[2], (DEPTH, D_MODEL)),
        "norm_ffn_pre": gain(ks[3], (DEPTH, D_MODEL)),
        "norm_ffn_post": gain(ks[4], (DEPTH, D_MODEL)),
        "w_in": nrm(ks[5], (DEPTH, D_MODEL, D_IN), D_MODEL ** -0.5),
        "b_igate": nrm(ks[6], (DEPTH, MLSTM_HEADS), 0.1),
        "b_fgate": 3.0 + nrm(ks[7], (DEPTH, MLSTM_HEADS), 0.1),
        "mlstm_norm": gain(ks[8], (DEPTH, MLSTM_W)),
        "lru_conv_w": nrm(ks[9], (DEPTH, LRU_CONV, LRU_W), LRU_CONV ** -0.5),
        "lru_conv_b": nrm(ks[10], (DEPTH, LRU_W), 0.01),
        "lru_w_a": nrm(ks[11], (DEPTH, LRU_BLOCKS, LRU_BW, LRU_BW), LRU_BW ** -0.5),
        "lru_b_a": nrm(ks[12], (DEPTH, LRU_W), 0.01),
        "lru_w_x": nrm(ks[13], (DEPTH, LRU_BLOCKS, LRU_BW, LRU_BW), LRU_BW ** -0.5),
        "lru_b_x": nrm(ks[14], (DEPTH, LRU_W), 0.01),
        "lru_lambda": lru_lambda,
        "w_out": nrm(ks[15], (DEPTH, MIX_W, D_MODEL), MIX_W ** -0.5),
        "ffn_up": nrm(ks[16], (DEPTH, D_MODEL, 2 * D_FF), D_MODEL ** -0.5),
        "ffn_conv_w": nrm(ks[17], (DEPTH, FFN_CONV, 2 * D_FF), FFN_CONV ** -0.5),
        "ffn_conv_b": nrm(ks[18], (DEPTH, 2 * D_FF), 0.01),
        "ffn_down": nrm(ks[19], (DEPTH, D_FF, D_MODEL), D_FF ** -0.5),
    }


def reference(x, norm_mix_pre, norm_mix_post, norm_ffn_pre, norm_ffn_post, w_in,
              b_igate, b_fgate, mlstm_norm, lru_conv_w, lru_conv_b, lru_w_a, lru_b_a,
              lru_w_x, lru_b_x, lru_lambda, w_out, ffn_up, ffn_conv_w, ffn_conv_b, ffn_down):
    for l in range(DEPTH):
        h = rmsnorm(x, norm_mix_pre[l])
        mix = hybrid_mixer(h, w_in[l], b_igate[l], b_fgate[l], mlstm_norm[l],
                           lru_conv_w[l], lru_conv_b[l], lru_w_a[l], lru_b_a[l],
                           lru_w_x[l], lru_b_x[l], lru_lambda[l], w_out[l])
        x = x + rmsnorm(mix, norm_mix_post[l])
        h = rmsnorm(x, norm_ffn_pre[l])
        x = x + rmsnorm(conv_ffn(h, ffn_up[l], ffn_conv_w[l], ffn_conv_b[l], ffn_down[l]),
                        norm_ffn_post[l])
    return x
```

```python
import os
from contextlib import ExitStack
import numpy as np
import concourse.bass as bass
import concourse.mybir as mybir
from concourse.bass_utils import run_bass_kernel_spmd

F32 = mybir.dt.float32
BF16 = mybir.dt.bfloat16
AF = mybir.ActivationFunctionType
ALU = mybir.AluOpType
AX = mybir.AxisListType

S = 2048
D = 1024
DEPTH = 2
KC = 8
TG = 512
NG = S // TG
D_IN = 3404
DFF = 2816
NFC = 22
NBIS = 16
ENGS = ("pe", "act", "dve", "pool", "sp")


class Res:
    __slots__ = ("name", "w", "r", "dsem", "dcnt")

    def __init__(self, name):
        self.name = name
        self.w = None
        self.r = {}
        self.dsem = None
        self.dcnt = 0


class Prog:
    def __init__(self, nc, stack):
        self.nc = nc
        self.stack = stack
        self.items = {e: [] for e in ENGS}
        self.cnt = {e: 0 for e in ENGS}
        self.sem = {e: stack.enter_context(nc.semaphore("sem_" + e)) for e in ENGS if e != "sp"}
        self.known = {e: {f: 0 for f in ENGS} for e in ENGS}
        self.vc = {e: [None] for e in ENGS}
        self.dknown = {e: {} for e in ENGS}
        self.nres = 0
        self.ntot = 0
        self.limit = int(os.environ.get("K_LIMIT", "0")) or None
        self.force = False

    def mark(self, name):
        if os.environ.get("K_MARK"):
            print("MARK", name, self.ntot)

    def res(self, name=None):
        self.nres += 1
        return Res(name or ("r%d" % self.nres))

    def _dsem(self, r):
        if r.dsem is None:
            self.nds = getattr(self, "nds", 0) + 1
            r.dsem = self.stack.enter_context(self.nc.semaphore("d%d_%s" % (self.nds, r.name)))
        return r.dsem

    def _deps(self, eng, reads, writes):
        deps = {}
        dd = []

        def add(e_i):
            if e_i is None:
                return
            e, i = e_i
            if e == eng and eng == "pe":
                return
            if deps.get(e, 0) < i:
                deps[e] = i
        for r in reads:
            add(r.w)
            if r.dcnt:
                dd.append(r)
        for r in writes:
            add(r.w)
            for e, i in r.r.items():
                add((e, i))
            if r.dcnt:
                dd.append(r)
        waits = []
        kn = self.known[eng]
        for e, i in deps.items():
            if kn[e] < i:
                waits.append((self.sem[e], i))
                v = self.vc[e][i]
                for f in ENGS:
                    if kn[f] < v[f]:
                        kn[f] = v[f]
        dk = self.dknown[eng]
        for r in dd:
            if dk.get(id(r), 0) < r.dcnt:
                waits.append((self._dsem(r), r.dcnt))
                dk[id(r)] = r.dcnt
        return waits

    def op(self, eng, fns, reads=(), writes=()):
        if getattr(self, "skip", False):
            return
        self.ntot += 1
        if self.limit and self.ntot > self.limit and not self.force:
            return
        if not isinstance(fns, (list, tuple)):
            fns = [fns]
        waits = self._deps(eng, reads, writes)
        self.cnt[eng] += 1
        idx = self.cnt[eng]
        v = dict(self.known[eng])
        v[eng] = idx
        self.vc[eng].append(v)
        for r in reads:
            r.r[eng] = idx
        for r in writes:
            r.w = (eng, idx)
            r.r = {}
        self.items[eng].append((waits, fns, ("c", self.sem[eng])))
        return idx

    def dma(self, q, fn, reads=(), writes=()):
        if getattr(self, "skip", False):
            return
        self.ntot += 1
        if self.limit and self.ntot > self.limit and not self.force:
            return
        waits = self._deps(q, reads, writes)
        rs = list(reads) + list(writes)
        assert len(rs) == 1
        sem = self._dsem(rs[0])
        rs[0].dcnt += 16
        for r in writes:
            r.w = None
            r.r = {}
        self.items[q].append((waits, [fn], ("d", sem)))

    def wait_all_dma(self, eng, ress):
        waits = []
        for r in ress:
            if r.dcnt:
                waits.append((self._dsem(r), r.dcnt))
        self.items[eng].append((waits, [], None))

    def emit(self):
        nc = self.nc
        with nc.Block() as block:
            def run(e, items):
                for waits, fns, inc in items:
                    for s, v in waits:
                        e.wait_ge(s, v)
                    last = None
                    for f in fns:
                        last = f(e)
                    if inc is not None:
                        kind, s = inc
                        last.then_inc(s, 1 if kind == "c" else 16)

            @block.tensor
            def _(e):
                run(e, self.items["pe"])

            @block.scalar
            def _(e):
                run(e, self.items["act"])

            @block.vector
            def _(e):
                run(e, self.items["dve"])

            @block.gpsimd
            def _(e):
                run(e, self.items["pool"])

            @block.sync
            def _(e):
                run(e, self.items["sp"])


def MM(out, lhsT, rhs, start=True, stop=True, **kw):
    return lambda e: e.matmul(out=out, lhsT=lhsT, rhs=rhs, start=start, stop=stop, **kw)


def TR(out, in_, ident):
    return lambda e: e.transpose(out=out, in_=in_, identity=ident)


def ACT(out, in_, func, **kw):
    return lambda e: e.activation(out=out, in_=in_, func=func, **kw)


def TS(out, in0, s1, s2=None, op0=ALU.mult, op1=None, **kw):
    if op1 is None:
        return lambda e: e.tensor_scalar(out=out, in0=in0, scalar1=s1, scalar2=None, op0=op0, **kw)
    return lambda e: e.tensor_scalar(out=out, in0=in0, scalar1=s1, scalar2=s2, op0=op0, op1=op1, **kw)


def STT(out, in0, scalar, in1, op0, op1):
    return lambda e: e.scalar_tensor_tensor(out=out, in0=in0, scalar=scalar, in1=in1, op0=op0, op1=op1)


def TT(out, in0, in1, op):
    return lambda e: e.tensor_tensor(out=out, in0=in0, in1=in1, op=op)


def CP(out, in_):
    return lambda e: e.tensor_copy(out=out, in_=in_)


def MS(ap, v):
    return lambda e: e.memset(ap, v)


def build(depth=DEPTH, dbg=0, stop_after=None):
    nc = bass.Bass("TRN2", target_bir_lowering=False)
    dt_in = lambda n, s: nc.dram_tensor(n, list(s), F32, kind="ExternalInput").ap()
    xT_d = dt_in("xT", [D, S])
    w_in_d = dt_in("w_in", [DEPTH, D, D_IN])
    w_out_d = dt_in("w_out", [DEPTH, D, D])
    ffn_up_d = dt_in("ffn_up", [DEPTH, D, 2 * DFF])
    ffn_down_d = dt_in("ffn_down", [DEPTH, DFF, D])
    gains_d = dt_in("gains", [128, DEPTH * 4 * 8])
    bgate_d = dt_in("bgate", [128, DEPTH * 8])
    mnorm_d = dt_in("mnorm", [128, DEPTH * 384])
    lruc_d = dt_in("lruc", [128, DEPTH * 3 * 8])
    lruw_d = dt_in("lruw", [128, DEPTH * 2 * 3 * 128])
    fconv_d = dt_in("fconv", [128, DEPTH * 44 * 4])
    cst_d = dt_in("cst", [128, 128 + 128 + 192 + NBIS])
    yT_d = nc.dram_tensor("yT", [D, S], F32, kind="ExternalOutput").ap()
    if dbg:
        dbg_d = nc.dram_tensor("dbg", [D, S], F32, kind="ExternalOutput").ap()

    with ExitStack() as st:
        P = Prog(nc, st)
        sbt = lambda name, shape, dt: st.enter_context(nc.sbuf_tensor(name, list(shape), dt))
        pst = lambda name, shape, dt: st.enter_context(nc.psum_tensor(name, list(shape), dt))

        xT = sbt("xT_sb", [128, KC, S], F32)
        r_x = [[P.res("x%d_%d" % (c, g)) for g in range(NG)] for c in range(KC)]
        rstdb = sbt("rstdb", [128, S], F32)
        r_rstd = [P.res("rstd%d" % g) for g in range(NG)]
        gains = sbt("gains_sb", [128, DEPTH, 4, 8], F32); r_par = P.res("par")
        bgate = sbt("bgate_sb", [128, DEPTH, 8], F32)
        mnorm = sbt("mnorm_sb", [128, DEPTH, 384], F32)
        lruc = sbt("lruc_sb", [128, DEPTH, 3, 8], F32)
        lruw = sbt("lruw_sb", [128, DEPTH, 2, 3, 128], BF16); r_lruw = P.res("lruw")
        fconv = sbt("fconv_sb", [128, DEPTH, 44, 4], F32)
        cst = sbt("cst_sb", [128, 128 + 128 + 192 + NBIS], F32)
        identF = cst[:, 0:128]
        U2f = cst[:, 128:256]
        Ef = cst[:, 256:448]
        pow2 = cst[:, 448:448 + NBIS]
        identB = sbt("identB", [128, 128], BF16); r_cb = P.res("cstb")
        onesB = sbt("onesB", [128, 128], BF16)
        lrud = sbt("lrud", [128, DEPTH, 3, 4], F32); r_lrud = P.res("lrud")

        ARENA = 64000
        arena = sbt("arena", [128, ARENA], BF16)
        apos = [0]

        def carve(nelem, dt, shape=None):
            n16 = nelem * (2 if dt == F32 else 1)
            a = apos[0]
            if dt == F32 and a % 2:
                a += 1
            apos[0] = a + n16
            assert apos[0] <= ARENA, ("arena overflow", apos[0], ARENA)
            v = arena[:, a:a + n16]
            if dt == F32:
                v = v.bitcast(F32)
            return v

        pb = [pst("pb%d" % i, [128, 512], F32) for i in range(8)]
        r_pb = [P.res("pb%d" % i) for i in range(8)]

        for c in range(KC):
            for g in range(NG):
                P.dma("sp", (lambda e, c=c, g=g: e.dma_start(out=xT[:, c, g * TG:(g + 1) * TG], in_=xT_d[c * 128:(c + 1) * 128, g * TG:(g + 1) * TG])),
                      writes=[r_x[c][g]])
        smalls = [(gains, gains_d), (bgate, bgate_d), (mnorm, mnorm_d), (lruc, lruc_d), (fconv, fconv_d), (cst, cst_d)]
        r_sm = []
        for i, (t, d) in enumerate(smalls):
            r = P.res("sm%d" % i)
            r_sm.append(r)
            flat = t[:] if len(t.shape) == 2 else t[:].rearrange({3: "p a b -> p (a b)", 4: "p a b c -> p (a b c)", 5: "p a b c d -> p (a b c d)"}[len(t.shape)])
            P.dma("sp", (lambda e, flat=flat, d=d: e.dma_start(out=flat, in_=d[:, :])), writes=[r])
        r_gains, r_bgate, r_mnorm, r_lruc, r_fconv, r_cst = r_sm
        P.op("dve", CP(identB[:], identF), reads=[r_cst], writes=[r_cb])
        P.op("dve", MS(onesB[:], 1.0), writes=[r_cb])
        P.dma("pool", (lambda e: e.dma_start(out=lruw[:].rearrange("p a b c d -> p (a b c d)"), in_=lruw_d[:, :])), writes=[r_lruw])
        lam_t = sbt("lam_t", [128, DEPTH, 3], F32); r_lam = P.res("lam")
        P.op("act", ACT(lam_t[:], lruc[:, :, :, 7], AF.Exp, scale=-1.0), reads=[r_lruc], writes=[r_lam])
        P.op("act", ACT(lam_t[:], lam_t[:], AF.Ln, bias=1.0), reads=[r_lam], writes=[r_lam])
        P.op("dve", TS(lrud[:, :, :, 0], lam_t[:], -8.0), reads=[r_lam], writes=[r_lrud])
        P.op("dve", TS(lrud[:, :, :, 1], lam_t[:], -16.0), reads=[r_lam], writes=[r_lrud])

        def weight_dma(dst_ap, src_ap, res):
            P.dma("pool", (lambda e: e.dma_start(out=dst_ap, in_=src_ap)), writes=[res])

        def barrier(old, new):
            P.op("pool", MS(dummy[:], 0.0), writes=list(old) + list(new) + [r_dummy])

        dummy = sbt("dummy_sb", [128, 8], F32); r_dummy = P.res("dummy")

        def rms_stats(l, g, sq_ap, r_sq, pbank):
            tok = slice(g * TG, (g + 1) * TG)
            for c in range(KC):
                P.op("act", ACT(sq_ap[:, c, :], xT[:, c, tok], AF.Square), reads=[r_x[c][g]], writes=[r_sq])
            P.op("pe", [MM(pb[pbank][:, :], onesB[:], sq_ap[:, c, :], start=(c == 0), stop=(c == KC - 1)) for c in range(KC)],
                 reads=[r_sq, r_cb], writes=[r_pb[pbank]])
            P.op("act", ACT(rstdb[:, tok], pb[pbank][:, :], AF.Ln, scale=1.0 / D, bias=1e-6), reads=[r_pb[pbank]], writes=[r_rstd[g]])
            P.op("act", ACT(rstdb[:, tok], rstdb[:, tok], AF.Exp, scale=-0.5), reads=[r_rstd[g]], writes=[r_rstd[g]])

        def make_h(l, which, g, h_ap, r_h):
            tok = slice(g * TG, (g + 1) * TG)
            for c in range(KC):
                P.op("dve", STT(h_ap[:, c, :], xT[:, c, tok], gains[:, l, which, c:c + 1], rstdb[:, tok], ALU.mult, ALU.mult),
                     reads=[r_x[c][g], r_rstd[g], r_gains], writes=[r_h])

        def post_norm_residual(l, which, tok0, ntok, src_aps, r_src, sq_ap, r_sq, pbank, rs_ap, r_rs, tmp_ap, r_tmp):
            g = tok0 // TG
            tok = slice(tok0, tok0 + ntok)
            for c in range(KC):
                P.op("act", ACT(sq_ap[:, c, :], src_aps[c], AF.Square), reads=[r_src[c]], writes=[r_sq])
            P.op("pe", [MM(pb[pbank][:, 0:ntok], onesB[:], sq_ap[:, c, :], start=(c == 0), stop=(c == KC - 1)) for c in range(KC)],
                 reads=[r_sq, r_cb], writes=[r_pb[pbank]])
            P.op("act", ACT(rs_ap, pb[pbank][:, 0:ntok], AF.Ln, scale=1.0 / D, bias=1e-6), reads=[r_pb[pbank]], writes=[r_rs])
            P.op("act", ACT(rs_ap, rs_ap, AF.Exp, scale=-0.5), reads=[r_rs], writes=[r_rs])
            for c in range(KC):
                P.op("dve", STT(tmp_ap, src_aps[c], gains[:, l, which, c:c + 1], rs_ap, ALU.mult, ALU.mult),
                     reads=[r_src[c], r_rs, r_gains], writes=[r_tmp])
                P.op("dve", TT(xT[:, c, tok], xT[:, c, tok], tmp_ap, ALU.add), reads=[r_tmp, r_x[c][g]], writes=[r_x[c][g]])

        for l in range(depth):
            apos[0] = 0
            concat = carve(KC * S, BF16).rearrange("p (c t) -> p c t", c=KC)
            r_cat = [[P.res("cat%d_%d" % (c, t)) for t in range(16)] for c in range(KC)]
            hT = [carve(KC * TG, BF16).rearrange("p (c t) -> p c t", c=KC) for _ in range(2)]
            r_hT = [P.res("hT0"), P.res("hT1")]
            W = carve(KC * 1544, BF16).rearrange("p (c n) -> p c n", c=KC)
            r_W = P.res("W")
            pl0 = apos[0]
            all_mixer_res = [r for row in r_cat for r in row] + r_hT + [r_W]
            if l > 0:
                barrier(prev_phase_res, all_mixer_res)

            P.skip = bool(os.environ.get("K_ONLY_E"))
            P.mark("A")
            weight_dma(W[:, :, 0:1544], w_in_d[l, :, 0:1544].rearrange("(c p) n -> p c n", p=128), r_W)
            sqb = carve(KC * TG, BF16).rearrange("p (c t) -> p c t", c=KC); r_sqb = P.res("sqb")
            qTg = carve(4 * TG, BF16).rearrange("p (h t) -> p h t", h=4); r_qTg = P.res("qTg")
            kTg = carve(4 * TG, BF16).rearrange("p (h t) -> p h t", h=4); r_kTg = P.res("kTg")
            NB = 2
            kt = [carve(384, BF16).rearrange("p (h d) -> p h d", h=4) for _ in range(NB)]; r_kt = [P.res() for _ in range(NB)]
            vx = [carve(4 * 97, BF16).rearrange("p (h d) -> p h d", h=4) for _ in range(NB)]; r_vx = [P.res() for _ in range(NB)]
            og = [carve(384, BF16) for _ in range(NB)]; r_og = [P.res() for _ in range(NB)]
            gi = [carve(8, F32) for _ in range(NB)]; r_gi = [P.res() for _ in range(NB)]
            nlf = [carve(4, F32) for _ in range(NB)]; r_nlf = [P.res() for _ in range(NB)]
            es = [carve(4, F32) for _ in range(NB)]; r_es = [P.res() for _ in range(NB)]
            emb = [carve(4, F32) for _ in range(NB)]; r_emb = [P.res() for _ in range(NB)]
            ebl = [carve(8, F32).rearrange("p (j h) -> p j h", j=2) for _ in range(NB)]; r_ebl = [P.res() for _ in range(NB)]
            tmp4 = [carve(4, F32) for _ in range(NB)]; r_tmp4 = [P.res() for _ in range(NB)]
            Stt = [carve(4 * 128, BF16).rearrange("p (h t) -> p h t", h=4) for _ in range(NB)]; r_St = [P.res() for _ in range(NB)]
            Cf = carve(4 * 97, F32).rearrange("p (h d) -> p h d", h=4); r_Cf = P.res("Cf")
            Ctmp = carve(4 * 97, F32).rearrange("p (h d) -> p h d", h=4); r_Ctmp = P.res("Ctmp")
            NCB = 4
            Cb = [carve(4 * 97, BF16).rearrange("p (h d) -> p h d", h=4) for _ in range(NCB)]; r_Cb = [P.res() for _ in range(NCB)]
            den = [carve(4, F32) for _ in range(NB)]; r_den = [P.res() for _ in range(NB)]
            hraw = [carve(384, F32).rearrange("p (h d) -> p h d", h=4) for _ in range(NB)]; r_hraw = [P.res() for _ in range(NB)]
            hsq = [carve(384, F32).rearrange("p (h d) -> p h d", h=4) for _ in range(NB)]; r_hsq = [P.res() for _ in range(NB)]
            ssq = [carve(4, F32) for _ in range(NB)]; r_ssq = [P.res() for _ in range(NB)]
            gm = [carve(384, F32) for _ in range(NB)]; r_gm = [P.res() for _ in range(NB)]
            hAb = [carve(384, BF16) for _ in range(NB)]; r_hAb = [P.res() for _ in range(NB)]
            phaseA_res = ([r_sqb, r_qTg, r_kTg, r_Cf, r_Ctmp] + r_kt + r_vx + r_og + r_gi + r_nlf + r_es + r_emb + r_ebl + r_tmp4 + r_St
                          + r_Cb + r_den + r_hraw + r_hsq + r_ssq + r_gm + r_hAb)
            if l > 0:
                barrier(prev_phase_res, phaseA_res)

            P.op("dve", MS(Cf[0:96], 0.0), writes=[r_Cf])
            P.op("dve", MS(Cb[0][0:96], 0.0), writes=[r_Cb[0]])
            for b in range(NB):
                P.op("dve", MS(vx[b][:, :, 96:97], 1.0), writes=[r_vx[b]])
            cbi = 0
            LN_SC = float(np.log(96.0 ** -0.5))
            lnsc = sbt("lnsc%d" % l, [128, 1], F32); r_lnsc = P.res("lnsc")
            P.op("dve", MS(lnsc[:], LN_SC), writes=[r_lnsc])

            for g in range(NG):
                hb = g % 2
                rms_stats(l, g, sqb, r_sqb, 7)
                make_h(l, 0, g, hT[hb], r_hT[hb])
                for qk in range(2):
                    for h in range(4):
                        bank = (qk * 4 + h) % 2
                        col0 = qk * 384 + h * 96
                        P.op("pe", [MM(pb[bank][0:96, :], W[:, c, col0:col0 + 96], hT[hb][:, c, :], start=(c == 0), stop=(c == KC - 1)) for c in range(KC)],
                             reads=[r_W, r_hT[hb]], writes=[r_pb[bank]])
                        dst = (qTg if qk == 0 else kTg)
                        P.op("act", ACT(dst[0:96, h, :], pb[bank][0:96, :], AF.Copy), reads=[r_pb[bank]], writes=[r_qTg if qk == 0 else r_kTg])
                for ti in range(4):
                    T = g * 4 + ti
                    b = T % NB
                    tl = slice(ti * 128, (ti + 1) * 128)
                    for (bank, c0, n) in ((2, 1152, 392), (3, 384, 384), (4, 768, 384)):
                        P.op("pe", [MM(pb[bank][:, 0:n], hT[hb][:, c, tl], W[:, c, c0:c0 + n], start=(c == 0), stop=(c == KC - 1)) for c in range(KC)],
                             reads=[r_W, r_hT[hb]], writes=[r_pb[bank]])
                    P.op("dve", TT(gi[b], pb[2][:, 384:392], bgate[:, l, :], ALU.add), reads=[r_pb[2], r_bgate], writes=[r_gi[b]])
                    P.op("act", ACT(nlf[b], gi[b][:, 4:8], AF.Exp, scale=-1.0), reads=[r_gi[b]], writes=[r_nlf[b]])
                    P.op("act", ACT(nlf[b], nlf[b], AF.Ln, bias=1.0), reads=[r_nlf[b]], writes=[r_nlf[b]])
                    P.op("act", ACT(og[b], pb[2][:, 0:384], AF.Sigmoid), reads=[r_pb[2], r_gi[b]], writes=[r_og[b]])
                    P.op("pe", [MM(pb[5][:, 0:4], U2f, nlf[b], True, True),
                                MM(pb[5][0:96, 8:12], Ef[:, 0:96], nlf[b], True, True),
                                MM(pb[5][0:96, 16:20], Ef[:, 96:192], nlf[b], True, True)],
                         reads=[r_nlf[b], r_cst], writes=[r_pb[5]])
                    P.op("dve", TT(tmp4[b], gi[b][:, 0:4], pb[5][:, 0:4], ALU.add), reads=[r_gi[b], r_pb[5]], writes=[r_tmp4[b]])
                    P.op("act", ACT(es[b], tmp4[b], AF.Exp, bias=lnsc[:]), reads=[r_tmp4[b], r_lnsc], writes=[r_es[b]])
                    P.op("act", ACT(emb[b], pb[5][:, 0:4], AF.Exp), reads=[r_pb[5], r_tmp4[b]], writes=[r_emb[b]])
                    P.op("act", ACT(ebl[b][0:96, 0, :], pb[5][0:96, 8:12], AF.Exp, scale=-1.0), reads=[r_pb[5]], writes=[r_ebl[b]])
                    P.op("act", ACT(ebl[b][0:96, 1, :], pb[5][0:96, 16:20], AF.Exp, scale=-1.0), reads=[r_pb[5]], writes=[r_ebl[b]])
                    P.op("dve", TT(kt[b][:], pb[3][:, 0:384].rearrange("p (h d) -> p h d", h=4), es[b].unsqueeze(2).to_broadcast([128, 4, 96]), ALU.mult),
                         reads=[r_pb[3], r_es[b]], writes=[r_kt[b]])
                    P.op("act", ACT(vx[b][:, :, 0:96], pb[4][:, 0:384].rearrange("p (h d) -> p h d", h=4), AF.Copy), reads=[r_pb[4]], writes=[r_vx[b]])
                    P.op("pe", [MM(pb[6][:, h * 128:(h + 1) * 128], kTg[0:96, h, tl], qTg[0:96, h, tl], True, True) for h in range(4)],
                         reads=[r_kTg, r_qTg], writes=[r_pb[6]])
                    for h in range(4):
                        P.op("dve", STT(Stt[b][:, h, :], pb[6][:, h * 128:(h + 1) * 128], es[b][:, h:h + 1], U2f, ALU.mult, ALU.mult),
                             reads=[r_pb[6], r_es[b], r_cst], writes=[r_St[b]])
                    ca = cbi
                    for j in range(2):
                        ps = slice(64 * j, 64 * j + 64)
                        P.op("pe", [MM(pb[7][0:96, h * 97:(h + 1) * 97], kt[b][ps, h, :], vx[b][ps, h, :], True, True) for h in range(4)],
                             reads=[r_kt[b], r_vx[b]], writes=[r_pb[7]])
                        P.op("dve", TT(Ctmp[0:96], pb[7][0:96, 0:388].rearrange("p (h d) -> p h d", h=4), Cf[0:96], ALU.add),
                             reads=[r_pb[7], r_Cf], writes=[r_Ctmp])
                        P.op("dve", TT(Cf[0:96], Ctmp[0:96], ebl[b][0:96, j, :].unsqueeze(2).to_broadcast([96, 4, 97]), ALU.mult),
                             reads=[r_Ctmp, r_ebl[b]], writes=[r_Cf])
                        nxt = (cbi + 1) % NCB
                        P.op("act", ACT(Cb[nxt][0:96], Cf[0:96], AF.Copy), reads=[r_Cf], writes=[r_Cb[nxt]])
                        cbi = nxt
                    c_a = ca
                    c_b = (ca + 1) % NCB
                    accb = 0 if (T % 2 == 0) else 1
                    fns = []
                    for h in range(4):
                        o = pb[accb][:, h * 97:(h + 1) * 97]
                        fns.append(MM(o, Stt[b][:, h, :], vx[b][:, h, :], True, False, skip_group_check=True))
                        fns.append(MM(pb[accb][0:64, h * 97:(h + 1) * 97], qTg[0:96, h, ti * 128:ti * 128 + 64], Cb[c_a][0:96, h, :], False, False, skip_group_check=True))
                        fns.append(MM(pb[accb][64:128, h * 97:(h + 1) * 97], qTg[0:96, h, ti * 128 + 64:ti * 128 + 128], Cb[c_b][0:96, h, :], False, True,
                                      skip_group_check=True, tile_position=(0, 64)))
                    P.op("pe", fns, reads=[r_St[b], r_vx[b], r_qTg, r_Cb[c_a], r_Cb[c_b]], writes=[r_pb[accb]])
                    acc = pb[accb][:, 0:388].rearrange("p (h d) -> p h d", h=4)
                    P.op("act", ACT(den[b], acc[:, :, 96], AF.Abs), reads=[r_pb[accb]], writes=[r_den[b]])
                    P.op("dve", TT(den[b], den[b], emb[b], ALU.max), reads=[r_den[b], r_emb[b]], writes=[r_den[b]])
                    P.op("dve", (lambda e, b=b: e.reciprocal(out=den[b], in_=den[b])), reads=[r_den[b]], writes=[r_den[b]])
                    P.op("dve", TT(hraw[b][:], acc[:, :, 0:96], den[b].unsqueeze(2).to_broadcast([128, 4, 96]), ALU.mult),
                         reads=[r_pb[accb], r_den[b]], writes=[r_hraw[b]])
                    P.op("dve", TT(hsq[b][:], hraw[b][:], hraw[b][:], ALU.mult), reads=[r_hraw[b]], writes=[r_hsq[b]])
                    P.op("dve", (lambda e, b=b: e.tensor_reduce(out=ssq[b], in_=hsq[b][:], axis=AX.X, op=ALU.add)), reads=[r_hsq[b]], writes=[r_ssq[b]])
                    P.op("act", ACT(ssq[b], ssq[b], AF.Ln, scale=1.0 / 96, bias=1e-6), reads=[r_ssq[b]], writes=[r_ssq[b]])
                    P.op("act", ACT(ssq[b], ssq[b], AF.Exp, scale=-0.5), reads=[r_ssq[b]], writes=[r_ssq[b]])
                    P.op("dve", TT(gm[b], og[b], mnorm[:, l, :], ALU.mult), reads=[r_og[b], r_mnorm], writes=[r_gm[b]])
                    P.op("dve", TT(hraw[b][:], hraw[b][:], ssq[b].unsqueeze(2).to_broadcast([128, 4, 96]), ALU.mult),
                         reads=[r_hraw[b], r_ssq[b]], writes=[r_hraw[b]])
                    P.op("dve", TT(hAb[b], hraw[b][:].rearrange("p h d -> p (h d)"), gm[b], ALU.mult), reads=[r_hraw[b], r_gm[b]], writes=[r_hAb[b]])
                    pT = pb[5][:].bitcast(BF16)
                    P.op("pe", [TR(pT[:, 128 * c:128 * (c + 1)], hAb[b][:, 128 * c:128 * (c + 1)], identB[:]) for c in range(3)],
                         reads=[r_hAb[b], r_cb], writes=[r_pb[5]])
                    P.op("act", ACT(concat[:, 0:3, T * 128:(T + 1) * 128], pT[:, 0:384].rearrange("p (c t) -> p c t", c=3), AF.Copy),
                         reads=[r_pb[5]], writes=[r_cat[0][T], r_cat[1][T], r_cat[2][T]])

            P.mark("B")
            apos[0] = pl0
            sqb = carve(KC * TG, BF16).rearrange("p (c t) -> p c t", c=KC); r_sqb = P.res("sqb")
            lxs = [carve(TG + 4, F32) for _ in range(3)]; r_lxs = [P.res() for _ in range(3)]
            NB = 2
            xc = [carve(TG, F32) for _ in range(NB)]; r_xc = [P.res() for _ in range(NB)]
            xcb = [carve(TG, BF16) for _ in range(NB)]; r_xcb = [P.res() for _ in range(NB)]
            rr = [carve(TG, F32) for _ in range(NB)]; r_rr = [P.res() for _ in range(NB)]
            ii = [carve(TG, F32) for _ in range(NB)]; r_ii = [P.res() for _ in range(NB)]
            aa = [carve(TG, F32) for _ in range(NB)]; r_aa = [P.res() for _ in range(NB)]
            a2 = [carve(TG, F32) for _ in range(NB)]; r_a2 = [P.res() for _ in range(NB)]
            uu = [carve(TG, F32) for _ in range(NB)]; r_uu = [P.res() for _ in range(NB)]
            hs = [carve(TG, F32) for _ in range(NB)]; r_hs = [P.res() for _ in range(NB)]
            gl = [carve(TG, F32) for _ in range(NB)]; r_gl = [P.res() for _ in range(NB)]
            hprev = carve(4, F32); r_hprev = [P.res() for _ in range(3)]
            phaseB_res = [r_sqb] + r_lxs + r_xc + r_xcb + r_rr + r_ii + r_aa + r_a2 + r_uu + r_hs + r_gl + r_hprev
            barrier(phaseA_res + [r_W], phaseB_res + [r_W])
            weight_dma(W[:, :, 0:768], w_in_d[l, :, 1544:2312].rearrange("(c p) n -> p c n", p=128), r_W)
            for c in range(3):
                P.op("dve", MS(lxs[c][:, 0:4], 0.0), writes=[r_lxs[c]])
                P.op("dve", MS(hprev[:, c:c + 1], 0.0), writes=[r_hprev[c]])
            k = 0
            for g in range(NG):
                hb = g % 2
                make_h(l, 0, g, hT[hb], r_hT[hb])
                for c in range(3):
                    b = k % NB
                    k += 1
                    blx, blg, bra, brx = (0, 1, 2, 3) if b == 0 else (4, 5, 6, 7)
                    P.op("pe", [MM(pb[blx][:, :], W[:, kc, c * 128:(c + 1) * 128], hT[hb][:, kc, :], start=(kc == 0), stop=(kc == KC - 1)) for kc in range(KC)],
                         reads=[r_W, r_hT[hb]], writes=[r_pb[blx]])
                    P.op("pe", [MM(pb[blg][:, :], W[:, kc, 384 + c * 128:384 + (c + 1) * 128], hT[hb][:, kc, :], start=(kc == 0), stop=(kc == KC - 1)) for kc in range(KC)],
                         reads=[r_W, r_hT[hb]], writes=[r_pb[blg]])
                    P.op("act", ACT(lxs[c][:, 4:4 + TG], pb[blx][:, :], AF.Copy), reads=[r_pb[blx]], writes=[r_lxs[c]])
                    P.op("dve", TS(xc[b], lxs[c][:, 4:4 + TG], lruc[:, l, c, 3:4], lruc[:, l, c, 4:5], ALU.mult, ALU.add),
                         reads=[r_lxs[c], r_lruc], writes=[r_xc[b]])
                    for j in range(3):
                        P.op("dve", STT(xc[b], lxs[c][:, 1 + j:1 + j + TG], lruc[:, l, c, j:j + 1], xc[b], ALU.mult, ALU.add),
                             reads=[r_lxs[c], r_lruc, r_xc[b]], writes=[r_xc[b]])
                    P.op("dve", CP(lxs[c][:, 1:4], lxs[c][:, TG + 1:TG + 4]), reads=[r_lxs[c]], writes=[r_lxs[c]])
                    P.op("act", ACT(xcb[b], xc[b], AF.Copy), reads=[r_xc[b]], writes=[r_xcb[b]])
                    P.op("pe", MM(pb[bra][:, :], lruw[:, l, 0, c, :], xcb[b], True, True), reads=[r_lruw, r_xcb[b]], writes=[r_pb[bra]])
                    P.op("pe", MM(pb[brx][:, :], lruw[:, l, 1, c, :], xcb[b], True, True), reads=[r_lruw, r_xcb[b]], writes=[r_pb[brx]])
                    P.op("act", ACT(rr[b], pb[bra][:, :], AF.Sigmoid, bias=lruc[:, l, c, 5:6]), reads=[r_pb[bra], r_lruc], writes=[r_rr[b]])
                    P.op("act", ACT(ii[b], pb[brx][:, :], AF.Sigmoid, bias=lruc[:, l, c, 6:7]), reads=[r_pb[brx], r_lruc], writes=[r_ii[b]])
                    P.op("act", ACT(aa[b], rr[b], AF.Exp, scale=lrud[:, l, c, 0:1]), reads=[r_rr[b], r_lrud], writes=[r_aa[b]])
                    P.op("act", ACT(a2[b], rr[b], AF.Exp, scale=lrud[:, l, c, 1:2]), reads=[r_rr[b], r_lrud], writes=[r_a2[b]])
                    P.op("act", ACT(a2[b], a2[b], AF.Sqrt, scale=-1.0, bias=1.0), reads=[r_a2[b]], writes=[r_a2[b]])
                    P.op("dve", TT(uu[b], a2[b], ii[b], ALU.mult), reads=[r_a2[b], r_ii[b]], writes=[r_uu[b]])
                    P.op("dve", TT(uu[b], uu[b], xc[b], ALU.mult), reads=[r_uu[b], r_xc[b]], writes=[r_uu[b]])
                    P.mark("scan-next")
                    SN = int(os.environ.get("K_SN", "128"))
                    for q0 in range(0, TG, SN):
                        ini = hprev[:, c:c + 1] if q0 == 0 else hs[b][:, q0 - 1:q0]
                        P.op("dve", (lambda e, b=b, ini=ini, q0=q0: e.tensor_tensor_scan(out=hs[b][:, q0:q0 + SN], data0=aa[b][:, q0:q0 + SN], data1=uu[b][:, q0:q0 + SN],
                                                                                       initial=ini, op0=ALU.mult, op1=ALU.add)),
                             reads=[r_aa[b], r_uu[b], r_hprev[c], r_hs[b]], writes=[r_hs[b]])
                    P.op("dve", CP(hprev[:, c:c + 1], hs[b][:, TG - 1:TG]), reads=[r_hs[b]], writes=[r_hprev[c]])
                    P.op("act", ACT(gl[b], pb[blg][:, :], AF.Gelu_apprx_tanh), reads=[r_pb[blg]], writes=[r_gl[b]])
                    P.op("dve", TT(concat[:, 3 + c, g * TG:(g + 1) * TG], hs[b], gl[b], ALU.mult), reads=[r_hs[b], r_gl[b]],
                         writes=[r_cat[3 + c][4 * g + i] for i in range(4)])

            P.mark("C")
            apos[0] = pl0
            akT = carve(2 * S, BF16).rearrange("p (c t) -> p c t", c=2); r_akT = [P.res() for _ in range(NG)]
            ikT = carve(S, BF16); r_ikT = [P.res() for _ in range(NG)]
            avx = carve(16 * 4 * 65, BF16).rearrange("p (t h d) -> p t h d", t=16, h=4); r_avx = [P.res() for _ in range(16)]
            iwt = carve(16 * 4, F32).rearrange("p (t h) -> p t h", t=16); r_iwt = [P.res() for _ in range(16)]
            aqT = carve(2 * TG, BF16).rearrange("p (c t) -> p c t", c=2); r_aqT = P.res("aqT")
            iqT = carve(2 * TG, BF16).rearrange("p (c t) -> p c t", c=2); r_iqT = P.res("iqT")
            sc = carve(S, F32); r_sc = P.res("sc")
            msk = carve(S, BF16); r_msk = P.res("msk")
            junk = msk; r_junk = r_msk
            mT = carve(S, BF16).rearrange("p (j q) -> p j q", j=16); r_mT = P.res("mT")
            rlb = [carve(TG, F32) for _ in range(2)]; r_rlb = [P.res() for _ in range(2)]
            exb = [carve(TG, BF16) for _ in range(2)]; r_exb = [P.res() for _ in range(2)]
            pTb = [carve(TG, BF16) for _ in range(2)]; r_pTb = [P.res() for _ in range(2)]
            bis = carve(8 + 2 * NBIS, F32); r_bis = P.res("bis")
            rc4 = carve(4, F32); r_rc4 = P.res("rc4")
            hcb = carve(256, BF16); r_hcb = P.res("hcb")
            phaseC_res = (r_akT + r_ikT + r_avx + r_iwt + [r_aqT, r_iqT, r_sc, r_msk, r_mT, r_bis, r_rc4, r_hcb] + r_rlb + r_exb + r_pTb)
            barrier(phaseB_res + [r_W], phaseC_res + [r_W])
            wsrc = w_in_d[l]
            r3 = lambda a, b_: wsrc[:, a:b_].rearrange("(c p) n -> p c n", p=128)
            weight_dma(W[:, :, 0:512], r3(2312, 2824), r_W)
            weight_dma(W[:, :, 512:768], r3(3080, 3336), r_W)
            weight_dma(W[:, :, 768:832], r3(3336, 3400), r_W)
            weight_dma(W[:, :, 832:896], r3(3336, 3400), r_W)
            weight_dma(W[:, :, 896:1152], r3(2824, 3080), r_W)
            weight_dma(W[:, :, 1152:1156], r3(3400, 3404), r_W)
            for T in range(16):
                P.op("dve", MS(avx[:, T, :, 64:65], 1.0), writes=[r_avx[T]])
            for g in range(NG):
                hb = g % 2
                make_h(l, 0, g, hT[hb], r_hT[hb])
                tokg = slice(g * TG, (g + 1) * TG)
                plan = [(0, aqT[:, 0, :], [r_aqT]), (128, aqT[:, 1, :], [r_aqT]), (256, akT[:, 0, tokg], [r_akT[g]]), (384, akT[:, 1, tokg], [r_akT[g]]),
                        (512, iqT[:, 0, :], [r_iqT]), (640, iqT[:, 1, :], [r_iqT]), (768, ikT[:, tokg], [r_ikT[g]])]
                for i, (c0, dst, rw) in enumerate(plan):
                    bank = i % 2
                    P.op("pe", [MM(pb[bank][:, :], W[:, kc, c0:c0 + 128], hT[hb][:, kc, :], start=(kc == 0), stop=(kc == KC - 1)) for kc in range(KC)],
                         reads=[r_W, r_hT[hb]], writes=[r_pb[bank]])
                    P.op("act", ACT(dst, pb[bank][:, :], AF.Copy), reads=[r_pb[bank]], writes=rw)
                for ti in range(4):
                    T = 4 * g + ti
                    bank = 2 + (ti % 2)
                    P.op("pe", [MM(pb[bank][:, 0:260], hT[hb][:, kc, ti * 128:(ti + 1) * 128], W[:, kc, 896:1156], start=(kc == 0), stop=(kc == KC - 1)) for kc in range(KC)],
                         reads=[r_W, r_hT[hb]], writes=[r_pb[bank]])
                    P.op("act", ACT(avx[:, T, :, 0:64], pb[bank][:, 0:256].rearrange("p (h d) -> p h d", h=4), AF.Copy), reads=[r_pb[bank]], writes=[r_avx[T]])
                    P.op("act", ACT(iwt[:, T, :], pb[bank][:, 256:260], AF.Copy), reads=[r_pb[bank]], writes=[r_iwt[T]])
                for ti in range(4):
                    T = 4 * g + ti
                    nk = 128 * (T + 1)
                    ql = slice(ti * 128, (ti + 1) * 128)
                    nkb = (nk + 511) // 512
                    key_res_k = [r_ikT[gg] for gg in range(g + 1)]
                    cnt = 0
                    for h in range(4):
                        hp = slice(64 * (h % 2), 64 * (h % 2) + 64)
                        for kb in range(nkb):
                            k0 = kb * 512
                            w = min(512, nk - k0)
                            bank = 4 + (cnt % 2)
                            rb = cnt % 2
                            cnt += 1
                            P.op("pe", MM(pb[bank][:, 0:w], iqT[hp, h // 2, ql], ikT[hp, k0:k0 + w], True, True),
                                 reads=[r_iqT] + key_res_k, writes=[r_pb[bank]])
                            P.op("act", ACT(rlb[rb][:, 0:w], pb[bank][:, 0:w], AF.Relu), reads=[r_pb[bank]], writes=[r_rlb[rb]])
                            if h == 0:
                                P.op("dve", TS(sc[:, k0:k0 + w], rlb[rb][:, 0:w], iwt[:, T, 0:1]), reads=[r_rlb[rb], r_iwt[T]], writes=[r_sc])
                            else:
                                P.op("dve", STT(sc[:, k0:k0 + w], rlb[rb][:, 0:w], iwt[:, T, h:h + 1], sc[:, k0:k0 + w], ALU.mult, ALU.add),
                                     reads=[r_rlb[rb], r_iwt[T], r_sc], writes=[r_sc])
                    if T >= 2:
                        P.op("dve", (lambda e, nk=nk: e.tensor_reduce(out=bis[:, 0:1], in_=sc[:, 0:nk], axis=AX.X, op=ALU.max)), reads=[r_sc], writes=[r_bis])
                        P.op("dve", (lambda e, nk=nk: e.tensor_reduce(out=bis[:, 1:2], in_=sc[:, 0:nk], axis=AX.X, op=ALU.min)), reads=[r_sc], writes=[r_bis])
                    P.op("dve", MS(sc[0:64, nk - 64:nk], -1e30), reads=[r_sc], writes=[r_sc])
                    if T >= 2:
                        P.op("dve", TT(bis[:, 2:3], bis[:, 0:1], bis[:, 1:2], ALU.subtract), reads=[r_bis], writes=[r_bis])
                        P.op("dve", TS(bis[:, 8:8 + NBIS], pow2, bis[:, 2:3]), reads=[r_bis, r_cst], writes=[r_bis])
                        P.op("dve", TS(bis[:, 8 + NBIS:8 + 2 * NBIS], bis[:, 8:8 + NBIS], -0.5), reads=[r_bis], writes=[r_bis])
                        P.op("dve", TT(bis[:, 3:4], bis[:, 1:2], bis[:, 8:9], ALU.add), reads=[r_bis], writes=[r_bis])
                        for kk in range(NBIS):
                            P.op("dve", (lambda e, nk=nk: e.tensor_scalar(out=junk[:, 0:nk], in0=sc[:, 0:nk], scalar1=bis[:, 3:4], scalar2=0.0,
                                                                            op0=ALU.is_ge, op1=ALU.add, accum_out=bis[:, 4:5])),
                                 reads=[r_sc, r_bis], writes=[r_msk, r_bis])
                            P.op("dve", STT(bis[:, 5:6], bis[:, 4:5], 255.5, bis[:, 8 + kk:9 + kk], ALU.is_ge, ALU.mult), reads=[r_bis], writes=[r_bis])
                            P.op("dve", STT(bis[:, 3:4], bis[:, 5:6], bis[:, 8 + NBIS + kk:9 + NBIS + kk], bis[:, 3:4], ALU.add, ALU.add), reads=[r_bis], writes=[r_bis])
                        P.op("dve", TT(bis[:, 3:4], bis[:, 3:4], bis[:, 8 + 2 * NBIS - 1:8 + 2 * NBIS], ALU.add), reads=[r_bis], writes=[r_bis])
                        P.op("dve", TS(msk[:, 0:nk], sc[:, 0:nk], bis[:, 3:4], None, ALU.is_ge), reads=[r_sc, r_bis], writes=[r_msk])
                    else:
                        P.op("dve", TS(msk[:, 0:nk], sc[:, 0:nk], -1e29, None, ALU.is_ge), reads=[r_sc], writes=[r_msk])
                    nb_ = T + 1
                    for j0 in range(0, nb_, 8):
                        bank = 6 + ((j0 // 8) % 2)
                        pT = pb[bank][:].bitcast(BF16)
                        n = min(8, nb_ - j0)
                        P.op("pe", [TR(pT[:, 128 * i:128 * (i + 1)], msk[:, (j0 + i) * 128:(j0 + i + 1) * 128], identB[:]) for i in range(n)],
                             reads=[r_msk, r_cb], writes=[r_pb[bank]])
                        P.op("act", ACT(mT[:, j0:j0 + n, :], pT[:, 0:128 * n].rearrange("p (j q) -> p j q", j=n), AF.Copy), reads=[r_pb[bank]], writes=[r_mT])
                    key_res_a = [r_akT[gg] for gg in range(g + 1)]
                    cnt = 0
                    for h in range(4):
                        hp = slice(64 * (h % 2), 64 * (h % 2) + 64)
                        groups = [(j0, min(4, nb_ - j0)) for j0 in range(0, nb_, 4)]
                        for gi_, (j0, n) in enumerate(groups):
                            bank = 4 + (cnt % 2)
                            eb = cnt % 2
                            cnt += 1
                            P.op("pe", [MM(pb[bank][:, 128 * i:128 * (i + 1)], akT[hp, h // 2, (j0 + i) * 128:(j0 + i + 1) * 128], aqT[hp, h // 2, ql], True, True) for i in range(n)],
                                 reads=[r_aqT] + key_res_a, writes=[r_pb[bank]])
                            P.op("act", ACT(exb[eb][:, 0:128 * n], pb[bank][:, 0:128 * n], AF.Exp, scale=0.125), reads=[r_pb[bank]], writes=[r_exb[eb]])
                            P.op("dve", TT(pTb[eb][:, 0:128 * n], exb[eb][:, 0:128 * n], mT[:, j0:j0 + n, :].rearrange("p j q -> p (j q)"), ALU.mult),
                                 reads=[r_exb[eb], r_mT], writes=[r_pTb[eb]])
                            P.op("pe", [MM(pb[3][:, h * 65:(h + 1) * 65], pTb[eb][:, 128 * i:128 * (i + 1)], avx[:, j0 + i, h, :],
                                           start=(j0 + i == 0), stop=(j0 + i == nb_ - 1), skip_group_check=True) for i in range(n)],
                                 reads=[r_pTb[eb]] + [r_avx[j0 + i] for i in range(n)], writes=[r_pb[3]])
                    oacc = pb[3][:, 0:260].rearrange("p (h d) -> p h d", h=4)
                    P.op("dve", (lambda e: e.reciprocal(out=rc4, in_=oacc[:, :, 64])), reads=[r_pb[3]], writes=[r_rc4])
                    P.op("dve", TT(hcb.rearrange("p (h d) -> p h d", h=4), oacc[:, :, 0:64], rc4.unsqueeze(2).to_broadcast([128, 4, 64]), ALU.mult),
                         reads=[r_pb[3], r_rc4], writes=[r_hcb])
                    pT = pb[2][:].bitcast(BF16)
                    P.op("pe", [TR(pT[:, 128 * c:128 * (c + 1)], hcb[:, 128 * c:128 * (c + 1)], identB[:]) for c in range(2)],
                         reads=[r_hcb, r_cb], writes=[r_pb[2]])
                    P.op("act", ACT(concat[:, 6:8, T * 128:(T + 1) * 128], pT[:, 0:256].rearrange("p (c t) -> p c t", c=2), AF.Copy),
                         reads=[r_pb[2]], writes=[r_cat[6][T], r_cat[7][T]])

            P.mark("Cend")
            if dbg:
                P.force = True
            apos[0] = pl0
            if dbg and l == dbg - 1:
                dtmp = carve(S, F32); r_dtmp = P.res("dtmp")
                barrier(phaseC_res, [r_dtmp])
                for c in range(KC):
                    P.op("dve", CP(dtmp[:], concat[:, c, :]), reads=[r_cat[c][t] for t in range(16)], writes=[r_dtmp])
                    P.dma("sp", (lambda e, c=c: e.dma_start(out=dbg_d[c * 128:(c + 1) * 128, :], in_=dtmp[:])), reads=[r_dtmp])

            if stop_after == "C" and l == depth - 1:
                break
            NTD = 256
            sqd = carve(KC * NTD, BF16).rearrange("p (c t) -> p c t", c=KC); r_sqd = P.res("sqd")
            rsd = carve(NTD, F32); r_rsd = P.res("rsd")
            tmpd = carve(NTD, F32); r_tmpd = P.res("tmpd")
            phaseD_res = [r_sqd, r_rsd, r_tmpd]
            barrier(phaseC_res + [r_W], phaseD_res + [r_W])
            weight_dma(W[:, :, 0:1024], w_out_d[l].rearrange("(c p) n -> p c n", p=128), r_W)
            for gd in range(S // NTD):
                t0 = gd * NTD
                tiles = [t0 // 128, t0 // 128 + 1]
                for c in range(KC):
                    bank = c // 2
                    half = (c % 2) * NTD
                    P.op("pe", [MM(pb[bank][:, half:half + NTD], W[:, kc, c * 128:(c + 1) * 128], concat[:, kc, t0:t0 + NTD], start=(kc == 0), stop=(kc == KC - 1)) for kc in range(KC)],
                         reads=[r_W] + [r_cat[kc][t] for kc in range(KC) for t in tiles], writes=[r_pb[bank]])
                srcs = [pb[c // 2][:, (c % 2) * NTD:(c % 2) * NTD + NTD] for c in range(KC)]
                post_norm_residual(l, 1, t0, NTD, srcs, [r_pb[c // 2] for c in range(KC)], sqd, r_sqd, 4, rsd, r_rsd, tmpd, r_tmpd)

            if stop_after == "D" and l == depth - 1:
                break

            P.skip = False
            P.mark("E")
            apos[0] = 0
            hTf = [carve(KC * TG, BF16).rearrange("p (c t) -> p c t", c=KC) for _ in range(2)]; r_hTf = [P.res() for _ in range(2)]
            sqf = carve(KC * TG, BF16).rearrange("p (c t) -> p c t", c=KC); r_sqf = P.res("sqf")
            actg = carve(NFC * TG, BF16).rearrange("p (k t) -> p k t", k=NFC); r_actg = [P.res() for _ in range(NFC)]
            ost = carve(KC * TG, F32).rearrange("p (c t) -> p c t", c=KC); r_ost = [P.res() for _ in range(KC)]
            NWU = 2
            wu = [carve(KC * 2 * 256, BF16).rearrange("p (c s n) -> p c s n", c=KC, s=2) for _ in range(NWU)]; r_wu = [P.res() for _ in range(NWU)]
            NWD = 2
            wd = [carve(NFC * 256, BF16).rearrange("p (k n) -> p k n", k=NFC) for _ in range(NWD)]; r_wd = [P.res() for _ in range(NWD)]
            NU = 2
            ub = [[carve(TG + 2, F32) for _ in range(2)] for _ in range(NU)]; r_ub = [[P.res() for _ in range(2)] for _ in range(NU)]
            cv = [[carve(TG, F32) for _ in range(2)] for _ in range(NU)]; r_cv = [[P.res() for _ in range(2)] for _ in range(NU)]
            uh = carve(44 * 2, F32).rearrange("p (f t) -> p f t", f=44); r_uh = [P.res() for _ in range(44)]
            rsf = carve(TG, F32); r_rsf = P.res("rsf")
            tmpf = carve(TG, F32); r_tmpf = P.res("tmpf")
            phaseE_res = (r_hTf + [r_sqf, r_rsf, r_tmpf] + r_actg + r_ost + r_wu + r_wd + [x for y in r_ub for x in y] + [x for y in r_cv for x in y] + r_uh)
            barrier(all_mixer_res + phaseD_res, phaseE_res)
            P.op("dve", MS(uh[:].rearrange("p f t -> p (f t)"), 0.0), writes=r_uh)
            wu_i = 0
            wd_i = 0
            up_d = ffn_up_d[l]
            dn_d = ffn_down_d[l]
            pair_k = 0
            for g in range(NG):
                hb = g % 2
                rms_stats(l, g, sqf, r_sqf, 7)
                make_h(l, 2, g, hTf[hb], r_hTf[hb])
                for j2 in range(NFC // 2):
                    ws = wu_i % NWU
                    wu_i += 1
                    weight_dma(wu[ws][:, :, 0, :], up_d[:, j2 * 256:(j2 + 1) * 256].rearrange("(c p) n -> p c n", p=128), r_wu[ws])
                    weight_dma(wu[ws][:, :, 1, :], up_d[:, DFF + j2 * 256:DFF + (j2 + 1) * 256].rearrange("(c p) n -> p c n", p=128), r_wu[ws])
                    for jj in range(2):
                        j = 2 * j2 + jj
                        u = pair_k % NU
                        pair_k += 1
                        bg, bu = (0, 1) if u == 0 else (2, 3)
                        for s_, bank in ((0, bg), (1, bu)):
                            P.op("pe", [MM(pb[bank][:, :], wu[ws][:, kc, s_, jj * 128:(jj + 1) * 128], hTf[hb][:, kc, :], start=(kc == 0), stop=(kc == KC - 1)) for kc in range(KC)],
                                 reads=[r_wu[ws], r_hTf[hb]], writes=[r_pb[bank]])
                        for s_, bank in ((0, bg), (1, bu)):
                            f = j + s_ * NFC
                            ubx = ub[u][s_]; rub = r_ub[u][s_]
                            cvx = cv[u][s_]; rcv = r_cv[u][s_]
                            P.op("act", ACT(ubx[:, 2:2 + TG], pb[bank][:, :], AF.Copy), reads=[r_pb[bank]], writes=[rub])
                            P.op("act", ACT(ubx[:, 0:2], uh[:, f, :], AF.Copy), reads=[r_uh[f]], writes=[rub])
                            P.op("dve", TS(cvx, ubx[:, 2:2 + TG], fconv[:, l, f, 2:3], fconv[:, l, f, 3:4], ALU.mult, ALU.add),
                                 reads=[rub, r_fconv], writes=[rcv])
                            P.op("dve", STT(cvx, ubx[:, 1:1 + TG], fconv[:, l, f, 1:2], cvx, ALU.mult, ALU.add), reads=[rub, r_fconv, rcv], writes=[rcv])
                            P.op("dve", STT(cvx, ubx[:, 0:TG], fconv[:, l, f, 0:1], cvx, ALU.mult, ALU.add), reads=[rub, r_fconv, rcv], writes=[rcv])
                            P.op("act", ACT(uh[:, f, :], ubx[:, TG:TG + 2], AF.Copy), reads=[rub], writes=[r_uh[f]])
                        P.op("act", ACT(cv[u][0], cv[u][0], AF.Gelu_apprx_tanh), reads=[r_cv[u][0]], writes=[r_cv[u][0]])
                        P.op("dve", TT(actg[:, j, :], cv[u][0], cv[u][1], ALU.mult), reads=[r_cv[u][0], r_cv[u][1]], writes=[r_actg[j]])
                for c2 in range(4):
                    ws = wd_i % NWD
                    wd_i += 1
                    for k0_ in (0, 11):
                        weight_dma(wd[ws][:, k0_:k0_ + 11, :], dn_d[k0_ * 128:(k0_ + 11) * 128, c2 * 256:(c2 + 1) * 256].rearrange("(k p) n -> p k n", p=128), r_wd[ws])
                    for cc in range(2):
                        c = 2 * c2 + cc
                        bank = 4 + (c % 2)
                        P.op("pe", [MM(pb[bank][:, :], wd[ws][:, k_, cc * 128:(cc + 1) * 128], actg[:, k_, :], start=(k_ == 0), stop=(k_ == NFC - 1)) for k_ in range(NFC)],
                             reads=[r_wd[ws]] + r_actg, writes=[r_pb[bank]])
                        P.op("act", ACT(ost[:, c, :], pb[bank][:, :], AF.Copy), reads=[r_pb[bank]], writes=[r_ost[c]])
                post_norm_residual(l, 3, g * TG, TG, [ost[:, c, :] for c in range(KC)], r_ost, sqf, r_sqf, 6, rsf, r_rsf, tmpf, r_tmpf)
            prev_phase_res = phaseE_res

        P.force = True
        for c in range(KC):
            for g in range(NG):
                P.dma("sp", (lambda e, c=c, g=g: e.dma_start(out=yT_d[c * 128:(c + 1) * 128, g * TG:(g + 1) * TG], in_=xT[:, c, g * TG:(g + 1) * TG])),
                      reads=[r_x[c][g]])
        P.wait_all_dma("sp", [r_x[c][g] for c in range(KC) for g in range(NG)] + ([r_dtmp] if dbg else []))
        P.emit()
    return nc


def host_params(inputs):
    f = np.float32
    L = DEPTH
    gains = np.zeros((128, L, 4, 8), f)
    for l in range(L):
        for i, nm in enumerate(("norm_mix_pre", "norm_mix_post", "norm_ffn_pre", "norm_ffn_post")):
            gains[:, l, i, :] = np.asarray(inputs[nm][l], f).reshape(8, 128).T
    bgate = np.zeros((128, L, 8), f)
    bgate[:, :, 0:4] = np.asarray(inputs["b_igate"], f)[None]
    bgate[:, :, 4:8] = np.asarray(inputs["b_fgate"], f)[None]
    mnorm = np.broadcast_to(np.asarray(inputs["mlstm_norm"], f)[None], (128, L, 384)).copy()
    lruc = np.zeros((128, L, 3, 8), f)
    for l in range(L):
        for j in range(4):
            lruc[:, l, :, j] = np.asarray(inputs["lru_conv_w"][l, j], f).reshape(3, 128).T
        lruc[:, l, :, 4] = np.asarray(inputs["lru_conv_b"][l], f).reshape(3, 128).T
        lruc[:, l, :, 5] = np.asarray(inputs["lru_b_a"][l], f).reshape(3, 128).T
        lruc[:, l, :, 6] = np.asarray(inputs["lru_b_x"][l], f).reshape(3, 128).T
        lruc[:, l, :, 7] = np.asarray(inputs["lru_lambda"][l], f).reshape(3, 128).T
    lruw = np.zeros((128, L, 2, 3, 128), f)
    for l in range(L):
        for i, nm in enumerate(("lru_w_a", "lru_w_x")):
            w = np.asarray(inputs[nm][l], f)
            for c in range(3):
                for a in range(2):
                    lruw[64 * a:64 * a + 64, l, i, c, 64 * a:64 * a + 64] = w[2 * c + a]
    fconv = np.zeros((128, L, 44, 4), f)
    for l in range(L):
        for j in range(3):
            fconv[:, l, :, j] = np.asarray(inputs["ffn_conv_w"][l, j], f).reshape(44, 128).T
        fconv[:, l, :, 3] = np.asarray(inputs["ffn_conv_b"][l], f).reshape(44, 128).T
    cst = np.zeros((128, 128 + 128 + 192 + NBIS), f)
    cst[:, 0:128] = np.eye(128, dtype=f)
    for s in range(128):
        for t in range(128):
            if s // 64 == t // 64 and s <= t:
                cst[s, 128 + t] = 1.0
    cst[0:64, 256:352] = 1.0
    cst[64:128, 352:448] = 1.0
    cst[:, 448:448 + NBIS] = (2.0 ** -(np.arange(NBIS) + 1.0))[None, :]
    return {"gains": gains.reshape(128, -1), "bgate": bgate.reshape(128, -1), "mnorm": mnorm.reshape(128, -1), "lruc": lruc.reshape(128, -1),
            "lruw": lruw.reshape(128, -1), "fconv": fconv.reshape(128, -1), "cst": cst}


_NC_CACHE = {}


def kernel(**inputs):
    x = np.asarray(inputs["x"], np.float32)
    B = x.shape[0]
    hp = host_params(inputs)
    shared = {"w_in": np.ascontiguousarray(inputs["w_in"], np.float32), "w_out": np.ascontiguousarray(inputs["w_out"], np.float32),
              "ffn_up": np.ascontiguousarray(inputs["ffn_up"], np.float32), "ffn_down": np.ascontiguousarray(inputs["ffn_down"], np.float32)}
    shared.update(hp)
    if "nc" not in _NC_CACHE:
        _NC_CACHE["nc"] = build()
    nc = _NC_CACHE["nc"]
    in_maps = []
    for b in range(B):
        m = dict(shared)
        m["xT"] = np.ascontiguousarray(x[b].T)
        in_maps.append(m)
    res = run_bass_kernel_spmd(nc, in_maps, core_ids=list(range(B)))
    out = np.stack([np.ascontiguousarray(res.results[b]["yT"].T) for b in range(B)], axis=0)
    return out.astype(np.float32)
```

```python
import os
from contextlib import ExitStack
import numpy as np
import concourse.bass as bass
import concourse.mybir as mybir
from concourse.bass_utils import run_bass_kernel_spmd

F32 = mybir.dt.float32
BF16 = mybir.dt.bfloat16
AF = mybir.ActivationFunctionType
ALU = mybir.AluOpType
AX = mybir.AxisListType

S = 2048
D = 1024
DEPTH = 2
KC = 8
TG = 512
NG = S // TG
D_IN = 3404
DFF = 2816
NFC = 22
NBIS = 14
ENGS = ("pe", "act", "dve", "pool", "sp")


class Res:
    __slots__ = ("name", "w", "r", "dsem", "dcnt")

    def __init__(self, name):
        self.name = name
        self.w = None
        self.r = {}
        self.dsem = None
        self.dcnt = 0


class Prog:
    def __init__(self, nc, stack):
        self.nc = nc
        self.stack = stack
        self.items = {e: [] for e in ENGS}
        self.cnt = {e: 0 for e in ENGS}
        self.sem = {e: stack.enter_context(nc.semaphore("sem_" + e)) for e in ENGS if e != "sp"}
        self.known = {e: {f: 0 for f in ENGS} for e in ENGS}
        self.vc = {e: [None] for e in ENGS}
        self.dknown = {e: {} for e in ENGS}
        self.nres = 0
        self.ntot = 0
        self.limit = int(os.environ.get("K_LIMIT", "0")) or None
        self.force = False

    def mark(self, name):
        if os.environ.get("K_MARK"):
            print("MARK", name, self.ntot)

    def res(self, name=None):
        self.nres += 1
        return Res(name or ("r%d" % self.nres))

    def _dsem(self, r):
        if r.dsem is None:
            self.nds = getattr(self, "nds", 0) + 1
            r.dsem = self.stack.enter_context(self.nc.semaphore("d%d_%s" % (self.nds, r.name)))
        return r.dsem

    def _deps(self, eng, reads, writes):
        deps = {}
        dd = []

        def add(e_i):
            if e_i is None:
                return
            e, i = e_i
            if e == eng and eng == "pe":
                return
            if deps.get(e, 0) < i:
                deps[e] = i
        for r in reads:
            add(r.w)
            if r.dcnt:
                dd.append(r)
        for r in writes:
            add(r.w)
            for e, i in r.r.items():
                add((e, i))
            if r.dcnt:
                dd.append(r)
        waits = []
        kn = self.known[eng]
        for e, i in deps.items():
            if kn[e] < i:
                waits.append((self.sem[e], i))
                v = self.vc[e][i]
                for f in ENGS:
                    if kn[f] < v[f]:
                        kn[f] = v[f]
        dk = self.dknown[eng]
        for r in dd:
            if dk.get(id(r), 0) < r.dcnt:
                waits.append((self._dsem(r), r.dcnt))
                dk[id(r)] = r.dcnt
        return waits

    def op(self, eng, fns, reads=(), writes=()):
        if getattr(self, "skip", False):
            return
        self.ntot += 1
        if self.limit and self.ntot > self.limit and not self.force:
            return
        if not isinstance(fns, (list, tuple)):
            fns = [fns]
        waits = self._deps(eng, reads, writes)
        self.cnt[eng] += 1
        idx = self.cnt[eng]
        v = dict(self.known[eng])
        v[eng] = idx
        self.vc[eng].append(v)
        for r in reads:
            r.r[eng] = idx
        for r in writes:
            r.w = (eng, idx)
            r.r = {}
        self.items[eng].append((waits, fns, ("c", self.sem[eng])))
        return idx

    def dma(self, q, fn, reads=(), writes=()):
        if getattr(self, "skip", False):
            return
        self.ntot += 1
        if self.limit and self.ntot > self.limit and not self.force:
            return
        waits = self._deps(q, reads, writes)
        rs = list(reads) + list(writes)
        assert len(rs) == 1
        sem = self._dsem(rs[0])
        rs[0].dcnt += 16
        for r in writes:
            r.w = None
            r.r = {}
        self.items[q].append((waits, [fn], ("d", sem)))

    def wait_all_dma(self, eng, ress):
        waits = []
        for r in ress:
            if r.dcnt:
                waits.append((self._dsem(r), r.dcnt))
        self.items[eng].append((waits, [], None))

    def emit(self):
        nc = self.nc
        with nc.Block() as block:
            def run(e, items):
                for waits, fns, inc in items:
                    for s, v in waits:
                        e.wait_ge(s, v)
                    last = None
                    for f in fns:
                        last = f(e)
                    if inc is not None:
                        kind, s = inc
                        last.then_inc(s, 1 if kind == "c" else 16)

            @block.tensor
            def _(e):
                run(e, self.items["pe"])

            @block.scalar
            def _(e):
                run(e, self.items["act"])

            @block.vector
            def _(e):
                run(e, self.items["dve"])

            @block.gpsimd
            def _(e):
                run(e, self.items["pool"])

            @block.sync
            def _(e):
                run(e, self.items["sp"])


def MM(out, lhsT, rhs, start=True, stop=True, **kw):
    return lambda e: e.matmul(out=out, lhsT=lhsT, rhs=rhs, start=start, stop=stop, **kw)


def TR(out, in_, ident):
    return lambda e: e.transpose(out=out, in_=in_, identity=ident)


def ACT(out, in_, func, **kw):
    return lambda e: e.activation(out=out, in_=in_, func=func, **kw)


def TS(out, in0, s1, s2=None, op0=ALU.mult, op1=None, **kw):
    if op1 is None:
        return lambda e: e.tensor_scalar(out=out, in0=in0, scalar1=s1, scalar2=None, op0=op0, **kw)
    return lambda e: e.tensor_scalar(out=out, in0=in0, scalar1=s1, scalar2=s2, op0=op0, op1=op1, **kw)


def STT(out, in0, scalar, in1, op0, op1):
    return lambda e: e.scalar_tensor_tensor(out=out, in0=in0, scalar=scalar, in1=in1, op0=op0, op1=op1)


def TT(out, in0, in1, op):
    return lambda e: e.tensor_tensor(out=out, in0=in0, in1=in1, op=op)


def CP(out, in_):
    return lambda e: e.tensor_copy(out=out, in_=in_)


def MS(ap, v):
    return lambda e: e.memset(ap, v)


def build(depth=DEPTH, dbg=0, stop_after=None):
    nc = bass.Bass("TRN2", target_bir_lowering=False)
    dt_in = lambda n, s: nc.dram_tensor(n, list(s), F32, kind="ExternalInput").ap()
    xT_d = dt_in("xT", [D, S])
    w_in_d = dt_in("w_in", [DEPTH, D, D_IN])
    w_out_d = dt_in("w_out", [DEPTH, D, D])
    ffn_up_d = dt_in("ffn_up", [DEPTH, D, 2 * DFF])
    ffn_down_d = dt_in("ffn_down", [DEPTH, DFF, D])
    gains_d = dt_in("gains", [128, DEPTH * 4 * 8])
    bgate_d = dt_in("bgate", [128, DEPTH * 8])
    mnorm_d = dt_in("mnorm", [128, DEPTH * 384])
    lruc_d = dt_in("lruc", [128, DEPTH * 3 * 8])
    lruw_d = dt_in("lruw", [128, DEPTH * 2 * 3 * 128])
    fconv_d = dt_in("fconv", [128, DEPTH * 44 * 4])
    cst_d = dt_in("cst", [128, 128 + 128 + 192 + NBIS])
    yT_d = nc.dram_tensor("yT", [D, S], F32, kind="ExternalOutput").ap()
    if dbg:
        dbg_d = nc.dram_tensor("dbg", [D, S], F32, kind="ExternalOutput").ap()

    with ExitStack() as st:
        P = Prog(nc, st)
        sbt = lambda name, shape, dt: st.enter_context(nc.sbuf_tensor(name, list(shape), dt))
        pst = lambda name, shape, dt: st.enter_context(nc.psum_tensor(name, list(shape), dt))

        xT = sbt("xT_sb", [128, KC, S], F32)
        r_x = [[P.res("x%d_%d" % (c, g)) for g in range(NG)] for c in range(KC)]
        rstdb = sbt("rstdb", [128, S], F32)
        r_rstd = [P.res("rstd%d" % g) for g in range(NG)]
        gains = sbt("gains_sb", [128, DEPTH, 4, 8], F32); r_par = P.res("par")
        bgate = sbt("bgate_sb", [128, DEPTH, 8], F32)
        mnorm = sbt("mnorm_sb", [128, DEPTH, 384], F32)
        lruc = sbt("lruc_sb", [128, DEPTH, 3, 8], F32)
        lruw = sbt("lruw_sb", [128, DEPTH, 2, 3, 128], BF16); r_lruw = P.res("lruw")
        fconv = sbt("fconv_sb", [128, DEPTH, 44, 4], F32)
        cst = sbt("cst_sb", [128, 128 + 128 + 192 + NBIS], F32)
        identF = cst[:, 0:128]
        U2f = cst[:, 128:256]
        Ef = cst[:, 256:448]
        pow2 = cst[:, 448:448 + NBIS]
        identB = sbt("identB", [128, 128], BF16); r_cb = P.res("cstb")
        onesB = sbt("onesB", [128, 128], BF16)
        lrud = sbt("lrud", [128, DEPTH, 3, 4], F32); r_lrud = P.res("lrud")

        ARENA = 64000
        arena = sbt("arena", [128, ARENA], BF16)
        apos = [0]

        def carve(nelem, dt, shape=None):
            n16 = nelem * (2 if dt == F32 else 1)
            a = apos[0]
            if dt == F32 and a % 2:
                a += 1
            apos[0] = a + n16
            assert apos[0] <= ARENA, ("arena overflow", apos[0], ARENA)
            v = arena[:, a:a + n16]
            if dt == F32:
                v = v.bitcast(F32)
            return v

        pb = [pst("pb%d" % i, [128, 512], F32) for i in range(8)]
        r_pb = [P.res("pb%d" % i) for i in range(8)]

        for c in range(KC):
            for g in range(NG):
                P.dma("sp", (lambda e, c=c, g=g: e.dma_start(out=xT[:, c, g * TG:(g + 1) * TG], in_=xT_d[c * 128:(c + 1) * 128, g * TG:(g + 1) * TG])),
                      writes=[r_x[c][g]])
        smalls = [(gains, gains_d), (bgate, bgate_d), (mnorm, mnorm_d), (lruc, lruc_d), (fconv, fconv_d), (cst, cst_d)]
        r_sm = []
        for i, (t, d) in enumerate(smalls):
            r = P.res("sm%d" % i)
            r_sm.append(r)
            flat = t[:] if len(t.shape) == 2 else t[:].rearrange({3: "p a b -> p (a b)", 4: "p a b c -> p (a b c)", 5: "p a b c d -> p (a b c d)"}[len(t.shape)])
            P.dma("sp", (lambda e, flat=flat, d=d: e.dma_start(out=flat, in_=d[:, :])), writes=[r])
        r_gains, r_bgate, r_mnorm, r_lruc, r_fconv, r_cst = r_sm
        P.op("dve", CP(identB[:], identF), reads=[r_cst], writes=[r_cb])
        P.op("dve", MS(onesB[:], 1.0), writes=[r_cb])
        P.dma("pool", (lambda e: e.dma_start(out=lruw[:].rearrange("p a b c d -> p (a b c d)"), in_=lruw_d[:, :])), writes=[r_lruw])
        lam_t = sbt("lam_t", [128, DEPTH, 3], F32); r_lam = P.res("lam")
        P.op("act", ACT(lam_t[:], lruc[:, :, :, 7], AF.Exp, scale=-1.0), reads=[r_lruc], writes=[r_lam])
        P.op("act", ACT(lam_t[:], lam_t[:], AF.Ln, bias=1.0), reads=[r_lam], writes=[r_lam])
        P.op("dve", TS(lrud[:, :, :, 0], lam_t[:], -8.0), reads=[r_lam], writes=[r_lrud])
        P.op("dve", TS(lrud[:, :, :, 1], lam_t[:], -16.0), reads=[r_lam], writes=[r_lrud])

        def weight_dma(dst_ap, src_ap, res):
            P.dma("pool", (lambda e: e.dma_start(out=dst_ap, in_=src_ap)), writes=[res])

        def barrier(old, new):
            P.op("pool", MS(dummy[:], 0.0), writes=list(old) + list(new) + [r_dummy])

        dummy = sbt("dummy_sb", [128, 8], F32); r_dummy = P.res("dummy")

        def rms_stats(l, g, sq_ap, r_sq, pbank):
            tok = slice(g * TG, (g + 1) * TG)
            for c in range(KC):
                P.op("act", ACT(sq_ap[:, c, :], xT[:, c, tok], AF.Square), reads=[r_x[c][g]], writes=[r_sq])
            P.op("pe", [MM(pb[pbank][:, :], onesB[:], sq_ap[:, c, :], start=(c == 0), stop=(c == KC - 1)) for c in range(KC)],
                 reads=[r_sq, r_cb], writes=[r_pb[pbank]])
            P.op("act", ACT(rstdb[:, tok], pb[pbank][:, :], AF.Ln, scale=1.0 / D, bias=1e-6), reads=[r_pb[pbank]], writes=[r_rstd[g]])
            P.op("act", ACT(rstdb[:, tok], rstdb[:, tok], AF.Exp, scale=-0.5), reads=[r_rstd[g]], writes=[r_rstd[g]])

        def make_h(l, which, g, h_ap, r_h):
            tok = slice(g * TG, (g + 1) * TG)
            for c in range(KC):
                P.op("dve", STT(h_ap[:, c, :], xT[:, c, tok], gains[:, l, which, c:c + 1], rstdb[:, tok], ALU.mult, ALU.mult),
                     reads=[r_x[c][g], r_rstd[g], r_gains], writes=[r_h])

        def post_norm_residual(l, which, tok0, ntok, src_aps, r_src, sq_ap, r_sq, pbank, rs_ap, r_rs, tmp_ap, r_tmp):
            g = tok0 // TG
            tok = slice(tok0, tok0 + ntok)
            for c in range(KC):
                P.op("act", ACT(sq_ap[:, c, :], src_aps[c], AF.Square), reads=[r_src[c]], writes=[r_sq])
            P.op("pe", [MM(pb[pbank][:, 0:ntok], onesB[:], sq_ap[:, c, :], start=(c == 0), stop=(c == KC - 1)) for c in range(KC)],
                 reads=[r_sq, r_cb], writes=[r_pb[pbank]])
            P.op("act", ACT(rs_ap, pb[pbank][:, 0:ntok], AF.Ln, scale=1.0 / D, bias=1e-6), reads=[r_pb[pbank]], writes=[r_rs])
            P.op("act", ACT(rs_ap, rs_ap, AF.Exp, scale=-0.5), reads=[r_rs], writes=[r_rs])
            for c in range(KC):
                P.op("dve", STT(tmp_ap, src_aps[c], gains[:, l, which, c:c + 1], rs_ap, ALU.mult, ALU.mult),
                     reads=[r_src[c], r_rs, r_gains], writes=[r_tmp])
                P.op("dve", TT(xT[:, c, tok], xT[:, c, tok], tmp_ap, ALU.add), reads=[r_tmp, r_x[c][g]], writes=[r_x[c][g]])

        for l in range(depth):
            apos[0] = 0
            concat = carve(KC * S, BF16).rearrange("p (c t) -> p c t", c=KC)
            r_cat = [[P.res("cat%d_%d" % (c, t)) for t in range(16)] for c in range(KC)]
            hT = [carve(KC * TG, BF16).rearrange("p (c t) -> p c t", c=KC) for _ in range(2)]
            r_hT = [P.res("hT0"), P.res("hT1")]
            W = carve(KC * 1544, BF16).rearrange("p (c n) -> p c n", c=KC)
            r_W = P.res("W")
            pl0 = apos[0]
            all_mixer_res = [r for row in r_cat for r in row] + r_hT + [r_W]
            if l > 0:
                barrier(prev_phase_res, all_mixer_res)

            P.skip = bool(os.environ.get("K_ONLY_E"))
            P.mark("A")
            weight_dma(W[:, :, 0:1544], w_in_d[l, :, 0:1544].rearrange("(c p) n -> p c n", p=128), r_W)
            sqb = carve(KC * TG, BF16).rearrange("p (c t) -> p c t", c=KC); r_sqb = P.res("sqb")
            qTg = carve(4 * TG, BF16).rearrange("p (h t) -> p h t", h=4); r_qTg = P.res("qTg")
            kTg = carve(4 * TG, BF16).rearrange("p (h t) -> p h t", h=4); r_kTg = P.res("kTg")
            NB = 2
            kt = [carve(384, BF16).rearrange("p (h d) -> p h d", h=4) for _ in range(NB)]; r_kt = [P.res() for _ in range(NB)]
            vx = [carve(4 * 97, BF16).rearrange("p (h d) -> p h d", h=4) for _ in range(NB)]; r_vx = [P.res() for _ in range(NB)]
            og = [carve(384, BF16) for _ in range(NB)]; r_og = [P.res() for _ in range(NB)]
            gi = [carve(8, F32) for _ in range(NB)]; r_gi = [P.res() for _ in range(NB)]
            nlf = [carve(4, F32) for _ in range(NB)]; r_nlf = [P.res() for _ in range(NB)]
            es = [carve(4, F32) for _ in range(NB)]; r_es = [P.res() for _ in range(NB)]
            emb = [carve(4, F32) for _ in range(NB)]; r_emb = [P.res() for _ in range(NB)]
            ebl = [carve(8, F32).rearrange("p (j h) -> p j h", j=2) for _ in range(NB)]; r_ebl = [P.res() for _ in range(NB)]
            tmp4 = [carve(4, F32) for _ in range(NB)]; r_tmp4 = [P.res() for _ in range(NB)]
            Stt = [carve(4 * 128, BF16).rearrange("p (h t) -> p h t", h=4) for _ in range(NB)]; r_St = [P.res() for _ in range(NB)]
            Cf = carve(4 * 97, F32).rearrange("p (h d) -> p h d", h=4); r_Cf = P.res("Cf")
            Ctmp = carve(4 * 97, F32).rearrange("p (h d) -> p h d", h=4); r_Ctmp = P.res("Ctmp")
            NCB = 4
            Cb = [carve(4 * 97, BF16).rearrange("p (h d) -> p h d", h=4) for _ in range(NCB)]; r_Cb = [P.res() for _ in range(NCB)]
            den = [carve(4, F32) for _ in range(NB)]; r_den = [P.res() for _ in range(NB)]
            hraw = [carve(384, F32).rearrange("p (h d) -> p h d", h=4) for _ in range(NB)]; r_hraw = [P.res() for _ in range(NB)]
            hsq = [carve(384, F32).rearrange("p (h d) -> p h d", h=4) for _ in range(NB)]; r_hsq = [P.res() for _ in range(NB)]
            ssq = [carve(4, F32) for _ in range(NB)]; r_ssq = [P.res() for _ in range(NB)]
            gm = [carve(384, F32) for _ in range(NB)]; r_gm = [P.res() for _ in range(NB)]
            hAb = [carve(384, BF16) for _ in range(NB)]; r_hAb = [P.res() for _ in range(NB)]
            phaseA_res = ([r_sqb, r_qTg, r_kTg, r_Cf, r_Ctmp] + r_kt + r_vx + r_og + r_gi + r_nlf + r_es + r_emb + r_ebl + r_tmp4 + r_St
                          + r_Cb + r_den + r_hraw + r_hsq + r_ssq + r_gm + r_hAb)
            if l > 0:
                barrier(prev_phase_res, phaseA_res)

            P.op("dve", MS(Cf[0:96], 0.0), writes=[r_Cf])
            P.op("dve", MS(Cb[0][0:96], 0.0), writes=[r_Cb[0]])
            for b in range(NB):
                P.op("dve", MS(vx[b][:, :, 96:97], 1.0), writes=[r_vx[b]])
            cbi = 0
            LN_SC = float(np.log(96.0 ** -0.5))
            lnsc = sbt("lnsc%d" % l, [128, 1], F32); r_lnsc = P.res("lnsc")
            P.op("dve", MS(lnsc[:], LN_SC), writes=[r_lnsc])

            for g in range(NG):
                hb = g % 2
                rms_stats(l, g, sqb, r_sqb, 7)
                make_h(l, 0, g, hT[hb], r_hT[hb])
                for qk in range(2):
                    for h in range(4):
                        bank = (qk * 4 + h) % 2
                        col0 = qk * 384 + h * 96
                        P.op("pe", [MM(pb[bank][0:96, :], W[:, c, col0:col0 + 96], hT[hb][:, c, :], start=(c == 0), stop=(c == KC - 1)) for c in range(KC)],
                             reads=[r_W, r_hT[hb]], writes=[r_pb[bank]])
                        dst = (qTg if qk == 0 else kTg)
                        P.op("act", ACT(dst[0:96, h, :], pb[bank][0:96, :], AF.Copy), reads=[r_pb[bank]], writes=[r_qTg if qk == 0 else r_kTg])
                for ti in range(4):
                    T = g * 4 + ti
                    b = T % NB
                    tl = slice(ti * 128, (ti + 1) * 128)
                    for (bank, c0, n) in ((2, 1152, 392), (3, 384, 384), (4, 768, 384)):
                        P.op("pe", [MM(pb[bank][:, 0:n], hT[hb][:, c, tl], W[:, c, c0:c0 + n], start=(c == 0), stop=(c == KC - 1)) for c in range(KC)],
                             reads=[r_W, r_hT[hb]], writes=[r_pb[bank]])
                    P.op("dve", TT(gi[b], pb[2][:, 384:392], bgate[:, l, :], ALU.add), reads=[r_pb[2], r_bgate], writes=[r_gi[b]])
                    P.op("act", ACT(nlf[b], gi[b][:, 4:8], AF.Exp, scale=-1.0), reads=[r_gi[b]], writes=[r_nlf[b]])
                    P.op("act", ACT(nlf[b], nlf[b], AF.Ln, bias=1.0), reads=[r_nlf[b]], writes=[r_nlf[b]])
                    P.op("act", ACT(og[b], pb[2][:, 0:384], AF.Sigmoid), reads=[r_pb[2], r_gi[b]], writes=[r_og[b]])
                    P.op("pe", [MM(pb[5][:, 0:4], U2f, nlf[b], True, True),
                                MM(pb[5][0:96, 8:12], Ef[:, 0:96], nlf[b], True, True),
                                MM(pb[5][0:96, 16:20], Ef[:, 96:192], nlf[b], True, True)],
                         reads=[r_nlf[b], r_cst], writes=[r_pb[5]])
                    P.op("dve", TT(tmp4[b], gi[b][:, 0:4], pb[5][:, 0:4], ALU.add), reads=[r_gi[b], r_pb[5]], writes=[r_tmp4[b]])
                    P.op("act", ACT(es[b], tmp4[b], AF.Exp, bias=lnsc[:]), reads=[r_tmp4[b], r_lnsc], writes=[r_es[b]])
                    P.op("act", ACT(emb[b], pb[5][:, 0:4], AF.Exp), reads=[r_pb[5], r_tmp4[b]], writes=[r_emb[b]])
                    P.op("act", ACT(ebl[b][0:96, 0, :], pb[5][0:96, 8:12], AF.Exp, scale=-1.0), reads=[r_pb[5]], writes=[r_ebl[b]])
                    P.op("act", ACT(ebl[b][0:96, 1, :], pb[5][0:96, 16:20], AF.Exp, scale=-1.0), reads=[r_pb[5]], writes=[r_ebl[b]])
                    P.op("dve", TT(kt[b][:], pb[3][:, 0:384].rearrange("p (h d) -> p h d", h=4), es[b].unsqueeze(2).to_broadcast([128, 4, 96]), ALU.mult),
                         reads=[r_pb[3], r_es[b]], writes=[r_kt[b]])
                    P.op("act", ACT(vx[b][:, :, 0:96], pb[4][:, 0:384].rearrange("p (h d) -> p h d", h=4), AF.Copy), reads=[r_pb[4]], writes=[r_vx[b]])
                    P.op("pe", [MM(pb[6][:, h * 128:(h + 1) * 128], kTg[0:96, h, tl], qTg[0:96, h, tl], True, True) for h in range(4)],
                         reads=[r_kTg, r_qTg], writes=[r_pb[6]])
                    for h in range(4):
                        P.op("dve", STT(Stt[b][:, h, :], pb[6][:, h * 128:(h + 1) * 128], es[b][:, h:h + 1], U2f, ALU.mult, ALU.mult),
                             reads=[r_pb[6], r_es[b], r_cst], writes=[r_St[b]])
                    ca = cbi
                    for j in range(2):
                        ps = slice(64 * j, 64 * j + 64)
                        P.op("pe", [MM(pb[7][0:96, h * 97:(h + 1) * 97], kt[b][ps, h, :], vx[b][ps, h, :], True, True) for h in range(4)],
                             reads=[r_kt[b], r_vx[b]], writes=[r_pb[7]])
                        P.op("dve", TT(Ctmp[0:96], pb[7][0:96, 0:388].rearrange("p (h d) -> p h d", h=4), Cf[0:96], ALU.add),
                             reads=[r_pb[7], r_Cf], writes=[r_Ctmp])
                        P.op("dve", TT(Cf[0:96], Ctmp[0:96], ebl[b][0:96, j, :].unsqueeze(2).to_broadcast([96, 4, 97]), ALU.mult),
                             reads=[r_Ctmp, r_ebl[b]], writes=[r_Cf])
                        nxt = (cbi + 1) % NCB
                        P.op("act", ACT(Cb[nxt][0:96], Cf[0:96], AF.Copy), reads=[r_Cf], writes=[r_Cb[nxt]])
                        cbi = nxt
                    c_a = ca
                    c_b = (ca + 1) % NCB
                    accb = 0 if (T % 2 == 0) else 1
                    fns = []
                    for h in range(4):
                        o = pb[accb][:, h * 97:(h + 1) * 97]
                        fns.append(MM(o, Stt[b][:, h, :], vx[b][:, h, :], True, False, skip_group_check=True))
                        fns.append(MM(pb[accb][0:64, h * 97:(h + 1) * 97], qTg[0:96, h, ti * 128:ti * 128 + 64], Cb[c_a][0:96, h, :], False, False, skip_group_check=True))
                        fns.append(MM(pb[accb][64:128, h * 97:(h + 1) * 97], qTg[0:96, h, ti * 128 + 64:ti * 128 + 128], Cb[c_b][0:96, h, :], False, True,
                                      skip_group_check=True, tile_position=(0, 64)))
                    P.op("pe", fns, reads=[r_St[b], r_vx[b], r_qTg, r_Cb[c_a], r_Cb[c_b]], writes=[r_pb[accb]])
                    acc = pb[accb][:, 0:388].rearrange("p (h d) -> p h d", h=4)
                    P.op("act", ACT(den[b], acc[:, :, 96], AF.Abs), reads=[r_pb[accb]], writes=[r_den[b]])
                    P.op("dve", TT(den[b], den[b], emb[b], ALU.max), reads=[r_den[b], r_emb[b]], writes=[r_den[b]])
                    P.op("dve", (lambda e, b=b: e.reciprocal(out=den[b], in_=den[b])), reads=[r_den[b]], writes=[r_den[b]])
                    P.op("dve", TT(hraw[b][:], acc[:, :, 0:96], den[b].unsqueeze(2).to_broadcast([128, 4, 96]), ALU.mult),
                         reads=[r_pb[accb], r_den[b]], writes=[r_hraw[b]])
                    P.op("dve", TT(hsq[b][:], hraw[b][:], hraw[b][:], ALU.mult), reads=[r_hraw[b]], writes=[r_hsq[b]])
                    P.op("dve", (lambda e, b=b: e.tensor_reduce(out=ssq[b], in_=hsq[b][:], axis=AX.X, op=ALU.add)), reads=[r_hsq[b]], writes=[r_ssq[b]])
                    P.op("act", ACT(ssq[b], ssq[b], AF.Ln, scale=1.0 / 96, bias=1e-6), reads=[r_ssq[b]], writes=[r_ssq[b]])
                    P.op("act", ACT(ssq[b], ssq[b], AF.Exp, scale=-0.5), reads=[r_ssq[b]], writes=[r_ssq[b]])
                    P.op("dve", TT(gm[b], og[b], mnorm[:, l, :], ALU.mult), reads=[r_og[b], r_mnorm], writes=[r_gm[b]])
                    P.op("dve", TT(hraw[b][:], hraw[b][:], ssq[b].unsqueeze(2).to_broadcast([128, 4, 96]), ALU.mult),
                         reads=[r_hraw[b], r_ssq[b]], writes=[r_hraw[b]])
                    P.op("dve", TT(hAb[b], hraw[b][:].rearrange("p h d -> p (h d)"), gm[b], ALU.mult), reads=[r_hraw[b], r_gm[b]], writes=[r_hAb[b]])
                    pT = pb[5][:].bitcast(BF16)
                    P.op("pe", [TR(pT[:, 128 * c:128 * (c + 1)], hAb[b][:, 128 * c:128 * (c + 1)], identB[:]) for c in range(3)],
                         reads=[r_hAb[b], r_cb], writes=[r_pb[5]])
                    P.op("act", ACT(concat[:, 0:3, T * 128:(T + 1) * 128], pT[:, 0:384].rearrange("p (c t) -> p c t", c=3), AF.Copy),
                         reads=[r_pb[5]], writes=[r_cat[0][T], r_cat[1][T], r_cat[2][T]])

            P.mark("B")
            apos[0] = pl0
            sqb = carve(KC * TG, BF16).rearrange("p (c t) -> p c t", c=KC); r_sqb = P.res("sqb")
            lxs = [carve(TG + 4, F32) for _ in range(3)]; r_lxs = [P.res() for _ in range(3)]
            NB = 2
            xc = [carve(TG, F32) for _ in range(NB)]; r_xc = [P.res() for _ in range(NB)]
            xcb = [carve(TG, BF16) for _ in range(NB)]; r_xcb = [P.res() for _ in range(NB)]
            rr = [carve(TG, F32) for _ in range(NB)]; r_rr = [P.res() for _ in range(NB)]
            ii = [carve(TG, F32) for _ in range(NB)]; r_ii = [P.res() for _ in range(NB)]
            aa = [carve(TG, F32) for _ in range(NB)]; r_aa = [P.res() for _ in range(NB)]
            a2 = [carve(TG, F32) for _ in range(NB)]; r_a2 = [P.res() for _ in range(NB)]
            uu = [carve(TG, F32) for _ in range(NB)]; r_uu = [P.res() for _ in range(NB)]
            hs = [carve(TG, F32) for _ in range(NB)]; r_hs = [P.res() for _ in range(NB)]
            gl = [carve(TG, F32) for _ in range(NB)]; r_gl = [P.res() for _ in range(NB)]
            hprev = carve(4, F32); r_hprev = [P.res() for _ in range(3)]
            phaseB_res = [r_sqb] + r_lxs + r_xc + r_xcb + r_rr + r_ii + r_aa + r_a2 + r_uu + r_hs + r_gl + r_hprev
            barrier(phaseA_res + [r_W], phaseB_res + [r_W])
            weight_dma(W[:, :, 0:768], w_in_d[l, :, 1544:2312].rearrange("(c p) n -> p c n", p=128), r_W)
            for c in range(3):
                P.op("dve", MS(lxs[c][:, 0:4], 0.0), writes=[r_lxs[c]])
                P.op("dve", MS(hprev[:, c:c + 1], 0.0), writes=[r_hprev[c]])
            k = 0
            for g in range(NG):
                hb = g % 2
                make_h(l, 0, g, hT[hb], r_hT[hb])
                for c in range(3):
                    b = k % NB
                    k += 1
                    blx, blg, bra, brx = (0, 1, 2, 3) if b == 0 else (4, 5, 6, 7)
                    P.op("pe", [MM(pb[blx][:, :], W[:, kc, c * 128:(c + 1) * 128], hT[hb][:, kc, :], start=(kc == 0), stop=(kc == KC - 1)) for kc in range(KC)],
                         reads=[r_W, r_hT[hb]], writes=[r_pb[blx]])
                    P.op("pe", [MM(pb[blg][:, :], W[:, kc, 384 + c * 128:384 + (c + 1) * 128], hT[hb][:, kc, :], start=(kc == 0), stop=(kc == KC - 1)) for kc in range(KC)],
                         reads=[r_W, r_hT[hb]], writes=[r_pb[blg]])
                    P.op("act", ACT(lxs[c][:, 4:4 + TG], pb[blx][:, :], AF.Copy), reads=[r_pb[blx]], writes=[r_lxs[c]])
                    P.op("dve", TS(xc[b], lxs[c][:, 4:4 + TG], lruc[:, l, c, 3:4], lruc[:, l, c, 4:5], ALU.mult, ALU.add),
                         reads=[r_lxs[c], r_lruc], writes=[r_xc[b]])
                    for j in range(3):
                        P.op("dve", STT(xc[b], lxs[c][:, 1 + j:1 + j + TG], lruc[:, l, c, j:j + 1], xc[b], ALU.mult, ALU.add),
                             reads=[r_lxs[c], r_lruc, r_xc[b]], writes=[r_xc[b]])
                    P.op("dve", CP(lxs[c][:, 1:4], lxs[c][:, TG + 1:TG + 4]), reads=[r_lxs[c]], writes=[r_lxs[c]])
                    P.op("act", ACT(xcb[b], xc[b], AF.Copy), reads=[r_xc[b]], writes=[r_xcb[b]])
                    P.op("pe", MM(pb[bra][:, :], lruw[:, l, 0, c, :], xcb[b], True, True), reads=[r_lruw, r_xcb[b]], writes=[r_pb[bra]])
                    P.op("pe", MM(pb[brx][:, :], lruw[:, l, 1, c, :], xcb[b], True, True), reads=[r_lruw, r_xcb[b]], writes=[r_pb[brx]])
                    P.op("act", ACT(rr[b], pb[bra][:, :], AF.Sigmoid, bias=lruc[:, l, c, 5:6]), reads=[r_pb[bra], r_lruc], writes=[r_rr[b]])
                    P.op("act", ACT(ii[b], pb[brx][:, :], AF.Sigmoid, bias=lruc[:, l, c, 6:7]), reads=[r_pb[brx], r_lruc], writes=[r_ii[b]])
                    P.op("act", ACT(aa[b], rr[b], AF.Exp, scale=lrud[:, l, c, 0:1]), reads=[r_rr[b], r_lrud], writes=[r_aa[b]])
                    P.op("act", ACT(a2[b], rr[b], AF.Exp, scale=lrud[:, l, c, 1:2]), reads=[r_rr[b], r_lrud], writes=[r_a2[b]])
                    P.op("act", ACT(a2[b], a2[b], AF.Sqrt, scale=-1.0, bias=1.0), reads=[r_a2[b]], writes=[r_a2[b]])
                    P.op("dve", TT(uu[b], a2[b], ii[b], ALU.mult), reads=[r_a2[b], r_ii[b]], writes=[r_uu[b]])
                    P.op("dve", TT(uu[b], uu[b], xc[b], ALU.mult), reads=[r_uu[b], r_xc[b]], writes=[r_uu[b]])
                    P.mark("scan-next")
                    SN = int(os.environ.get("K_SN", "128"))
                    for q0 in range(0, TG, SN):
                        ini = hprev[:, c:c + 1] if q0 == 0 else hs[b][:, q0 - 1:q0]
                        P.op("dve", (lambda e, b=b, ini=ini, q0=q0: e.tensor_tensor_scan(out=hs[b][:, q0:q0 + SN], data0=aa[b][:, q0:q0 + SN], data1=uu[b][:, q0:q0 + SN],
                                                                                       initial=ini, op0=ALU.mult, op1=ALU.add)),
                             reads=[r_aa[b], r_uu[b], r_hprev[c], r_hs[b]], writes=[r_hs[b]])
                    P.op("dve", CP(hprev[:, c:c + 1], hs[b][:, TG - 1:TG]), reads=[r_hs[b]], writes=[r_hprev[c]])
                    P.op("act", ACT(gl[b], pb[blg][:, :], AF.Gelu_apprx_tanh), reads=[r_pb[blg]], writes=[r_gl[b]])
                    P.op("dve", TT(concat[:, 3 + c, g * TG:(g + 1) * TG], hs[b], gl[b], ALU.mult), reads=[r_hs[b], r_gl[b]],
                         writes=[r_cat[3 + c][4 * g + i] for i in range(4)])

            P.mark("C")
            apos[0] = pl0
            akT = carve(2 * S, BF16).rearrange("p (c t) -> p c t", c=2); r_akT = [P.res() for _ in range(NG)]
            ikT = carve(S, BF16); r_ikT = [P.res() for _ in range(NG)]
            avx = carve(16 * 4 * 65, BF16).rearrange("p (t h d) -> p t h d", t=16, h=4); r_avx = [P.res() for _ in range(16)]
            iwt = carve(16 * 4, F32).rearrange("p (t h) -> p t h", t=16); r_iwt = [P.res() for _ in range(16)]
            aqT = carve(2 * TG, BF16).rearrange("p (c t) -> p c t", c=2); r_aqT = P.res("aqT")
            iqT = carve(2 * TG, BF16).rearrange("p (c t) -> p c t", c=2); r_iqT = P.res("iqT")
            sc = carve(S, F32); r_sc = P.res("sc")
            msk = carve(S, BF16); r_msk = P.res("msk")
            junk = msk; r_junk = r_msk
            mT = carve(S, BF16).rearrange("p (j q) -> p j q", j=16); r_mT = P.res("mT")
            rlb = [carve(TG, F32) for _ in range(2)]; r_rlb = [P.res() for _ in range(2)]
            exb = [carve(TG, BF16) for _ in range(2)]; r_exb = [P.res() for _ in range(2)]
            pTb = [carve(TG, BF16) for _ in range(2)]; r_pTb = [P.res() for _ in range(2)]
            bis = carve(8 + 2 * NBIS, F32); r_bis = P.res("bis")
            rc4 = carve(4, F32); r_rc4 = P.res("rc4")
            hcb = carve(256, BF16); r_hcb = P.res("hcb")
            phaseC_res = (r_akT + r_ikT + r_avx + r_iwt + [r_aqT, r_iqT, r_sc, r_msk, r_mT, r_bis, r_rc4, r_hcb] + r_rlb + r_exb + r_pTb)
            barrier(phaseB_res + [r_W], phaseC_res + [r_W])
            wsrc = w_in_d[l]
            r3 = lambda a, b_: wsrc[:, a:b_].rearrange("(c p) n -> p c n", p=128)
            weight_dma(W[:, :, 0:512], r3(2312, 2824), r_W)
            weight_dma(W[:, :, 512:768], r3(3080, 3336), r_W)
            weight_dma(W[:, :, 768:832], r3(3336, 3400), r_W)
            weight_dma(W[:, :, 832:896], r3(3336, 3400), r_W)
            weight_dma(W[:, :, 896:1152], r3(2824, 3080), r_W)
            weight_dma(W[:, :, 1152:1156], r3(3400, 3404), r_W)
            for T in range(16):
                P.op("dve", MS(avx[:, T, :, 64:65], 1.0), writes=[r_avx[T]])
            for g in range(NG):
                hb = g % 2
                make_h(l, 0, g, hT[hb], r_hT[hb])
                tokg = slice(g * TG, (g + 1) * TG)
                plan = [(0, aqT[:, 0, :], [r_aqT]), (128, aqT[:, 1, :], [r_aqT]), (256, akT[:, 0, tokg], [r_akT[g]]), (384, akT[:, 1, tokg], [r_akT[g]]),
                        (512, iqT[:, 0, :], [r_iqT]), (640, iqT[:, 1, :], [r_iqT]), (768, ikT[:, tokg], [r_ikT[g]])]
                for i, (c0, dst, rw) in enumerate(plan):
                    bank = i % 2
                    P.op("pe", [MM(pb[bank][:, :], W[:, kc, c0:c0 + 128], hT[hb][:, kc, :], start=(kc == 0), stop=(kc == KC - 1)) for kc in range(KC)],
                         reads=[r_W, r_hT[hb]], writes=[r_pb[bank]])
                    P.op("act", ACT(dst, pb[bank][:, :], AF.Copy), reads=[r_pb[bank]], writes=rw)
                for ti in range(4):
                    T = 4 * g + ti
                    bank = 2 + (ti % 2)
                    P.op("pe", [MM(pb[bank][:, 0:260], hT[hb][:, kc, ti * 128:(ti + 1) * 128], W[:, kc, 896:1156], start=(kc == 0), stop=(kc == KC - 1)) for kc in range(KC)],
                         reads=[r_W, r_hT[hb]], writes=[r_pb[bank]])
                    P.op("act", ACT(avx[:, T, :, 0:64], pb[bank][:, 0:256].rearrange("p (h d) -> p h d", h=4), AF.Copy), reads=[r_pb[bank]], writes=[r_avx[T]])
                    P.op("act", ACT(iwt[:, T, :], pb[bank][:, 256:260], AF.Copy), reads=[r_pb[bank]], writes=[r_iwt[T]])
                for ti in range(4):
                    T = 4 * g + ti
                    nk = 128 * (T + 1)
                    ql = slice(ti * 128, (ti + 1) * 128)
                    nkb = (nk + 511) // 512
                    key_res_k = [r_ikT[gg] for gg in range(g + 1)]
                    cnt = 0
                    for h in range(4):
                        hp = slice(64 * (h % 2), 64 * (h % 2) + 64)
                        for kb in range(nkb):
                            k0 = kb * 512
                            w = min(512, nk - k0)
                            bank = 4 + (cnt % 2)
                            rb = cnt % 2
                            cnt += 1
                            P.op("pe", MM(pb[bank][:, 0:w], iqT[hp, h // 2, ql], ikT[hp, k0:k0 + w], True, True),
                                 reads=[r_iqT] + key_res_k, writes=[r_pb[bank]])
                            P.op("act", ACT(rlb[rb][:, 0:w], pb[bank][:, 0:w], AF.Relu), reads=[r_pb[bank]], writes=[r_rlb[rb]])
                            if h == 0:
                                P.op("dve", TS(sc[:, k0:k0 + w], rlb[rb][:, 0:w], iwt[:, T, 0:1]), reads=[r_rlb[rb], r_iwt[T]], writes=[r_sc])
                            else:
                                P.op("dve", STT(sc[:, k0:k0 + w], rlb[rb][:, 0:w], iwt[:, T, h:h + 1], sc[:, k0:k0 + w], ALU.mult, ALU.add),
                                     reads=[r_rlb[rb], r_iwt[T], r_sc], writes=[r_sc])
                    if T >= 2:
                        P.op("dve", (lambda e, nk=nk: e.tensor_reduce(out=bis[:, 0:1], in_=sc[:, 0:nk], axis=AX.X, op=ALU.max)), reads=[r_sc], writes=[r_bis])
                        P.op("dve", (lambda e, nk=nk: e.tensor_reduce(out=bis[:, 1:2], in_=sc[:, 0:nk], axis=AX.X, op=ALU.min)), reads=[r_sc], writes=[r_bis])
                    P.op("dve", MS(sc[0:64, nk - 64:nk], -1e30), reads=[r_sc], writes=[r_sc])
                    if T >= 2:
                        P.op("dve", TT(bis[:, 2:3], bis[:, 0:1], bis[:, 1:2], ALU.subtract), reads=[r_bis], writes=[r_bis])
                        P.op("dve", TS(bis[:, 8:8 + NBIS], pow2, bis[:, 2:3]), reads=[r_bis, r_cst], writes=[r_bis])
                        P.op("dve", TS(bis[:, 8 + NBIS:8 + 2 * NBIS], bis[:, 8:8 + NBIS], -0.5), reads=[r_bis], writes=[r_bis])
                        P.op("dve", TT(bis[:, 3:4], bis[:, 1:2], bis[:, 8:9], ALU.add), reads=[r_bis], writes=[r_bis])
                        for kk in range(NBIS):
                            P.op("dve", (lambda e, nk=nk: e.tensor_scalar(out=junk[:, 0:nk], in0=sc[:, 0:nk], scalar1=bis[:, 3:4], scalar2=0.0,
                                                                            op0=ALU.is_ge, op1=ALU.add, accum_out=bis[:, 4:5])),
                                 reads=[r_sc, r_bis], writes=[r_msk, r_bis])
                            P.op("dve", STT(bis[:, 5:6], bis[:, 4:5], 255.5, bis[:, 8 + kk:9 + kk], ALU.is_ge, ALU.mult), reads=[r_bis], writes=[r_bis])
                            P.op("dve", STT(bis[:, 3:4], bis[:, 5:6], bis[:, 8 + NBIS + kk:9 + NBIS + kk], bis[:, 3:4], ALU.add, ALU.add), reads=[r_bis], writes=[r_bis])
                        P.op("dve", TT(bis[:, 3:4], bis[:, 3:4], bis[:, 8 + 2 * NBIS - 1:8 + 2 * NBIS], ALU.add), reads=[r_bis], writes=[r_bis])
                        P.op("dve", TS(msk[:, 0:nk], sc[:, 0:nk], bis[:, 3:4], None, ALU.is_ge), reads=[r_sc, r_bis], writes=[r_msk])
                    else:
                        P.op("dve", TS(msk[:, 0:nk], sc[:, 0:nk], -1e29, None, ALU.is_ge), reads=[r_sc], writes=[r_msk])
                    nb_ = T + 1
                    for j0 in range(0, nb_, 8):
                        bank = 6 + ((j0 // 8) % 2)
                        pT = pb[bank][:].bitcast(BF16)
                        n = min(8, nb_ - j0)
                        P.op("pe", [TR(pT[:, 128 * i:128 * (i + 1)], msk[:, (j0 + i) * 128:(j0 + i + 1) * 128], identB[:]) for i in range(n)],
                             reads=[r_msk, r_cb], writes=[r_pb[bank]])
                        P.op("act", ACT(mT[:, j0:j0 + n, :], pT[:, 0:128 * n].rearrange("p (j q) -> p j q", j=n), AF.Copy), reads=[r_pb[bank]], writes=[r_mT])
                    key_res_a = [r_akT[gg] for gg in range(g + 1)]
                    cnt = 0
                    for h in range(4):
                        hp = slice(64 * (h % 2), 64 * (h % 2) + 64)
                        groups = [(j0, min(4, nb_ - j0)) for j0 in range(0, nb_, 4)]
                        for gi_, (j0, n) in enumerate(groups):
                            bank = 4 + (cnt % 2)
                            eb = cnt % 2
                            cnt += 1
                            P.op("pe", [MM(pb[bank][:, 128 * i:128 * (i + 1)], akT[hp, h // 2, (j0 + i) * 128:(j0 + i + 1) * 128], aqT[hp, h // 2, ql], True, True) for i in range(n)],
                                 reads=[r_aqT] + key_res_a, writes=[r_pb[bank]])
                            P.op("act", ACT(exb[eb][:, 0:128 * n], pb[bank][:, 0:128 * n], AF.Exp, scale=0.125), reads=[r_pb[bank]], writes=[r_exb[eb]])
                            P.op("dve", TT(pTb[eb][:, 0:128 * n], exb[eb][:, 0:128 * n], mT[:, j0:j0 + n, :].rearrange("p j q -> p (j q)"), ALU.mult),
                                 reads=[r_exb[eb], r_mT], writes=[r_pTb[eb]])
                            P.op("pe", [MM(pb[3][:, h * 65:(h + 1) * 65], pTb[eb][:, 128 * i:128 * (i + 1)], avx[:, j0 + i, h, :],
                                           start=(j0 + i == 0), stop=(j0 + i == nb_ - 1), skip_group_check=True) for i in range(n)],
                                 reads=[r_pTb[eb]] + [r_avx[j0 + i] for i in range(n)], writes=[r_pb[3]])
                    oacc = pb[3][:, 0:260].rearrange("p (h d) -> p h d", h=4)
                    P.op("dve", (lambda e: e.reciprocal(out=rc4, in_=oacc[:, :, 64])), reads=[r_pb[3]], writes=[r_rc4])
                    P.op("dve", TT(hcb.rearrange("p (h d) -> p h d", h=4), oacc[:, :, 0:64], rc4.unsqueeze(2).to_broadcast([128, 4, 64]), ALU.mult),
                         reads=[r_pb[3], r_rc4], writes=[r_hcb])
                    pT = pb[2][:].bitcast(BF16)
                    P.op("pe", [TR(pT[:, 128 * c:128 * (c + 1)], hcb[:, 128 * c:128 * (c + 1)], identB[:]) for c in range(2)],
                         reads=[r_hcb, r_cb], writes=[r_pb[2]])
                    P.op("act", ACT(concat[:, 6:8, T * 128:(T + 1) * 128], pT[:, 0:256].rearrange("p (c t) -> p c t", c=2), AF.Copy),
                         reads=[r_pb[2]], writes=[r_cat[6][T], r_cat[7][T]])

            P.mark("Cend")
            if dbg:
                P.force = True
            apos[0] = pl0
            if dbg and l == dbg - 1:
                dtmp = carve(S, F32); r_dtmp = P.res("dtmp")
                barrier(phaseC_res, [r_dtmp])
                for c in range(KC):
                    P.op("dve", CP(dtmp[:], concat[:, c, :]), reads=[r_cat[c][t] for t in range(16)], writes=[r_dtmp])
                    P.dma("sp", (lambda e, c=c: e.dma_start(out=dbg_d[c * 128:(c + 1) * 128, :], in_=dtmp[:])), reads=[r_dtmp])

            if stop_after == "C" and l == depth - 1:
                break
            NTD = 256
            sqd = carve(KC * NTD, BF16).rearrange("p (c t) -> p c t", c=KC); r_sqd = P.res("sqd")
            rsd = carve(NTD, F32); r_rsd = P.res("rsd")
            tmpd = carve(NTD, F32); r_tmpd = P.res("tmpd")
            phaseD_res = [r_sqd, r_rsd, r_tmpd]
            barrier(phaseC_res + [r_W], phaseD_res + [r_W])
            weight_dma(W[:, :, 0:1024], w_out_d[l].rearrange("(c p) n -> p c n", p=128), r_W)
            for gd in range(S // NTD):
                t0 = gd * NTD
                tiles = [t0 // 128, t0 // 128 + 1]
                for c in range(KC):
                    bank = c // 2
                    half = (c % 2) * NTD
                    P.op("pe", [MM(pb[bank][:, half:half + NTD], W[:, kc, c * 128:(c + 1) * 128], concat[:, kc, t0:t0 + NTD], start=(kc == 0), stop=(kc == KC - 1)) for kc in range(KC)],
                         reads=[r_W] + [r_cat[kc][t] for kc in range(KC) for t in tiles], writes=[r_pb[bank]])
                srcs = [pb[c // 2][:, (c % 2) * NTD:(c % 2) * NTD + NTD] for c in range(KC)]
                post_norm_residual(l, 1, t0, NTD, srcs, [r_pb[c // 2] for c in range(KC)], sqd, r_sqd, 4, rsd, r_rsd, tmpd, r_tmpd)

            if stop_after == "D" and l == depth - 1:
                break

            P.skip = False
            P.mark("E")
            apos[0] = 0
            hTf = [carve(KC * TG, BF16).rearrange("p (c t) -> p c t", c=KC) for _ in range(2)]; r_hTf = [P.res() for _ in range(2)]
            sqf = carve(KC * TG, BF16).rearrange("p (c t) -> p c t", c=KC); r_sqf = P.res("sqf")
            actg = carve(NFC * TG, BF16).rearrange("p (k t) -> p k t", k=NFC); r_actg = [P.res() for _ in range(NFC)]
            ost = carve(KC * TG, F32).rearrange("p (c t) -> p c t", c=KC); r_ost = [P.res() for _ in range(KC)]
            NWU = 2
            wu = [carve(KC * 2 * 256, BF16).rearrange("p (c s n) -> p c s n", c=KC, s=2) for _ in range(NWU)]; r_wu = [P.res() for _ in range(NWU)]
            NWD = 2
            wd = [carve(NFC * 256, BF16).rearrange("p (k n) -> p k n", k=NFC) for _ in range(NWD)]; r_wd = [P.res() for _ in range(NWD)]
            NU = 2
            ub = [[carve(TG + 2, F32) for _ in range(2)] for _ in range(NU)]; r_ub = [[P.res() for _ in range(2)] for _ in range(NU)]
            cv = [[carve(TG, F32) for _ in range(2)] for _ in range(NU)]; r_cv = [[P.res() for _ in range(2)] for _ in range(NU)]
            uh = carve(44 * 2, F32).rearrange("p (f t) -> p f t", f=44); r_uh = [P.res() for _ in range(44)]
            rsf = carve(TG, F32); r_rsf = P.res("rsf")
            tmpf = carve(TG, F32); r_tmpf = P.res("tmpf")
            phaseE_res = (r_hTf + [r_sqf, r_rsf, r_tmpf] + r_actg + r_ost + r_wu + r_wd + [x for y in r_ub for x in y] + [x for y in r_cv for x in y] + r_uh)
            barrier(all_mixer_res + phaseD_res, phaseE_res)
            P.op("dve", MS(uh[:].rearrange("p f t -> p (f t)"), 0.0), writes=r_uh)
            wu_i = 0
            wd_i = 0
            up_d = ffn_up_d[l]
            dn_d = ffn_down_d[l]
            pair_k = 0
            for g in range(NG):
                hb = g % 2
                rms_stats(l, g, sqf, r_sqf, 7)
                make_h(l, 2, g, hTf[hb], r_hTf[hb])
                for j2 in range(NFC // 2):
                    ws = wu_i % NWU
                    wu_i += 1
                    weight_dma(wu[ws][:, :, 0, :], up_d[:, j2 * 256:(j2 + 1) * 256].rearrange("(c p) n -> p c n", p=128), r_wu[ws])
                    weight_dma(wu[ws][:, :, 1, :], up_d[:, DFF + j2 * 256:DFF + (j2 + 1) * 256].rearrange("(c p) n -> p c n", p=128), r_wu[ws])
                    for jj in range(2):
                        j = 2 * j2 + jj
                        u = pair_k % NU
                        pair_k += 1
                        bg, bu = (0, 1) if u == 0 else (2, 3)
                        for s_, bank in ((0, bg), (1, bu)):
                            P.op("pe", [MM(pb[bank][:, :], wu[ws][:, kc, s_, jj * 128:(jj + 1) * 128], hTf[hb][:, kc, :], start=(kc == 0), stop=(kc == KC - 1)) for kc in range(KC)],
                                 reads=[r_wu[ws], r_hTf[hb]], writes=[r_pb[bank]])
                        for s_, bank in ((0, bg), (1, bu)):
                            f = j + s_ * NFC
                            ubx = ub[u][s_]; rub = r_ub[u][s_]
                            cvx = cv[u][s_]; rcv = r_cv[u][s_]
                            P.op("act", ACT(ubx[:, 2:2 + TG], pb[bank][:, :], AF.Copy), reads=[r_pb[bank]], writes=[rub])
                            P.op("act", ACT(ubx[:, 0:2], uh[:, f, :], AF.Copy), reads=[r_uh[f]], writes=[rub])
                            P.op("act", ACT(cvx, pb[bank][:, :], AF.Identity, scale=fconv[:, l, f, 2:3], bias=fconv[:, l, f, 3:4]),
                                 reads=[r_pb[bank], r_fconv], writes=[rcv])
                            P.op("dve", STT(cvx, ubx[:, 1:1 + TG], fconv[:, l, f, 1:2], cvx, ALU.mult, ALU.add), reads=[rub, r_fconv, rcv], writes=[rcv])
                            P.op("dve", STT(cvx, ubx[:, 0:TG], fconv[:, l, f, 0:1], cvx, ALU.mult, ALU.add), reads=[rub, r_fconv, rcv], writes=[rcv])
                            P.op("act", ACT(uh[:, f, :], ubx[:, TG:TG + 2], AF.Copy), reads=[rub], writes=[r_uh[f]])
                        P.op("act", ACT(cv[u][0], cv[u][0], AF.Gelu_apprx_tanh), reads=[r_cv[u][0]], writes=[r_cv[u][0]])
                        P.op("dve", TT(actg[:, j, :], cv[u][0], cv[u][1], ALU.mult), reads=[r_cv[u][0], r_cv[u][1]], writes=[r_actg[j]])
                for c2 in range(4):
                    ws = wd_i % NWD
                    wd_i += 1
                    for k0_ in (0, 11):
                        weight_dma(wd[ws][:, k0_:k0_ + 11, :], dn_d[k0_ * 128:(k0_ + 11) * 128, c2 * 256:(c2 + 1) * 256].rearrange("(k p) n -> p k n", p=128), r_wd[ws])
                    for cc in range(2):
                        c = 2 * c2 + cc
                        bank = 4 + (c % 2)
                        P.op("pe", [MM(pb[bank][:, :], wd[ws][:, k_, cc * 128:(cc + 1) * 128], actg[:, k_, :], start=(k_ == 0), stop=(k_ == NFC - 1)) for k_ in range(NFC)],
                             reads=[r_wd[ws]] + r_actg, writes=[r_pb[bank]])
                        P.op("act", ACT(ost[:, c, :], pb[bank][:, :], AF.Copy), reads=[r_pb[bank]], writes=[r_ost[c]])
                post_norm_residual(l, 3, g * TG, TG, [ost[:, c, :] for c in range(KC)], r_ost, sqf, r_sqf, 6, rsf, r_rsf, tmpf, r_tmpf)
            prev_phase_res = phaseE_res

        P.force = True
        for c in range(KC):
            for g in range(NG):
                P.dma("sp", (lambda e, c=c, g=g: e.dma_start(out=yT_d[c * 128:(c + 1) * 128, g * TG:(g + 1) * TG], in_=xT[:, c, g * TG:(g + 1) * TG])),
                      reads=[r_x[c][g]])
        P.wait_all_dma("sp", [r_x[c][g] for c in range(KC) for g in range(NG)] + ([r_dtmp] if dbg else []))
        P.emit()
    return nc


def host_params(inputs):
    f = np.float32
    L = DEPTH
    gains = np.zeros((128, L, 4, 8), f)
    for l in range(L):
        for i, nm in enumerate(("norm_mix_pre", "norm_mix_post", "norm_ffn_pre", "norm_ffn_post")):
            gains[:, l, i, :] = np.asarray(inputs[nm][l], f).reshape(8, 128).T
    bgate = np.zeros((128, L, 8), f)
    bgate[:, :, 0:4] = np.asarray(inputs["b_igate"], f)[None]
    bgate[:, :, 4:8] = np.asarray(inputs["b_fgate"], f)[None]
    mnorm = np.broadcast_to(np.asarray(inputs["mlstm_norm"], f)[None], (128, L, 384)).copy()
    lruc = np.zeros((128, L, 3, 8), f)
    for l in range(L):
        for j in range(4):
            lruc[:, l, :, j] = np.asarray(inputs["lru_conv_w"][l, j], f).reshape(3, 128).T
        lruc[:, l, :, 4] = np.asarray(inputs["lru_conv_b"][l], f).reshape(3, 128).T
        lruc[:, l, :, 5] = np.asarray(inputs["lru_b_a"][l], f).reshape(3, 128).T
        lruc[:, l, :, 6] = np.asarray(inputs["lru_b_x"][l], f).reshape(3, 128).T
        lruc[:, l, :, 7] = np.asarray(inputs["lru_lambda"][l], f).reshape(3, 128).T
    lruw = np.zeros((128, L, 2, 3, 128), f)
    for l in range(L):
        for i, nm in enumerate(("lru_w_a", "lru_w_x")):
            w = np.asarray(inputs[nm][l], f)
            for c in range(3):
                for a in range(2):
                    lruw[64 * a:64 * a + 64, l, i, c, 64 * a:64 * a + 64] = w[2 * c + a]
    fconv = np.zeros((128, L, 44, 4), f)
    for l in range(L):
        for j in range(3):
            fconv[:, l, :, j] = np.asarray(inputs["ffn_conv_w"][l, j], f).reshape(44, 128).T
        fconv[:, l, :, 3] = np.asarray(inputs["ffn_conv_b"][l], f).reshape(44, 128).T
    cst = np.zeros((128, 128 + 128 + 192 + NBIS), f)
    cst[:, 0:128] = np.eye(128, dtype=f)
    for s in range(128):
        for t in range(128):
            if s // 64 == t // 64 and s <= t:
                cst[s, 128 + t] = 1.0
    cst[0:64, 256:352] = 1.0
    cst[64:128, 352:448] = 1.0
    cst[:, 448:448 + NBIS] = (2.0 ** -(np.arange(NBIS) + 1.0))[None, :]
    return {"gains": gains.reshape(128, -1), "bgate": bgate.reshape(128, -1), "mnorm": mnorm.reshape(128, -1), "lruc": lruc.reshape(128, -1),
            "lruw": lruw.reshape(128, -1), "fconv": fconv.reshape(128, -1), "cst": cst}


_NC_CACHE = {}


def kernel(**inputs):
    x = np.asarray(inputs["x"], np.float32)
    B = x.shape[0]
    hp = host_params(inputs)
    shared = {"w_in": np.ascontiguousarray(inputs["w_in"], np.float32), "w_out": np.ascontiguousarray(inputs["w_out"], np.float32),
              "ffn_up": np.ascontiguousarray(inputs["ffn_up"], np.float32), "ffn_down": np.ascontiguousarray(inputs["ffn_down"], np.float32)}
    shared.update(hp)
    if "nc" not in _NC_CACHE:
        _NC_CACHE["nc"] = build()
    nc = _NC_CACHE["nc"]
    in_maps = []
    for b in range(B):
        m = dict(shared)
        m["xT"] = np.ascontiguousarray(x[b].T)
        in_maps.append(m)
    res = run_bass_kernel_spmd(nc, in_maps, core_ids=list(range(B)))
    out = np.stack([np.ascontiguousarray(res.results[b]["yT"].T) for b in range(B)], axis=0)
    return out.astype(np.float32)
```

```python
import os
from contextlib import ExitStack
import numpy as np
import concourse.bass as bass
import concourse.mybir as mybir
from concourse.bass_utils import run_bass_kernel_spmd

F32 = mybir.dt.float32
BF16 = mybir.dt.bfloat16
AF = mybir.ActivationFunctionType
ALU = mybir.AluOpType
AX = mybir.AxisListType

S = 2048
D = 1024
DEPTH = 2
KC = 8
TG = 512
NG = S // TG
D_IN = 3404
DFF = 2816
NFC = 22
NBIS = 14
ENGS = ("pe", "act", "dve", "pool", "sp")


class Res:
    __slots__ = ("name", "w", "r", "dsem", "dcnt")

    def __init__(self, name):
        self.name = name
        self.w = None
        self.r = {}
        self.dsem = None
        self.dcnt = 0


class Prog:
    def __init__(self, nc, stack):
        self.nc = nc
        self.stack = stack
        self.items = {e: [] for e in ENGS}
        self.cnt = {e: 0 for e in ENGS}
        self.sem = {e: stack.enter_context(nc.semaphore("sem_" + e)) for e in ENGS if e != "sp"}
        self.known = {e: {f: 0 for f in ENGS} for e in ENGS}
        self.vc = {e: [None] for e in ENGS}
        self.dknown = {e: {} for e in ENGS}
        self.nres = 0
        self.ntot = 0
        self.limit = int(os.environ.get("K_LIMIT", "0")) or None
        self.force = False

    def mark(self, name):
        if os.environ.get("K_MARK"):
            print("MARK", name, self.ntot)

    def res(self, name=None):
        self.nres += 1
        return Res(name or ("r%d" % self.nres))

    def _dsem(self, r):
        if r.dsem is None:
            self.nds = getattr(self, "nds", 0) + 1
            r.dsem = self.stack.enter_context(self.nc.semaphore("d%d_%s" % (self.nds, r.name)))
        return r.dsem

    def _deps(self, eng, reads, writes):
        deps = {}
        dd = []

        def add(e_i):
            if e_i is None:
                return
            e, i = e_i
            if e == eng and eng == "pe":
                return
            if deps.get(e, 0) < i:
                deps[e] = i
        for r in reads:
            add(r.w)
            if r.dcnt:
                dd.append(r)
        for r in writes:
            add(r.w)
            for e, i in r.r.items():
                add((e, i))
            if r.dcnt:
                dd.append(r)
        waits = []
        kn = self.known[eng]
        for e, i in deps.items():
            if kn[e] < i:
                waits.append((self.sem[e], i))
                v = self.vc[e][i]
                for f in ENGS:
                    if kn[f] < v[f]:
                        kn[f] = v[f]
        dk = self.dknown[eng]
        for r in dd:
            if dk.get(id(r), 0) < r.dcnt:
                waits.append((self._dsem(r), r.dcnt))
                dk[id(r)] = r.dcnt
        return waits

    def op(self, eng, fns, reads=(), writes=()):
        if getattr(self, "skip", False):
            return
        self.ntot += 1
        if self.limit and self.ntot > self.limit and not self.force:
            return
        if not isinstance(fns, (list, tuple)):
            fns = [fns]
        waits = self._deps(eng, reads, writes)
        self.cnt[eng] += 1
        idx = self.cnt[eng]
        v = dict(self.known[eng])
        v[eng] = idx
        self.vc[eng].append(v)
        for r in reads:
            r.r[eng] = idx
        for r in writes:
            r.w = (eng, idx)
            r.r = {}
        self.items[eng].append((waits, fns, ("c", self.sem[eng])))
        return idx

    def dma(self, q, fn, reads=(), writes=()):
        if getattr(self, "skip", False):
            return
        self.ntot += 1
        if self.limit and self.ntot > self.limit and not self.force:
            return
        waits = self._deps(q, reads, writes)
        rs = list(reads) + list(writes)
        assert len(rs) == 1
        sem = self._dsem(rs[0])
        rs[0].dcnt += 16
        for r in writes:
            r.w = None
            r.r = {}
        self.items[q].append((waits, [fn], ("d", sem)))

    def wait_all_dma(self, eng, ress):
        waits = []
        for r in ress:
            if r.dcnt:
                waits.append((self._dsem(r), r.dcnt))
        self.items[eng].append((waits, [], None))

    def emit(self):
        nc = self.nc
        with nc.Block() as block:
            def run(e, items):
                for waits, fns, inc in items:
                    for s, v in waits:
                        e.wait_ge(s, v)
                    last = None
                    for f in fns:
                        last = f(e)
                    if inc is not None:
                        kind, s = inc
                        last.then_inc(s, 1 if kind == "c" else 16)

            @block.tensor
            def _(e):
                run(e, self.items["pe"])

            @block.scalar
            def _(e):
                run(e, self.items["act"])

            @block.vector
            def _(e):
                run(e, self.items["dve"])

            @block.gpsimd
            def _(e):
                run(e, self.items["pool"])

            @block.sync
            def _(e):
                run(e, self.items["sp"])


def MM(out, lhsT, rhs, start=True, stop=True, **kw):
    return lambda e: e.matmul(out=out, lhsT=lhsT, rhs=rhs, start=start, stop=stop, **kw)


def TR(out, in_, ident):
    return lambda e: e.transpose(out=out, in_=in_, identity=ident)


def ACT(out, in_, func, **kw):
    return lambda e: e.activation(out=out, in_=in_, func=func, **kw)


def TS(out, in0, s1, s2=None, op0=ALU.mult, op1=None, **kw):
    if op1 is None:
        return lambda e: e.tensor_scalar(out=out, in0=in0, scalar1=s1, scalar2=None, op0=op0, **kw)
    return lambda e: e.tensor_scalar(out=out, in0=in0, scalar1=s1, scalar2=s2, op0=op0, op1=op1, **kw)


def STT(out, in0, scalar, in1, op0, op1):
    return lambda e: e.scalar_tensor_tensor(out=out, in0=in0, scalar=scalar, in1=in1, op0=op0, op1=op1)


def TT(out, in0, in1, op):
    return lambda e: e.tensor_tensor(out=out, in0=in0, in1=in1, op=op)


def CP(out, in_):
    return lambda e: e.tensor_copy(out=out, in_=in_)


def MS(ap, v):
    return lambda e: e.memset(ap, v)


def build(depth=DEPTH, dbg=0, stop_after=None):
    nc = bass.Bass("TRN2", target_bir_lowering=False)
    dt_in = lambda n, s: nc.dram_tensor(n, list(s), F32, kind="ExternalInput").ap()
    xT_d = dt_in("xT", [D, S])
    w_in_d = dt_in("w_in", [DEPTH, D, D_IN])
    w_out_d = dt_in("w_out", [DEPTH, D, D])
    ffn_up_d = dt_in("ffn_up", [DEPTH, D, 2 * DFF])
    ffn_down_d = dt_in("ffn_down", [DEPTH, DFF, D])
    gains_d = dt_in("gains", [128, DEPTH * 4 * 8])
    bgate_d = dt_in("bgate", [128, DEPTH * 8])
    mnorm_d = dt_in("mnorm", [128, DEPTH * 384])
    lruc_d = dt_in("lruc", [128, DEPTH * 3 * 8])
    lruw_d = dt_in("lruw", [128, DEPTH * 2 * 3 * 128])
    fconv_d = dt_in("fconv", [128, DEPTH * 44 * 4])
    cst_d = dt_in("cst", [128, 128 + 128 + 192 + NBIS])
    yT_d = nc.dram_tensor("yT", [D, S], F32, kind="ExternalOutput").ap()
    upbf_d = nc.dram_tensor("ffn_up_bf", [DEPTH, 128, 11, KC, 2, 256], BF16, kind="Internal").ap()
    dnbf_d = nc.dram_tensor("ffn_dn_bf", [DEPTH, 128, 4, NFC, 256], BF16, kind="Internal").ap()
    if dbg:
        dbg_d = nc.dram_tensor("dbg", [D, S], F32, kind="ExternalOutput").ap()

    with ExitStack() as st:
        P = Prog(nc, st)
        sbt = lambda name, shape, dt: st.enter_context(nc.sbuf_tensor(name, list(shape), dt))
        pst = lambda name, shape, dt: st.enter_context(nc.psum_tensor(name, list(shape), dt))

        xT = sbt("xT_sb", [128, KC, S], F32)
        r_x = [[P.res("x%d_%d" % (c, g)) for g in range(NG)] for c in range(KC)]
        rstdb = sbt("rstdb", [128, S], F32)
        r_rstd = [P.res("rstd%d" % g) for g in range(NG)]
        gains = sbt("gains_sb", [128, DEPTH, 4, 8], F32); r_par = P.res("par")
        bgate = sbt("bgate_sb", [128, DEPTH, 8], F32)
        mnorm = sbt("mnorm_sb", [128, DEPTH, 384], F32)
        lruc = sbt("lruc_sb", [128, DEPTH, 3, 8], F32)
        lruw = sbt("lruw_sb", [128, DEPTH, 2, 3, 128], BF16); r_lruw = P.res("lruw")
        fconv = sbt("fconv_sb", [128, DEPTH, 44, 4], F32)
        cst = sbt("cst_sb", [128, 128 + 128 + 192 + NBIS], F32)
        identF = cst[:, 0:128]
        U2f = cst[:, 128:256]
        Ef = cst[:, 256:448]
        pow2 = cst[:, 448:448 + NBIS]
        identB = sbt("identB", [128, 128], BF16); r_cb = P.res("cstb")
        onesB = sbt("onesB", [128, 128], BF16)
        lrud = sbt("lrud", [128, DEPTH, 3, 4], F32); r_lrud = P.res("lrud")

        ARENA = 64000
        arena = sbt("arena", [128, ARENA], BF16)
        apos = [0]

        def carve(nelem, dt, shape=None):
            n16 = nelem * (2 if dt == F32 else 1)
            a = apos[0]
            if dt == F32 and a % 2:
                a += 1
            apos[0] = a + n16
            assert apos[0] <= ARENA, ("arena overflow", apos[0], ARENA)
            v = arena[:, a:a + n16]
            if dt == F32:
                v = v.bitcast(F32)
            return v

        pb = [pst("pb%d" % i, [128, 512], F32) for i in range(8)]
        r_pb = [P.res("pb%d" % i) for i in range(8)]

        for c in range(KC):
            for g in range(NG):
                P.dma("sp", (lambda e, c=c, g=g: e.dma_start(out=xT[:, c, g * TG:(g + 1) * TG], in_=xT_d[c * 128:(c + 1) * 128, g * TG:(g + 1) * TG])),
                      writes=[r_x[c][g]])
        smalls = [(gains, gains_d), (bgate, bgate_d), (mnorm, mnorm_d), (lruc, lruc_d), (fconv, fconv_d), (cst, cst_d)]
        r_sm = []
        for i, (t, d) in enumerate(smalls):
            r = P.res("sm%d" % i)
            r_sm.append(r)
            flat = t[:] if len(t.shape) == 2 else t[:].rearrange({3: "p a b -> p (a b)", 4: "p a b c -> p (a b c)", 5: "p a b c d -> p (a b c d)"}[len(t.shape)])
            P.dma("sp", (lambda e, flat=flat, d=d: e.dma_start(out=flat, in_=d[:, :])), writes=[r])
        r_gains, r_bgate, r_mnorm, r_lruc, r_fconv, r_cst = r_sm
        P.op("dve", CP(identB[:], identF), reads=[r_cst], writes=[r_cb])
        P.op("dve", MS(onesB[:], 1.0), writes=[r_cb])
        P.dma("pool", (lambda e: e.dma_start(out=lruw[:].rearrange("p a b c d -> p (a b c d)"), in_=lruw_d[:, :])), writes=[r_lruw])
        lam_t = sbt("lam_t", [128, DEPTH, 3], F32); r_lam = P.res("lam")
        P.op("act", ACT(lam_t[:], lruc[:, :, :, 7], AF.Exp, scale=-1.0), reads=[r_lruc], writes=[r_lam])
        P.op("act", ACT(lam_t[:], lam_t[:], AF.Ln, bias=1.0), reads=[r_lam], writes=[r_lam])
        P.op("dve", TS(lrud[:, :, :, 0], lam_t[:], -8.0), reads=[r_lam], writes=[r_lrud])
        P.op("dve", TS(lrud[:, :, :, 1], lam_t[:], -16.0), reads=[r_lam], writes=[r_lrud])

        def weight_dma(dst_ap, src_ap, res):
            P.dma("pool", (lambda e: e.dma_start(out=dst_ap, in_=src_ap)), writes=[res])

        def barrier(old, new):
            P.op("pool", MS(dummy[:], 0.0), writes=list(old) + list(new) + [r_dummy])

        dummy = sbt("dummy_sb", [128, 8], F32); r_dummy = P.res("dummy")

        def rms_stats(l, g, sq_ap, r_sq, pbank):
            tok = slice(g * TG, (g + 1) * TG)
            for c in range(KC):
                P.op("act", ACT(sq_ap[:, c, :], xT[:, c, tok], AF.Square), reads=[r_x[c][g]], writes=[r_sq])
            P.op("pe", [MM(pb[pbank][:, :], onesB[:], sq_ap[:, c, :], start=(c == 0), stop=(c == KC - 1)) for c in range(KC)],
                 reads=[r_sq, r_cb], writes=[r_pb[pbank]])
            P.op("act", ACT(rstdb[:, tok], pb[pbank][:, :], AF.Ln, scale=1.0 / D, bias=1e-6), reads=[r_pb[pbank]], writes=[r_rstd[g]])
            P.op("act", ACT(rstdb[:, tok], rstdb[:, tok], AF.Exp, scale=-0.5), reads=[r_rstd[g]], writes=[r_rstd[g]])

        def make_h(l, which, g, h_ap, r_h):
            tok = slice(g * TG, (g + 1) * TG)
            for c in range(KC):
                P.op("dve", STT(h_ap[:, c, :], xT[:, c, tok], gains[:, l, which, c:c + 1], rstdb[:, tok], ALU.mult, ALU.mult),
                     reads=[r_x[c][g], r_rstd[g], r_gains], writes=[r_h])

        def post_norm_residual(l, which, tok0, ntok, src_aps, r_src, sq_ap, r_sq, pbank, rs_ap, r_rs, tmp_ap, r_tmp):
            g = tok0 // TG
            tok = slice(tok0, tok0 + ntok)
            for c in range(KC):
                P.op("act", ACT(sq_ap[:, c, :], src_aps[c], AF.Square), reads=[r_src[c]], writes=[r_sq])
            P.op("pe", [MM(pb[pbank][:, 0:ntok], onesB[:], sq_ap[:, c, :], start=(c == 0), stop=(c == KC - 1)) for c in range(KC)],
                 reads=[r_sq, r_cb], writes=[r_pb[pbank]])
            P.op("act", ACT(rs_ap, pb[pbank][:, 0:ntok], AF.Ln, scale=1.0 / D, bias=1e-6), reads=[r_pb[pbank]], writes=[r_rs])
            P.op("act", ACT(rs_ap, rs_ap, AF.Exp, scale=-0.5), reads=[r_rs], writes=[r_rs])
            for c in range(KC):
                P.op("dve", STT(tmp_ap, src_aps[c], gains[:, l, which, c:c + 1], rs_ap, ALU.mult, ALU.mult),
                     reads=[r_src[c], r_rs, r_gains], writes=[r_tmp])
                P.op("dve", TT(xT[:, c, tok], xT[:, c, tok], tmp_ap, ALU.add), reads=[r_tmp, r_x[c][g]], writes=[r_x[c][g]])

        for l in range(depth):
            apos[0] = 0
            concat = carve(KC * S, BF16).rearrange("p (c t) -> p c t", c=KC)
            r_cat = [[P.res("cat%d_%d" % (c, t)) for t in range(16)] for c in range(KC)]
            hT = [carve(KC * TG, BF16).rearrange("p (c t) -> p c t", c=KC) for _ in range(2)]
            r_hT = [P.res("hT0"), P.res("hT1")]
            W = carve(KC * 1544, BF16).rearrange("p (c n) -> p c n", c=KC)
            r_W = P.res("W")
            pl0 = apos[0]
            all_mixer_res = [r for row in r_cat for r in row] + r_hT + [r_W]
            if l > 0:
                barrier(prev_phase_res, all_mixer_res)

            P.skip = bool(os.environ.get("K_ONLY_E"))
            P.mark("A")
            weight_dma(W[:, :, 0:1544], w_in_d[l, :, 0:1544].rearrange("(c p) n -> p c n", p=128), r_W)
            r_cv_up = P.res("cvup%d" % l)
            r_cv_dn = P.res("cvdn%d" % l)
            for rr_ in range(KC):
                for s_ in range(2):
                    P.dma("pool", (lambda e, rr_=rr_, s_=s_, l=l: e.dma_start(
                        out=upbf_d[l, :, :, rr_, s_, :],
                        in_=ffn_up_d[l, rr_ * 128:(rr_ + 1) * 128, s_ * DFF:(s_ + 1) * DFF].rearrange("p (j n) -> p j n", j=11))), writes=[r_cv_up])
            for rr_ in range(NFC):
                P.dma("pool", (lambda e, rr_=rr_, l=l: e.dma_start(
                    out=dnbf_d[l, :, :, rr_, :],
                    in_=ffn_down_d[l, rr_ * 128:(rr_ + 1) * 128, :].rearrange("p (c n) -> p c n", c=4))), writes=[r_cv_dn])
            sqb = carve(KC * TG, BF16).rearrange("p (c t) -> p c t", c=KC); r_sqb = P.res("sqb")
            qTg = carve(4 * TG, BF16).rearrange("p (h t) -> p h t", h=4); r_qTg = P.res("qTg")
            kTg = carve(4 * TG, BF16).rearrange("p (h t) -> p h t", h=4); r_kTg = P.res("kTg")
            NB = 2
            kt = [carve(384, BF16).rearrange("p (h d) -> p h d", h=4) for _ in range(NB)]; r_kt = [P.res() for _ in range(NB)]
            vx = [carve(4 * 97, BF16).rearrange("p (h d) -> p h d", h=4) for _ in range(NB)]; r_vx = [P.res() for _ in range(NB)]
            og = [carve(384, BF16) for _ in range(NB)]; r_og = [P.res() for _ in range(NB)]
            gi = [carve(8, F32) for _ in range(NB)]; r_gi = [P.res() for _ in range(NB)]
            nlf = [carve(4, F32) for _ in range(NB)]; r_nlf = [P.res() for _ in range(NB)]
            es = [carve(4, F32) for _ in range(NB)]; r_es = [P.res() for _ in range(NB)]
            emb = [carve(4, F32) for _ in range(NB)]; r_emb = [P.res() for _ in range(NB)]
            ebl = [carve(8, F32).rearrange("p (j h) -> p j h", j=2) for _ in range(NB)]; r_ebl = [P.res() for _ in range(NB)]
            tmp4 = [carve(4, F32) for _ in range(NB)]; r_tmp4 = [P.res() for _ in range(NB)]
            Stt = [carve(4 * 128, BF16).rearrange("p (h t) -> p h t", h=4) for _ in range(NB)]; r_St = [P.res() for _ in range(NB)]
            Cf = carve(4 * 97, F32).rearrange("p (h d) -> p h d", h=4); r_Cf = P.res("Cf")
            Ctmp = carve(4 * 97, F32).rearrange("p (h d) -> p h d", h=4); r_Ctmp = P.res("Ctmp")
            NCB = 4
            Cb = [carve(4 * 97, BF16).rearrange("p (h d) -> p h d", h=4) for _ in range(NCB)]; r_Cb = [P.res() for _ in range(NCB)]
            den = [carve(4, F32) for _ in range(NB)]; r_den = [P.res() for _ in range(NB)]
            hraw = [carve(384, F32).rearrange("p (h d) -> p h d", h=4) for _ in range(NB)]; r_hraw = [P.res() for _ in range(NB)]
            hsq = [carve(384, F32).rearrange("p (h d) -> p h d", h=4) for _ in range(NB)]; r_hsq = [P.res() for _ in range(NB)]
            ssq = [carve(4, F32) for _ in range(NB)]; r_ssq = [P.res() for _ in range(NB)]
            gm = [carve(384, F32) for _ in range(NB)]; r_gm = [P.res() for _ in range(NB)]
            hAb = [carve(384, BF16) for _ in range(NB)]; r_hAb = [P.res() for _ in range(NB)]
            phaseA_res = ([r_sqb, r_qTg, r_kTg, r_Cf, r_Ctmp] + r_kt + r_vx + r_og + r_gi + r_nlf + r_es + r_emb + r_ebl + r_tmp4 + r_St
                          + r_Cb + r_den + r_hraw + r_hsq + r_ssq + r_gm + r_hAb)
            if l > 0:
                barrier(prev_phase_res, phaseA_res)

            P.op("dve", MS(Cf[0:96], 0.0), writes=[r_Cf])
            P.op("dve", MS(Cb[0][0:96], 0.0), writes=[r_Cb[0]])
            for b in range(NB):
                P.op("dve", MS(vx[b][:, :, 96:97], 1.0), writes=[r_vx[b]])
            cbi = 0
            LN_SC = float(np.log(96.0 ** -0.5))
            lnsc = sbt("lnsc%d" % l, [128, 1], F32); r_lnsc = P.res("lnsc")
            P.op("dve", MS(lnsc[:], LN_SC), writes=[r_lnsc])

            for g in range(NG):
                hb = g % 2
                rms_stats(l, g, sqb, r_sqb, 7)
                make_h(l, 0, g, hT[hb], r_hT[hb])
                for qk in range(2):
                    for h in range(4):
                        bank = (qk * 4 + h) % 2
                        col0 = qk * 384 + h * 96
                        P.op("pe", [MM(pb[bank][0:96, :], W[:, c, col0:col0 + 96], hT[hb][:, c, :], start=(c == 0), stop=(c == KC - 1)) for c in range(KC)],
                             reads=[r_W, r_hT[hb]], writes=[r_pb[bank]])
                        dst = (qTg if qk == 0 else kTg)
                        P.op("act", ACT(dst[0:96, h, :], pb[bank][0:96, :], AF.Copy), reads=[r_pb[bank]], writes=[r_qTg if qk == 0 else r_kTg])
                for ti in range(4):
                    T = g * 4 + ti
                    b = T % NB
                    tl = slice(ti * 128, (ti + 1) * 128)
                    for (bank, c0, n) in ((2, 1152, 392), (3, 384, 384), (4, 768, 384)):
                        P.op("pe", [MM(pb[bank][:, 0:n], hT[hb][:, c, tl], W[:, c, c0:c0 + n], start=(c == 0), stop=(c == KC - 1)) for c in range(KC)],
                             reads=[r_W, r_hT[hb]], writes=[r_pb[bank]])
                    P.op("dve", TT(gi[b], pb[2][:, 384:392], bgate[:, l, :], ALU.add), reads=[r_pb[2], r_bgate], writes=[r_gi[b]])
                    P.op("act", ACT(nlf[b], gi[b][:, 4:8], AF.Exp, scale=-1.0), reads=[r_gi[b]], writes=[r_nlf[b]])
                    P.op("act", ACT(nlf[b], nlf[b], AF.Ln, bias=1.0), reads=[r_nlf[b]], writes=[r_nlf[b]])
                    P.op("act", ACT(og[b], pb[2][:, 0:384], AF.Sigmoid), reads=[r_pb[2], r_gi[b]], writes=[r_og[b]])
                    P.op("pe", [MM(pb[5][:, 0:4], U2f, nlf[b], True, True),
                                MM(pb[5][0:96, 8:12], Ef[:, 0:96], nlf[b], True, True),
                                MM(pb[5][0:96, 16:20], Ef[:, 96:192], nlf[b], True, True)],
                         reads=[r_nlf[b], r_cst], writes=[r_pb[5]])
                    P.op("dve", TT(tmp4[b], gi[b][:, 0:4], pb[5][:, 0:4], ALU.add), reads=[r_gi[b], r_pb[5]], writes=[r_tmp4[b]])
                    P.op("act", ACT(es[b], tmp4[b], AF.Exp, bias=lnsc[:]), reads=[r_tmp4[b], r_lnsc], writes=[r_es[b]])
                    P.op("act", ACT(emb[b], pb[5][:, 0:4], AF.Exp), reads=[r_pb[5], r_tmp4[b]], writes=[r_emb[b]])
                    P.op("act", ACT(ebl[b][0:96, 0, :], pb[5][0:96, 8:12], AF.Exp, scale=-1.0), reads=[r_pb[5]], writes=[r_ebl[b]])
                    P.op("act", ACT(ebl[b][0:96, 1, :], pb[5][0:96, 16:20], AF.Exp, scale=-1.0), reads=[r_pb[5]], writes=[r_ebl[b]])
                    P.op("dve", TT(kt[b][:], pb[3][:, 0:384].rearrange("p (h d) -> p h d", h=4), es[b].unsqueeze(2).to_broadcast([128, 4, 96]), ALU.mult),
                         reads=[r_pb[3], r_es[b]], writes=[r_kt[b]])
                    P.op("act", ACT(vx[b][:, :, 0:96], pb[4][:, 0:384].rearrange("p (h d) -> p h d", h=4), AF.Copy), reads=[r_pb[4]], writes=[r_vx[b]])
                    P.op("pe", [MM(pb[6][:, h * 128:(h + 1) * 128], kTg[0:96, h, tl], qTg[0:96, h, tl], True, True) for h in range(4)],
                         reads=[r_kTg, r_qTg], writes=[r_pb[6]])
                    for h in range(4):
                        P.op("dve", STT(Stt[b][:, h, :], pb[6][:, h * 128:(h + 1) * 128], es[b][:, h:h + 1], U2f, ALU.mult, ALU.mult),
                             reads=[r_pb[6], r_es[b], r_cst], writes=[r_St[b]])
                    ca = cbi
                    for j in range(2):
                        ps = slice(64 * j, 64 * j + 64)
                        P.op("pe", [MM(pb[7][0:96, h * 97:(h + 1) * 97], kt[b][ps, h, :], vx[b][ps, h, :], True, True) for h in range(4)],
                             reads=[r_kt[b], r_vx[b]], writes=[r_pb[7]])
                        P.op("dve", TT(Ctmp[0:96], pb[7][0:96, 0:388].rearrange("p (h d) -> p h d", h=4), Cf[0:96], ALU.add),
                             reads=[r_pb[7], r_Cf], writes=[r_Ctmp])
                        P.op("dve", TT(Cf[0:96], Ctmp[0:96], ebl[b][0:96, j, :].unsqueeze(2).to_broadcast([96, 4, 97]), ALU.mult),
                             reads=[r_Ctmp, r_ebl[b]], writes=[r_Cf])
                        nxt = (cbi + 1) % NCB
                        P.op("act", ACT(Cb[nxt][0:96], Cf[0:96], AF.Copy), reads=[r_Cf], writes=[r_Cb[nxt]])
                        cbi = nxt
                    c_a = ca
                    c_b = (ca + 1) % NCB
                    accb = 0 if (T % 2 == 0) else 1
                    fns = []
                    for h in range(4):
                        o = pb[accb][:, h * 97:(h + 1) * 97]
                        fns.append(MM(o, Stt[b][:, h, :], vx[b][:, h, :], True, False, skip_group_check=True))
                        fns.append(MM(pb[accb][0:64, h * 97:(h + 1) * 97], qTg[0:96, h, ti * 128:ti * 128 + 64], Cb[c_a][0:96, h, :], False, False, skip_group_check=True))
                        fns.append(MM(pb[accb][64:128, h * 97:(h + 1) * 97], qTg[0:96, h, ti * 128 + 64:ti * 128 + 128], Cb[c_b][0:96, h, :], False, True,
                                      skip_group_check=True, tile_position=(0, 64)))
                    P.op("pe", fns, reads=[r_St[b], r_vx[b], r_qTg, r_Cb[c_a], r_Cb[c_b]], writes=[r_pb[accb]])
                    acc = pb[accb][:, 0:388].rearrange("p (h d) -> p h d", h=4)
                    P.op("act", ACT(den[b], acc[:, :, 96], AF.Abs), reads=[r_pb[accb]], writes=[r_den[b]])
                    P.op("dve", TT(den[b], den[b], emb[b], ALU.max), reads=[r_den[b], r_emb[b]], writes=[r_den[b]])
                    P.op("dve", (lambda e, b=b: e.reciprocal(out=den[b], in_=den[b])), reads=[r_den[b]], writes=[r_den[b]])
                    P.op("dve", TT(hraw[b][:], acc[:, :, 0:96], den[b].unsqueeze(2).to_broadcast([128, 4, 96]), ALU.mult),
                         reads=[r_pb[accb], r_den[b]], writes=[r_hraw[b]])
                    P.op("dve", TT(hsq[b][:], hraw[b][:], hraw[b][:], ALU.mult), reads=[r_hraw[b]], writes=[r_hsq[b]])
                    P.op("dve", (lambda e, b=b: e.tensor_reduce(out=ssq[b], in_=hsq[b][:], axis=AX.X, op=ALU.add)), reads=[r_hsq[b]], writes=[r_ssq[b]])
                    P.op("act", ACT(ssq[b], ssq[b], AF.Ln, scale=1.0 / 96, bias=1e-6), reads=[r_ssq[b]], writes=[r_ssq[b]])
                    P.op("act", ACT(ssq[b], ssq[b], AF.Exp, scale=-0.5), reads=[r_ssq[b]], writes=[r_ssq[b]])
                    P.op("dve", TT(gm[b], og[b], mnorm[:, l, :], ALU.mult), reads=[r_og[b], r_mnorm], writes=[r_gm[b]])
                    P.op("dve", TT(hraw[b][:], hraw[b][:], ssq[b].unsqueeze(2).to_broadcast([128, 4, 96]), ALU.mult),
                         reads=[r_hraw[b], r_ssq[b]], writes=[r_hraw[b]])
                    P.op("dve", TT(hAb[b], hraw[b][:].rearrange("p h d -> p (h d)"), gm[b], ALU.mult), reads=[r_hraw[b], r_gm[b]], writes=[r_hAb[b]])
                    pT = pb[5][:].bitcast(BF16)
                    P.op("pe", [TR(pT[:, 128 * c:128 * (c + 1)], hAb[b][:, 128 * c:128 * (c + 1)], identB[:]) for c in range(3)],
                         reads=[r_hAb[b], r_cb], writes=[r_pb[5]])
                    P.op("act", ACT(concat[:, 0:3, T * 128:(T + 1) * 128], pT[:, 0:384].rearrange("p (c t) -> p c t", c=3), AF.Copy),
                         reads=[r_pb[5]], writes=[r_cat[0][T], r_cat[1][T], r_cat[2][T]])

            P.mark("B")
            apos[0] = pl0
            sqb = carve(KC * TG, BF16).rearrange("p (c t) -> p c t", c=KC); r_sqb = P.res("sqb")
            lxs = [carve(TG + 4, F32) for _ in range(3)]; r_lxs = [P.res() for _ in range(3)]
            NB = 2
            xc = [carve(TG, F32) for _ in range(NB)]; r_xc = [P.res() for _ in range(NB)]
            xcb = [carve(TG, BF16) for _ in range(NB)]; r_xcb = [P.res() for _ in range(NB)]
            rr = [carve(TG, F32) for _ in range(NB)]; r_rr = [P.res() for _ in range(NB)]
            ii = [carve(TG, F32) for _ in range(NB)]; r_ii = [P.res() for _ in range(NB)]
            aa = [carve(TG, F32) for _ in range(NB)]; r_aa = [P.res() for _ in range(NB)]
            a2 = [carve(TG, F32) for _ in range(NB)]; r_a2 = [P.res() for _ in range(NB)]
            uu = [carve(TG, F32) for _ in range(NB)]; r_uu = [P.res() for _ in range(NB)]
            hs = [carve(TG, F32) for _ in range(NB)]; r_hs = [P.res() for _ in range(NB)]
            gl = [carve(TG, F32) for _ in range(NB)]; r_gl = [P.res() for _ in range(NB)]
            hprev = carve(4, F32); r_hprev = [P.res() for _ in range(3)]
            phaseB_res = [r_sqb] + r_lxs + r_xc + r_xcb + r_rr + r_ii + r_aa + r_a2 + r_uu + r_hs + r_gl + r_hprev
            barrier(phaseA_res + [r_W], phaseB_res + [r_W])
            weight_dma(W[:, :, 0:768], w_in_d[l, :, 1544:2312].rearrange("(c p) n -> p c n", p=128), r_W)
            for c in range(3):
                P.op("dve", MS(lxs[c][:, 0:4], 0.0), writes=[r_lxs[c]])
                P.op("dve", MS(hprev[:, c:c + 1], 0.0), writes=[r_hprev[c]])
            k = 0
            for g in range(NG):
                hb = g % 2
                make_h(l, 0, g, hT[hb], r_hT[hb])
                for c in range(3):
                    b = k % NB
                    k += 1
                    blx, blg, bra, brx = (0, 1, 2, 3) if b == 0 else (4, 5, 6, 7)
                    P.op("pe", [MM(pb[blx][:, :], W[:, kc, c * 128:(c + 1) * 128], hT[hb][:, kc, :], start=(kc == 0), stop=(kc == KC - 1)) for kc in range(KC)],
                         reads=[r_W, r_hT[hb]], writes=[r_pb[blx]])
                    P.op("pe", [MM(pb[blg][:, :], W[:, kc, 384 + c * 128:384 + (c + 1) * 128], hT[hb][:, kc, :], start=(kc == 0), stop=(kc == KC - 1)) for kc in range(KC)],
                         reads=[r_W, r_hT[hb]], writes=[r_pb[blg]])
                    P.op("act", ACT(lxs[c][:, 4:4 + TG], pb[blx][:, :], AF.Copy), reads=[r_pb[blx]], writes=[r_lxs[c]])
                    P.op("dve", TS(xc[b], lxs[c][:, 4:4 + TG], lruc[:, l, c, 3:4], lruc[:, l, c, 4:5], ALU.mult, ALU.add),
                         reads=[r_lxs[c], r_lruc], writes=[r_xc[b]])
                    for j in range(3):
                        P.op("dve", STT(xc[b], lxs[c][:, 1 + j:1 + j + TG], lruc[:, l, c, j:j + 1], xc[b], ALU.mult, ALU.add),
                             reads=[r_lxs[c], r_lruc, r_xc[b]], writes=[r_xc[b]])
                    P.op("dve", CP(lxs[c][:, 1:4], lxs[c][:, TG + 1:TG + 4]), reads=[r_lxs[c]], writes=[r_lxs[c]])
                    P.op("act", ACT(xcb[b], xc[b], AF.Copy), reads=[r_xc[b]], writes=[r_xcb[b]])
                    P.op("pe", MM(pb[bra][:, :], lruw[:, l, 0, c, :], xcb[b], True, True), reads=[r_lruw, r_xcb[b]], writes=[r_pb[bra]])
                    P.op("pe", MM(pb[brx][:, :], lruw[:, l, 1, c, :], xcb[b], True, True), reads=[r_lruw, r_xcb[b]], writes=[r_pb[brx]])
                    P.op("act", ACT(rr[b], pb[bra][:, :], AF.Sigmoid, bias=lruc[:, l, c, 5:6]), reads=[r_pb[bra], r_lruc], writes=[r_rr[b]])
                    P.op("act", ACT(ii[b], pb[brx][:, :], AF.Sigmoid, bias=lruc[:, l, c, 6:7]), reads=[r_pb[brx], r_lruc], writes=[r_ii[b]])
                    P.op("act", ACT(aa[b], rr[b], AF.Exp, scale=lrud[:, l, c, 0:1]), reads=[r_rr[b], r_lrud], writes=[r_aa[b]])
                    P.op("act", ACT(a2[b], rr[b], AF.Exp, scale=lrud[:, l, c, 1:2]), reads=[r_rr[b], r_lrud], writes=[r_a2[b]])
                    P.op("act", ACT(a2[b], a2[b], AF.Sqrt, scale=-1.0, bias=1.0), reads=[r_a2[b]], writes=[r_a2[b]])
                    P.op("dve", TT(uu[b], a2[b], ii[b], ALU.mult), reads=[r_a2[b], r_ii[b]], writes=[r_uu[b]])
                    P.op("dve", TT(uu[b], uu[b], xc[b], ALU.mult), reads=[r_uu[b], r_xc[b]], writes=[r_uu[b]])
                    P.mark("scan-next")
                    SN = int(os.environ.get("K_SN", "128"))
                    for q0 in range(0, TG, SN):
                        ini = hprev[:, c:c + 1] if q0 == 0 else hs[b][:, q0 - 1:q0]
                        P.op("dve", (lambda e, b=b, ini=ini, q0=q0: e.tensor_tensor_scan(out=hs[b][:, q0:q0 + SN], data0=aa[b][:, q0:q0 + SN], data1=uu[b][:, q0:q0 + SN],
                                                                                       initial=ini, op0=ALU.mult, op1=ALU.add)),
                             reads=[r_aa[b], r_uu[b], r_hprev[c], r_hs[b]], writes=[r_hs[b]])
                    P.op("dve", CP(hprev[:, c:c + 1], hs[b][:, TG - 1:TG]), reads=[r_hs[b]], writes=[r_hprev[c]])
                    P.op("act", ACT(gl[b], pb[blg][:, :], AF.Gelu_apprx_tanh), reads=[r_pb[blg]], writes=[r_gl[b]])
                    P.op("dve", TT(concat[:, 3 + c, g * TG:(g + 1) * TG], hs[b], gl[b], ALU.mult), reads=[r_hs[b], r_gl[b]],
                         writes=[r_cat[3 + c][4 * g + i] for i in range(4)])

            P.mark("C")
            apos[0] = pl0
            akT = carve(2 * S, BF16).rearrange("p (c t) -> p c t", c=2); r_akT = [P.res() for _ in range(NG)]
            ikT = carve(S, BF16); r_ikT = [P.res() for _ in range(NG)]
            avx = carve(16 * 4 * 65, BF16).rearrange("p (t h d) -> p t h d", t=16, h=4); r_avx = [P.res() for _ in range(16)]
            iwt = carve(16 * 4, F32).rearrange("p (t h) -> p t h", t=16); r_iwt = [P.res() for _ in range(16)]
            aqT = carve(2 * TG, BF16).rearrange("p (c t) -> p c t", c=2); r_aqT = P.res("aqT")
            iqT = carve(2 * TG, BF16).rearrange("p (c t) -> p c t", c=2); r_iqT = P.res("iqT")
            sc = carve(S, F32); r_sc = P.res("sc")
            msk = carve(S, BF16); r_msk = P.res("msk")
            junk = msk; r_junk = r_msk
            mT = carve(S, BF16).rearrange("p (j q) -> p j q", j=16); r_mT = P.res("mT")
            rlb = [carve(TG, F32) for _ in range(2)]; r_rlb = [P.res() for _ in range(2)]
            exb = [carve(TG, BF16) for _ in range(2)]; r_exb = [P.res() for _ in range(2)]
            pTb = [carve(TG, BF16) for _ in range(2)]; r_pTb = [P.res() for _ in range(2)]
            bis = carve(8 + 2 * NBIS, F32); r_bis = P.res("bis")
            rc4 = carve(4, F32); r_rc4 = P.res("rc4")
            hcb = carve(256, BF16); r_hcb = P.res("hcb")
            phaseC_res = (r_akT + r_ikT + r_avx + r_iwt + [r_aqT, r_iqT, r_sc, r_msk, r_mT, r_bis, r_rc4, r_hcb] + r_rlb + r_exb + r_pTb)
            barrier(phaseB_res + [r_W], phaseC_res + [r_W])
            wsrc = w_in_d[l]
            r3 = lambda a, b_: wsrc[:, a:b_].rearrange("(c p) n -> p c n", p=128)
            weight_dma(W[:, :, 0:512], r3(2312, 2824), r_W)
            weight_dma(W[:, :, 512:768], r3(3080, 3336), r_W)
            weight_dma(W[:, :, 768:832], r3(3336, 3400), r_W)
            weight_dma(W[:, :, 832:896], r3(3336, 3400), r_W)
            weight_dma(W[:, :, 896:1152], r3(2824, 3080), r_W)
            weight_dma(W[:, :, 1152:1156], r3(3400, 3404), r_W)
            for T in range(16):
                P.op("dve", MS(avx[:, T, :, 64:65], 1.0), writes=[r_avx[T]])
            for g in range(NG):
                hb = g % 2
                make_h(l, 0, g, hT[hb], r_hT[hb])
                tokg = slice(g * TG, (g + 1) * TG)
                plan = [(0, aqT[:, 0, :], [r_aqT]), (128, aqT[:, 1, :], [r_aqT]), (256, akT[:, 0, tokg], [r_akT[g]]), (384, akT[:, 1, tokg], [r_akT[g]]),
                        (512, iqT[:, 0, :], [r_iqT]), (640, iqT[:, 1, :], [r_iqT]), (768, ikT[:, tokg], [r_ikT[g]])]
                for i, (c0, dst, rw) in enumerate(plan):
                    bank = i % 2
                    P.op("pe", [MM(pb[bank][:, :], W[:, kc, c0:c0 + 128], hT[hb][:, kc, :], start=(kc == 0), stop=(kc == KC - 1)) for kc in range(KC)],
                         reads=[r_W, r_hT[hb]], writes=[r_pb[bank]])
                    P.op("act", ACT(dst, pb[bank][:, :], AF.Copy), reads=[r_pb[bank]], writes=rw)
                for ti in range(4):
                    T = 4 * g + ti
                    bank = 2 + (ti % 2)
                    P.op("pe", [MM(pb[bank][:, 0:260], hT[hb][:, kc, ti * 128:(ti + 1) * 128], W[:, kc, 896:1156], start=(kc == 0), stop=(kc == KC - 1)) for kc in range(KC)],
                         reads=[r_W, r_hT[hb]], writes=[r_pb[bank]])
                    P.op("act", ACT(avx[:, T, :, 0:64], pb[bank][:, 0:256].rearrange("p (h d) -> p h d", h=4), AF.Copy), reads=[r_pb[bank]], writes=[r_avx[T]])
                    P.op("act", ACT(iwt[:, T, :], pb[bank][:, 256:260], AF.Copy), reads=[r_pb[bank]], writes=[r_iwt[T]])
                for ti in range(4):
                    T = 4 * g + ti
                    nk = 128 * (T + 1)
                    ql = slice(ti * 128, (ti + 1) * 128)
                    nkb = (nk + 511) // 512
                    key_res_k = [r_ikT[gg] for gg in range(g + 1)]
                    cnt = 0
                    for h in range(4):
                        hp = slice(64 * (h % 2), 64 * (h % 2) + 64)
                        for kb in range(nkb):
                            k0 = kb * 512
                            w = min(512, nk - k0)
                            bank = 4 + (cnt % 2)
                            rb = cnt % 2
                            cnt += 1
                            P.op("pe", MM(pb[bank][:, 0:w], iqT[hp, h // 2, ql], ikT[hp, k0:k0 + w], True, True),
                                 reads=[r_iqT] + key_res_k, writes=[r_pb[bank]])
                            P.op("act", ACT(rlb[rb][:, 0:w], pb[bank][:, 0:w], AF.Relu), reads=[r_pb[bank]], writes=[r_rlb[rb]])
                            if h == 0:
                                P.op("dve", TS(sc[:, k0:k0 + w], rlb[rb][:, 0:w], iwt[:, T, 0:1]), reads=[r_rlb[rb], r_iwt[T]], writes=[r_sc])
                            else:
                                P.op("dve", STT(sc[:, k0:k0 + w], rlb[rb][:, 0:w], iwt[:, T, h:h + 1], sc[:, k0:k0 + w], ALU.mult, ALU.add),
                                     reads=[r_rlb[rb], r_iwt[T], r_sc], writes=[r_sc])
                    if T >= 2:
                        P.op("dve", (lambda e, nk=nk: e.tensor_reduce(out=bis[:, 0:1], in_=sc[:, 0:nk], axis=AX.X, op=ALU.max)), reads=[r_sc], writes=[r_bis])
                        P.op("dve", (lambda e, nk=nk: e.tensor_reduce(out=bis[:, 1:2], in_=sc[:, 0:nk], axis=AX.X, op=ALU.min)), reads=[r_sc], writes=[r_bis])
                    P.op("dve", MS(sc[0:64, nk - 64:nk], -1e30), reads=[r_sc], writes=[r_sc])
                    if T >= 2:
                        P.op("dve", TT(bis[:, 2:3], bis[:, 0:1], bis[:, 1:2], ALU.subtract), reads=[r_bis], writes=[r_bis])
                        P.op("dve", TS(bis[:, 8:8 + NBIS], pow2, bis[:, 2:3]), reads=[r_bis, r_cst], writes=[r_bis])
                        P.op("dve", TS(bis[:, 8 + NBIS:8 + 2 * NBIS], bis[:, 8:8 + NBIS], -0.5), reads=[r_bis], writes=[r_bis])
                        P.op("dve", TT(bis[:, 3:4], bis[:, 1:2], bis[:, 8:9], ALU.add), reads=[r_bis], writes=[r_bis])
                        for kk in range(NBIS):
                            P.op("dve", (lambda e, nk=nk: e.tensor_scalar(out=junk[:, 0:nk], in0=sc[:, 0:nk], scalar1=bis[:, 3:4], scalar2=0.0,
                                                                            op0=ALU.is_ge, op1=ALU.add, accum_out=bis[:, 4:5])),
                                 reads=[r_sc, r_bis], writes=[r_msk, r_bis])
                            P.op("dve", STT(bis[:, 5:6], bis[:, 4:5], 255.5, bis[:, 8 + kk:9 + kk], ALU.is_ge, ALU.mult), reads=[r_bis], writes=[r_bis])
                            P.op("dve", STT(bis[:, 3:4], bis[:, 5:6], bis[:, 8 + NBIS + kk:9 + NBIS + kk], bis[:, 3:4], ALU.add, ALU.add), reads=[r_bis], writes=[r_bis])
                        P.op("dve", TT(bis[:, 3:4], bis[:, 3:4], bis[:, 8 + 2 * NBIS - 1:8 + 2 * NBIS], ALU.add), reads=[r_bis], writes=[r_bis])
                        P.op("dve", TS(msk[:, 0:nk], sc[:, 0:nk], bis[:, 3:4], None, ALU.is_ge), reads=[r_sc, r_bis], writes=[r_msk])
                    else:
                        P.op("dve", TS(msk[:, 0:nk], sc[:, 0:nk], -1e29, None, ALU.is_ge), reads=[r_sc], writes=[r_msk])
                    nb_ = T + 1
                    for j0 in range(0, nb_, 8):
                        bank = 6 + ((j0 // 8) % 2)
                        pT = pb[bank][:].bitcast(BF16)
                        n = min(8, nb_ - j0)
                        P.op("pe", [TR(pT[:, 128 * i:128 * (i + 1)], msk[:, (j0 + i) * 128:(j0 + i + 1) * 128], identB[:]) for i in range(n)],
                             reads=[r_msk, r_cb], writes=[r_pb[bank]])
                        P.op("act", ACT(mT[:, j0:j0 + n, :], pT[:, 0:128 * n].rearrange("p (j q) -> p j q", j=n), AF.Copy), reads=[r_pb[bank]], writes=[r_mT])
                    key_res_a = [r_akT[gg] for gg in range(g + 1)]
                    cnt = 0
                    for h in range(4):
                        hp = slice(64 * (h % 2), 64 * (h % 2) + 64)
                        groups = [(j0, min(4, nb_ - j0)) for j0 in range(0, nb_, 4)]
                        for gi_, (j0, n) in enumerate(groups):
                            bank = 4 + (cnt % 2)
                            eb = cnt % 2
                            cnt += 1
                            P.op("pe", [MM(pb[bank][:, 128 * i:128 * (i + 1)], akT[hp, h // 2, (j0 + i) * 128:(j0 + i + 1) * 128], aqT[hp, h // 2, ql], True, True) for i in range(n)],
                                 reads=[r_aqT] + key_res_a, writes=[r_pb[bank]])
                            P.op("act", ACT(exb[eb][:, 0:128 * n], pb[bank][:, 0:128 * n], AF.Exp, scale=0.125), reads=[r_pb[bank]], writes=[r_exb[eb]])
                            P.op("dve", TT(pTb[eb][:, 0:128 * n], exb[eb][:, 0:128 * n], mT[:, j0:j0 + n, :].rearrange("p j q -> p (j q)"), ALU.mult),
                                 reads=[r_exb[eb], r_mT], writes=[r_pTb[eb]])
                            P.op("pe", [MM(pb[3][:, h * 65:(h + 1) * 65], pTb[eb][:, 128 * i:128 * (i + 1)], avx[:, j0 + i, h, :],
                                           start=(j0 + i == 0), stop=(j0 + i == nb_ - 1), skip_group_check=True) for i in range(n)],
                                 reads=[r_pTb[eb]] + [r_avx[j0 + i] for i in range(n)], writes=[r_pb[3]])
                    oacc = pb[3][:, 0:260].rearrange("p (h d) -> p h d", h=4)
                    P.op("dve", (lambda e: e.reciprocal(out=rc4, in_=oacc[:, :, 64])), reads=[r_pb[3]], writes=[r_rc4])
                    P.op("dve", TT(hcb.rearrange("p (h d) -> p h d", h=4), oacc[:, :, 0:64], rc4.unsqueeze(2).to_broadcast([128, 4, 64]), ALU.mult),
                         reads=[r_pb[3], r_rc4], writes=[r_hcb])
                    pT = pb[2][:].bitcast(BF16)
                    P.op("pe", [TR(pT[:, 128 * c:128 * (c + 1)], hcb[:, 128 * c:128 * (c + 1)], identB[:]) for c in range(2)],
                         reads=[r_hcb, r_cb], writes=[r_pb[2]])
                    P.op("act", ACT(concat[:, 6:8, T * 128:(T + 1) * 128], pT[:, 0:256].rearrange("p (c t) -> p c t", c=2), AF.Copy),
                         reads=[r_pb[2]], writes=[r_cat[6][T], r_cat[7][T]])

            P.mark("Cend")
            if dbg:
                P.force = True
            apos[0] = pl0
            if dbg and l == dbg - 1:
                dtmp = carve(S, F32); r_dtmp = P.res("dtmp")
                barrier(phaseC_res, [r_dtmp])
                for c in range(KC):
                    P.op("dve", CP(dtmp[:], concat[:, c, :]), reads=[r_cat[c][t] for t in range(16)], writes=[r_dtmp])
                    P.dma("sp", (lambda e, c=c: e.dma_start(out=dbg_d[c * 128:(c + 1) * 128, :], in_=dtmp[:])), reads=[r_dtmp])

            if stop_after == "C" and l == depth - 1:
                break
            NTD = 256
            sqd = carve(KC * NTD, BF16).rearrange("p (c t) -> p c t", c=KC); r_sqd = P.res("sqd")
            rsd = carve(NTD, F32); r_rsd = P.res("rsd")
            tmpd = carve(NTD, F32); r_tmpd = P.res("tmpd")
            phaseD_res = [r_sqd, r_rsd, r_tmpd]
            barrier(phaseC_res + [r_W], phaseD_res + [r_W])
            weight_dma(W[:, :, 0:1024], w_out_d[l].rearrange("(c p) n -> p c n", p=128), r_W)
            for gd in range(S // NTD):
                t0 = gd * NTD
                tiles = [t0 // 128, t0 // 128 + 1]
                for c in range(KC):
                    bank = c // 2
                    half = (c % 2) * NTD
                    P.op("pe", [MM(pb[bank][:, half:half + NTD], W[:, kc, c * 128:(c + 1) * 128], concat[:, kc, t0:t0 + NTD], start=(kc == 0), stop=(kc == KC - 1)) for kc in range(KC)],
                         reads=[r_W] + [r_cat[kc][t] for kc in range(KC) for t in tiles], writes=[r_pb[bank]])
                srcs = [pb[c // 2][:, (c % 2) * NTD:(c % 2) * NTD + NTD] for c in range(KC)]
                post_norm_residual(l, 1, t0, NTD, srcs, [r_pb[c // 2] for c in range(KC)], sqd, r_sqd, 4, rsd, r_rsd, tmpd, r_tmpd)

            if stop_after == "D" and l == depth - 1:
                break

            P.skip = False
            P.mark("E")
            apos[0] = 0
            hTf = [carve(KC * TG, BF16).rearrange("p (c t) -> p c t", c=KC) for _ in range(2)]; r_hTf = [P.res() for _ in range(2)]
            sqf = carve(KC * TG, BF16).rearrange("p (c t) -> p c t", c=KC); r_sqf = P.res("sqf")
            actg = carve(NFC * TG, BF16).rearrange("p (k t) -> p k t", k=NFC); r_actg = [P.res() for _ in range(NFC)]
            ost = carve(KC * TG, F32).rearrange("p (c t) -> p c t", c=KC); r_ost = [P.res() for _ in range(KC)]
            NWU = 2
            wu = [carve(KC * 2 * 256, BF16).rearrange("p (c s n) -> p c s n", c=KC, s=2) for _ in range(NWU)]; r_wu = [P.res() for _ in range(NWU)]
            NWD = 2
            wd = [carve(NFC * 256, BF16).rearrange("p (k n) -> p k n", k=NFC) for _ in range(NWD)]; r_wd = [P.res() for _ in range(NWD)]
            NU = 2
            ub = [[carve(TG + 2, F32) for _ in range(2)] for _ in range(NU)]; r_ub = [[P.res() for _ in range(2)] for _ in range(NU)]
            cv = [[carve(TG, F32) for _ in range(2)] for _ in range(NU)]; r_cv = [[P.res() for _ in range(2)] for _ in range(NU)]
            uh = carve(44 * 2, F32).rearrange("p (f t) -> p f t", f=44); r_uh = [P.res() for _ in range(44)]
            rsf = carve(TG, F32); r_rsf = P.res("rsf")
            tmpf = carve(TG, F32); r_tmpf = P.res("tmpf")
            phaseE_res = (r_hTf + [r_sqf, r_rsf, r_tmpf] + r_actg + r_ost + r_wu + r_wd + [x for y in r_ub for x in y] + [x for y in r_cv for x in y] + r_uh)
            barrier(all_mixer_res + phaseD_res, phaseE_res)
            P.op("dve", MS(uh[:].rearrange("p f t -> p (f t)"), 0.0), writes=r_uh)
            wu_i = 0
            wd_i = 0
            up_d = upbf_d[l]
            dn_d = dnbf_d[l]
            P.wait_all_dma("sp", [r_cv_up, r_cv_dn])

            def weight_dma_sp(dst_ap, src_ap, res):
                P.dma("sp", (lambda e: e.dma_start(out=dst_ap, in_=src_ap)), writes=[res])
            pair_k = 0
            for g in range(NG):
                hb = g % 2
                rms_stats(l, g, sqf, r_sqf, 7)
                make_h(l, 2, g, hTf[hb], r_hTf[hb])
                for j2 in range(NFC // 2):
                    ws = wu_i % NWU
                    wu_i += 1
                    weight_dma_sp(wu[ws][:], up_d[:, j2], r_wu[ws])
                    for jj in range(2):
                        j = 2 * j2 + jj
                        u = pair_k % NU
                        pair_k += 1
                        bg, bu = (0, 1) if u == 0 else (2, 3)
                        for s_, bank in ((0, bg), (1, bu)):
                            P.op("pe", [MM(pb[bank][:, :], wu[ws][:, kc, s_, jj * 128:(jj + 1) * 128], hTf[hb][:, kc, :], start=(kc == 0), stop=(kc == KC - 1)) for kc in range(KC)],
                                 reads=[r_wu[ws], r_hTf[hb]], writes=[r_pb[bank]])
                        for s_, bank in ((0, bg), (1, bu)):
                            f = j + s_ * NFC
                            ubx = ub[u][s_]; rub = r_ub[u][s_]
                            cvx = cv[u][s_]; rcv = r_cv[u][s_]
                            P.op("act", ACT(ubx[:, 2:2 + TG], pb[bank][:, :], AF.Copy), reads=[r_pb[bank]], writes=[rub])
                            P.op("act", ACT(ubx[:, 0:2], uh[:, f, :], AF.Copy), reads=[r_uh[f]], writes=[rub])
                            P.op("act", ACT(cvx, pb[bank][:, :], AF.Identity, scale=fconv[:, l, f, 2:3], bias=fconv[:, l, f, 3:4]),
                                 reads=[r_pb[bank], r_fconv], writes=[rcv])
                            P.op("dve", STT(cvx, ubx[:, 1:1 + TG], fconv[:, l, f, 1:2], cvx, ALU.mult, ALU.add), reads=[rub, r_fconv, rcv], writes=[rcv])
                            P.op("dve", STT(cvx, ubx[:, 0:TG], fconv[:, l, f, 0:1], cvx, ALU.mult, ALU.add), reads=[rub, r_fconv, rcv], writes=[rcv])
                            P.op("act", ACT(uh[:, f, :], ubx[:, TG:TG + 2], AF.Copy), reads=[rub], writes=[r_uh[f]])
                        P.op("act", ACT(cv[u][0], cv[u][0], AF.Gelu_apprx_tanh), reads=[r_cv[u][0]], writes=[r_cv[u][0]])
                        P.op("dve", TT(actg[:, j, :], cv[u][0], cv[u][1], ALU.mult), reads=[r_cv[u][0], r_cv[u][1]], writes=[r_actg[j]])
                for c2 in range(4):
                    ws = wd_i % NWD
                    wd_i += 1
                    weight_dma_sp(wd[ws][:], dn_d[:, c2], r_wd[ws])
                    for cc in range(2):
                        c = 2 * c2 + cc
                        bank = 4 + (c % 2)
                        P.op("pe", [MM(pb[bank][:, :], wd[ws][:, k_, cc * 128:(cc + 1) * 128], actg[:, k_, :], start=(k_ == 0), stop=(k_ == NFC - 1)) for k_ in range(NFC)],
                             reads=[r_wd[ws]] + r_actg, writes=[r_pb[bank]])
                        P.op("act", ACT(ost[:, c, :], pb[bank][:, :], AF.Copy), reads=[r_pb[bank]], writes=[r_ost[c]])
                post_norm_residual(l, 3, g * TG, TG, [ost[:, c, :] for c in range(KC)], r_ost, sqf, r_sqf, 6, rsf, r_rsf, tmpf, r_tmpf)
            prev_phase_res = phaseE_res

        P.force = True
        for c in range(KC):
            for g in range(NG):
                P.dma("sp", (lambda e, c=c, g=g: e.dma_start(out=yT_d[c * 128:(c + 1) * 128, g * TG:(g + 1) * TG], in_=xT[:, c, g * TG:(g + 1) * TG])),
                      reads=[r_x[c][g]])
        P.wait_all_dma("sp", [r_x[c][g] for c in range(KC) for g in range(NG)] + ([r_dtmp] if dbg else []))
        P.emit()
    return nc


def host_params(inputs):
    f = np.float32
    L = DEPTH
    gains = np.zeros((128, L, 4, 8), f)
    for l in range(L):
        for i, nm in enumerate(("norm_mix_pre", "norm_mix_post", "norm_ffn_pre", "norm_ffn_post")):
            gains[:, l, i, :] = np.asarray(inputs[nm][l], f).reshape(8, 128).T
    bgate = np.zeros((128, L, 8), f)
    bgate[:, :, 0:4] = np.asarray(inputs["b_igate"], f)[None]
    bgate[:, :, 4:8] = np.asarray(inputs["b_fgate"], f)[None]
    mnorm = np.broadcast_to(np.asarray(inputs["mlstm_norm"], f)[None], (128, L, 384)).copy()
    lruc = np.zeros((128, L, 3, 8), f)
    for l in range(L):
        for j in range(4):
            lruc[:, l, :, j] = np.asarray(inputs["lru_conv_w"][l, j], f).reshape(3, 128).T
        lruc[:, l, :, 4] = np.asarray(inputs["lru_conv_b"][l], f).reshape(3, 128).T
        lruc[:, l, :, 5] = np.asarray(inputs["lru_b_a"][l], f).reshape(3, 128).T
        lruc[:, l, :, 6] = np.asarray(inputs["lru_b_x"][l], f).reshape(3, 128).T
        lruc[:, l, :, 7] = np.asarray(inputs["lru_lambda"][l], f).reshape(3, 128).T
    lruw = np.zeros((128, L, 2, 3, 128), f)
    for l in range(L):
        for i, nm in enumerate(("lru_w_a", "lru_w_x")):
            w = np.asarray(inputs[nm][l], f)
            for c in range(3):
                for a in range(2):
                    lruw[64 * a:64 * a + 64, l, i, c, 64 * a:64 * a + 64] = w[2 * c + a]
    fconv = np.zeros((128, L, 44, 4), f)
    for l in range(L):
        for j in range(3):
            fconv[:, l, :, j] = np.asarray(inputs["ffn_conv_w"][l, j], f).reshape(44, 128).T
        fconv[:, l, :, 3] = np.asarray(inputs["ffn_conv_b"][l], f).reshape(44, 128).T
    cst = np.zeros((128, 128 + 128 + 192 + NBIS), f)
    cst[:, 0:128] = np.eye(128, dtype=f)
    for s in range(128):
        for t in range(128):
            if s // 64 == t // 64 and s <= t:
                cst[s, 128 + t] = 1.0
    cst[0:64, 256:352] = 1.0
    cst[64:128, 352:448] = 1.0
    cst[:, 448:448 + NBIS] = (2.0 ** -(np.arange(NBIS) + 1.0))[None, :]
    return {"gains": gains.reshape(128, -1), "bgate": bgate.reshape(128, -1), "mnorm": mnorm.reshape(128, -1), "lruc": lruc.reshape(128, -1),
            "lruw": lruw.reshape(128, -1), "fconv": fconv.reshape(128, -1), "cst": cst}


_NC_CACHE = {}


def kernel(**inputs):
    x = np.asarray(inputs["x"], np.float32)
    B = x.shape[0]
    hp = host_params(inputs)
    shared = {"w_in": np.ascontiguousarray(inputs["w_in"], np.float32), "w_out": np.ascontiguousarray(inputs["w_out"], np.float32),
              "ffn_up": np.ascontiguousarray(inputs["ffn_up"], np.float32), "ffn_down": np.ascontiguousarray(inputs["ffn_down"], np.float32)}
    shared.update(hp)
    if "nc" not in _NC_CACHE:
        _NC_CACHE["nc"] = build()
    nc = _NC_CACHE["nc"]
    in_maps = []
    for b in range(B):
        m = dict(shared)
        m["xT"] = np.ascontiguousarray(x[b].T)
        in_maps.append(m)
    res = run_bass_kernel_spmd(nc, in_maps, core_ids=list(range(B)))
    out = np.stack([np.ascontiguousarray(res.results[b]["yT"].T) for b in range(B)], axis=0)
    return out.astype(np.float32)
```

```python
import os
from contextlib import ExitStack
import numpy as np
import concourse.bass as bass
import concourse.mybir as mybir
from concourse.bass_utils import run_bass_kernel_spmd

F32 = mybir.dt.float32
BF16 = mybir.dt.bfloat16
AF = mybir.ActivationFunctionType
ALU = mybir.AluOpType
AX = mybir.AxisListType

S = 2048
D = 1024
DEPTH = 2
KC = 8
TG = 512
NG = S // TG
D_IN = 3404
DFF = 2816
NFC = 22
NBIS = 12
ENGS = ("pe", "act", "dve", "pool", "sp")


class Res:
    __slots__ = ("name", "w", "r", "dsem", "dcnt")

    def __init__(self, name):
        self.name = name
        self.w = None
        self.r = {}
        self.dsem = None
        self.dcnt = 0


class Prog:
    def __init__(self, nc, stack):
        self.nc = nc
        self.stack = stack
        self.items = {e: [] for e in ENGS}
        self.cnt = {e: 0 for e in ENGS}
        self.sem = {e: stack.enter_context(nc.semaphore("sem_" + e)) for e in ENGS if e != "sp"}
        self.known = {e: {f: 0 for f in ENGS} for e in ENGS}
        self.vc = {e: [None] for e in ENGS}
        self.dknown = {e: {} for e in ENGS}
        self.nres = 0
        self.ntot = 0
        self.limit = int(os.environ.get("K_LIMIT", "0")) or None
        self.force = False

    def mark(self, name):
        if os.environ.get("K_MARK"):
            print("MARK", name, self.ntot)

    def res(self, name=None):
        self.nres += 1
        return Res(name or ("r%d" % self.nres))

    def _dsem(self, r):
        if r.dsem is None:
            self.nds = getattr(self, "nds", 0) + 1
            r.dsem = self.stack.enter_context(self.nc.semaphore("d%d_%s" % (self.nds, r.name)))
        return r.dsem

    def _deps(self, eng, reads, writes):
        deps = {}
        dd = []

        def add(e_i):
            if e_i is None:
                return
            e, i = e_i
            if e == eng and eng == "pe":
                return
            if deps.get(e, 0) < i:
                deps[e] = i
        for r in reads:
            add(r.w)
            if r.dcnt:
                dd.append(r)
        for r in writes:
            add(r.w)
            for e, i in r.r.items():
                add((e, i))
            if r.dcnt:
                dd.append(r)
        waits = []
        kn = self.known[eng]
        for e, i in deps.items():
            if kn[e] < i:
                waits.append((self.sem[e], i))
                v = self.vc[e][i]
                for f in ENGS:
                    if kn[f] < v[f]:
                        kn[f] = v[f]
        dk = self.dknown[eng]
        for r in dd:
            if dk.get(id(r), 0) < r.dcnt:
                waits.append((self._dsem(r), r.dcnt))
                dk[id(r)] = r.dcnt
        return waits

    def op(self, eng, fns, reads=(), writes=()):
        if getattr(self, "skip", False):
            return
        self.ntot += 1
        if self.limit and self.ntot > self.limit and not self.force:
            return
        if not isinstance(fns, (list, tuple)):
            fns = [fns]
        waits = self._deps(eng, reads, writes)
        self.cnt[eng] += 1
        idx = self.cnt[eng]
        v = dict(self.known[eng])
        v[eng] = idx
        self.vc[eng].append(v)
        for r in reads:
            r.r[eng] = idx
        for r in writes:
            r.w = (eng, idx)
            r.r = {}
        self.items[eng].append((waits, fns, ("c", self.sem[eng])))
        return idx

    def dma(self, q, fn, reads=(), writes=()):
        if getattr(self, "skip", False):
            return
        self.ntot += 1
        if self.limit and self.ntot > self.limit and not self.force:
            return
        waits = self._deps(q, reads, writes)
        rs = list(reads) + list(writes)
        assert len(rs) == 1
        sem = self._dsem(rs[0])
        rs[0].dcnt += 16
        for r in writes:
            r.w = None
            r.r = {}
        self.items[q].append((waits, [fn], ("d", sem)))

    def wait_all_dma(self, eng, ress):
        waits = []
        for r in ress:
            if r.dcnt:
                waits.append((self._dsem(r), r.dcnt))
        self.items[eng].append((waits, [], None))

    def emit(self):
        nc = self.nc
        with nc.Block() as block:
            def run(e, items):
                for waits, fns, inc in items:
                    for s, v in waits:
                        e.wait_ge(s, v)
                    last = None
                    for f in fns:
                        last = f(e)
                    if inc is not None:
                        kind, s = inc
                        last.then_inc(s, 1 if kind == "c" else 16)

            @block.tensor
            def _(e):
                run(e, self.items["pe"])

            @block.scalar
            def _(e):
                run(e, self.items["act"])

            @block.vector
            def _(e):
                run(e, self.items["dve"])

            @block.gpsimd
            def _(e):
                run(e, self.items["pool"])

            @block.sync
            def _(e):
                run(e, self.items["sp"])


def MM(out, lhsT, rhs, start=True, stop=True, **kw):
    return lambda e: e.matmul(out=out, lhsT=lhsT, rhs=rhs, start=start, stop=stop, **kw)


def TR(out, in_, ident):
    return lambda e: e.transpose(out=out, in_=in_, identity=ident)


def ACT(out, in_, func, **kw):
    return lambda e: e.activation(out=out, in_=in_, func=func, **kw)


def TS(out, in0, s1, s2=None, op0=ALU.mult, op1=None, **kw):
    if op1 is None:
        return lambda e: e.tensor_scalar(out=out, in0=in0, scalar1=s1, scalar2=None, op0=op0, **kw)
    return lambda e: e.tensor_scalar(out=out, in0=in0, scalar1=s1, scalar2=s2, op0=op0, op1=op1, **kw)


def STT(out, in0, scalar, in1, op0, op1):
    return lambda e: e.scalar_tensor_tensor(out=out, in0=in0, scalar=scalar, in1=in1, op0=op0, op1=op1)


def TT(out, in0, in1, op):
    return lambda e: e.tensor_tensor(out=out, in0=in0, in1=in1, op=op)


def CP(out, in_):
    return lambda e: e.tensor_copy(out=out, in_=in_)


def MS(ap, v):
    return lambda e: e.memset(ap, v)


def build(depth=DEPTH, dbg=0, stop_after=None):
    nc = bass.Bass("TRN2", target_bir_lowering=False)
    dt_in = lambda n, s: nc.dram_tensor(n, list(s), F32, kind="ExternalInput").ap()
    xT_d = dt_in("xT", [D, S])
    w_in_d = dt_in("w_in", [DEPTH, D, D_IN])
    w_out_d = dt_in("w_out", [DEPTH, D, D])
    ffn_up_d = dt_in("ffn_up", [DEPTH, D, 2 * DFF])
    ffn_down_d = dt_in("ffn_down", [DEPTH, DFF, D])
    gains_d = dt_in("gains", [128, DEPTH * 4 * 8])
    bgate_d = dt_in("bgate", [128, DEPTH * 8])
    mnorm_d = dt_in("mnorm", [128, DEPTH * 384])
    lruc_d = dt_in("lruc", [128, DEPTH * 3 * 8])
    lruw_d = dt_in("lruw", [128, DEPTH * 2 * 3 * 128])
    fconv_d = dt_in("fconv", [128, DEPTH * 44 * 4])
    cst_d = dt_in("cst", [128, 128 + 128 + 192 + NBIS])
    yT_d = nc.dram_tensor("yT", [D, S], F32, kind="ExternalOutput").ap()
    upbf_d = nc.dram_tensor("ffn_up_bf", [DEPTH, 11, 128, KC * 2 * 256], BF16, kind="Internal").ap()
    dnbf_d = nc.dram_tensor("ffn_dn_bf", [DEPTH, 4, 128, NFC * 256], BF16, kind="Internal").ap()
    if dbg:
        dbg_d = nc.dram_tensor("dbg", [D, S], F32, kind="ExternalOutput").ap()

    with ExitStack() as st:
        P = Prog(nc, st)
        sbt = lambda name, shape, dt: st.enter_context(nc.sbuf_tensor(name, list(shape), dt))
        pst = lambda name, shape, dt: st.enter_context(nc.psum_tensor(name, list(shape), dt))

        xT = sbt("xT_sb", [128, KC, S], F32)
        r_x = [[P.res("x%d_%d" % (c, g)) for g in range(NG)] for c in range(KC)]
        rstdb = sbt("rstdb", [128, S], F32)
        r_rstd = [P.res("rstd%d" % g) for g in range(NG)]
        gains = sbt("gains_sb", [128, DEPTH, 4, 8], F32); r_par = P.res("par")
        bgate = sbt("bgate_sb", [128, DEPTH, 8], F32)
        mnorm = sbt("mnorm_sb", [128, DEPTH, 384], F32)
        lruc = sbt("lruc_sb", [128, DEPTH, 3, 8], F32)
        lruw = sbt("lruw_sb", [128, DEPTH, 2, 3, 128], BF16); r_lruw = P.res("lruw")
        fconv = sbt("fconv_sb", [128, DEPTH, 44, 4], F32)
        cst = sbt("cst_sb", [128, 128 + 128 + 192 + NBIS], F32)
        identF = cst[:, 0:128]
        U2f = cst[:, 128:256]
        Ef = cst[:, 256:448]
        pow2 = cst[:, 448:448 + NBIS]
        identB = sbt("identB", [128, 128], BF16); r_cb = P.res("cstb")
        onesB = sbt("onesB", [128, 128], BF16)
        lrud = sbt("lrud", [128, DEPTH, 3, 4], F32); r_lrud = P.res("lrud")

        ARENA = 64000
        arena = sbt("arena", [128, ARENA], BF16)
        apos = [0]

        def carve(nelem, dt, shape=None):
            n16 = nelem * (2 if dt == F32 else 1)
            a = apos[0]
            if dt == F32 and a % 2:
                a += 1
            apos[0] = a + n16
            assert apos[0] <= ARENA, ("arena overflow", apos[0], ARENA)
            v = arena[:, a:a + n16]
            if dt == F32:
                v = v.bitcast(F32)
            return v

        pb = [pst("pb%d" % i, [128, 512], F32) for i in range(8)]
        r_pb = [P.res("pb%d" % i) for i in range(8)]

        for c in range(KC):
            for g in range(NG):
                P.dma("sp", (lambda e, c=c, g=g: e.dma_start(out=xT[:, c, g * TG:(g + 1) * TG], in_=xT_d[c * 128:(c + 1) * 128, g * TG:(g + 1) * TG])),
                      writes=[r_x[c][g]])
        smalls = [(gains, gains_d), (bgate, bgate_d), (mnorm, mnorm_d), (lruc, lruc_d), (fconv, fconv_d), (cst, cst_d)]
        r_sm = []
        for i, (t, d) in enumerate(smalls):
            r = P.res("sm%d" % i)
            r_sm.append(r)
            flat = t[:] if len(t.shape) == 2 else t[:].rearrange({3: "p a b -> p (a b)", 4: "p a b c -> p (a b c)", 5: "p a b c d -> p (a b c d)"}[len(t.shape)])
            P.dma("sp", (lambda e, flat=flat, d=d: e.dma_start(out=flat, in_=d[:, :])), writes=[r])
        r_gains, r_bgate, r_mnorm, r_lruc, r_fconv, r_cst = r_sm
        P.op("dve", CP(identB[:], identF), reads=[r_cst], writes=[r_cb])
        P.op("dve", MS(onesB[:], 1.0), writes=[r_cb])
        P.dma("pool", (lambda e: e.dma_start(out=lruw[:].rearrange("p a b c d -> p (a b c d)"), in_=lruw_d[:, :])), writes=[r_lruw])
        lam_t = sbt("lam_t", [128, DEPTH, 3], F32); r_lam = P.res("lam")
        P.op("act", ACT(lam_t[:], lruc[:, :, :, 7], AF.Exp, scale=-1.0), reads=[r_lruc], writes=[r_lam])
        P.op("act", ACT(lam_t[:], lam_t[:], AF.Ln, bias=1.0), reads=[r_lam], writes=[r_lam])
        P.op("dve", TS(lrud[:, :, :, 0], lam_t[:], -8.0), reads=[r_lam], writes=[r_lrud])
        P.op("dve", TS(lrud[:, :, :, 1], lam_t[:], -16.0), reads=[r_lam], writes=[r_lrud])

        def weight_dma(dst_ap, src_ap, res):
            P.dma("pool", (lambda e: e.dma_start(out=dst_ap, in_=src_ap)), writes=[res])

        def barrier(old, new):
            P.op("pool", MS(dummy[:], 0.0), writes=list(old) + list(new) + [r_dummy])

        dummy = sbt("dummy_sb", [128, 8], F32); r_dummy = P.res("dummy")

        def rms_stats(l, g, sq_ap, r_sq, pbank):
            tok = slice(g * TG, (g + 1) * TG)
            for c in range(KC):
                P.op("act", ACT(sq_ap[:, c, :], xT[:, c, tok], AF.Square), reads=[r_x[c][g]], writes=[r_sq])
            P.op("pe", [MM(pb[pbank][:, :], onesB[:], sq_ap[:, c, :], start=(c == 0), stop=(c == KC - 1)) for c in range(KC)],
                 reads=[r_sq, r_cb], writes=[r_pb[pbank]])
            P.op("act", ACT(rstdb[:, tok], pb[pbank][:, :], AF.Ln, scale=1.0 / D, bias=1e-6), reads=[r_pb[pbank]], writes=[r_rstd[g]])
            P.op("act", ACT(rstdb[:, tok], rstdb[:, tok], AF.Exp, scale=-0.5), reads=[r_rstd[g]], writes=[r_rstd[g]])

        def make_h(l, which, g, h_ap, r_h):
            tok = slice(g * TG, (g + 1) * TG)
            for c in range(KC):
                P.op("dve", STT(h_ap[:, c, :], xT[:, c, tok], gains[:, l, which, c:c + 1], rstdb[:, tok], ALU.mult, ALU.mult),
                     reads=[r_x[c][g], r_rstd[g], r_gains], writes=[r_h])

        def post_norm_residual(l, which, tok0, ntok, src_aps, r_src, sq_ap, r_sq, pbank, rs_ap, r_rs, tmp_ap, r_tmp):
            g = tok0 // TG
            tok = slice(tok0, tok0 + ntok)
            for c in range(KC):
                P.op("act", ACT(sq_ap[:, c, :], src_aps[c], AF.Square), reads=[r_src[c]], writes=[r_sq])
            P.op("pe", [MM(pb[pbank][:, 0:ntok], onesB[:], sq_ap[:, c, :], start=(c == 0), stop=(c == KC - 1)) for c in range(KC)],
                 reads=[r_sq, r_cb], writes=[r_pb[pbank]])
            P.op("act", ACT(rs_ap, pb[pbank][:, 0:ntok], AF.Ln, scale=1.0 / D, bias=1e-6), reads=[r_pb[pbank]], writes=[r_rs])
            P.op("act", ACT(rs_ap, rs_ap, AF.Exp, scale=-0.5), reads=[r_rs], writes=[r_rs])
            for c in range(KC):
                P.op("dve", STT(tmp_ap, src_aps[c], gains[:, l, which, c:c + 1], rs_ap, ALU.mult, ALU.mult),
                     reads=[r_src[c], r_rs, r_gains], writes=[r_tmp])
                P.op("dve", TT(xT[:, c, tok], xT[:, c, tok], tmp_ap, ALU.add), reads=[r_tmp, r_x[c][g]], writes=[r_x[c][g]])

        for l in range(depth):
            apos[0] = 0
            concat = carve(KC * S, BF16).rearrange("p (c t) -> p c t", c=KC)
            r_cat = [[P.res("cat%d_%d" % (c, t)) for t in range(16)] for c in range(KC)]
            hT = [carve(KC * TG, BF16).rearrange("p (c t) -> p c t", c=KC) for _ in range(2)]
            r_hT = [P.res("hT0"), P.res("hT1")]
            W = carve(KC * 1544, BF16).rearrange("p (c n) -> p c n", c=KC)
            r_W = P.res("W")
            pl0 = apos[0]
            all_mixer_res = [r for row in r_cat for r in row] + r_hT + [r_W]
            if l > 0:
                barrier(prev_phase_res, all_mixer_res)

            P.skip = bool(os.environ.get("K_ONLY_E"))
            P.mark("A")
            weight_dma(W[:, :, 0:1544], w_in_d[l, :, 0:1544].rearrange("(c p) n -> p c n", p=128), r_W)
            sqb = carve(KC * TG, BF16).rearrange("p (c t) -> p c t", c=KC); r_sqb = P.res("sqb")
            qTg = carve(4 * TG, BF16).rearrange("p (h t) -> p h t", h=4); r_qTg = P.res("qTg")
            kTg = carve(4 * TG, BF16).rearrange("p (h t) -> p h t", h=4); r_kTg = P.res("kTg")
            NB = 2
            kt = [carve(384, BF16).rearrange("p (h d) -> p h d", h=4) for _ in range(NB)]; r_kt = [P.res() for _ in range(NB)]
            vx = [carve(4 * 97, BF16).rearrange("p (h d) -> p h d", h=4) for _ in range(NB)]; r_vx = [P.res() for _ in range(NB)]
            og = [carve(384, BF16) for _ in range(NB)]; r_og = [P.res() for _ in range(NB)]
            gi = [carve(8, F32) for _ in range(NB)]; r_gi = [P.res() for _ in range(NB)]
            nlf = [carve(4, F32) for _ in range(NB)]; r_nlf = [P.res() for _ in range(NB)]
            es = [carve(4, F32) for _ in range(NB)]; r_es = [P.res() for _ in range(NB)]
            emb = [carve(4, F32) for _ in range(NB)]; r_emb = [P.res() for _ in range(NB)]
            ebl = [carve(8, F32).rearrange("p (j h) -> p j h", j=2) for _ in range(NB)]; r_ebl = [P.res() for _ in range(NB)]
            tmp4 = [carve(4, F32) for _ in range(NB)]; r_tmp4 = [P.res() for _ in range(NB)]
            Stt = [carve(4 * 128, BF16).rearrange("p (h t) -> p h t", h=4) for _ in range(NB)]; r_St = [P.res() for _ in range(NB)]
            Cf = carve(4 * 97, F32).rearrange("p (h d) -> p h d", h=4); r_Cf = P.res("Cf")
            Ctmp = carve(4 * 97, F32).rearrange("p (h d) -> p h d", h=4); r_Ctmp = P.res("Ctmp")
            NCB = 4
            Cb = [carve(4 * 97, BF16).rearrange("p (h d) -> p h d", h=4) for _ in range(NCB)]; r_Cb = [P.res() for _ in range(NCB)]
            den = [carve(4, F32) for _ in range(NB)]; r_den = [P.res() for _ in range(NB)]
            hraw = [carve(384, F32).rearrange("p (h d) -> p h d", h=4) for _ in range(NB)]; r_hraw = [P.res() for _ in range(NB)]
            hsq = [carve(384, F32).rearrange("p (h d) -> p h d", h=4) for _ in range(NB)]; r_hsq = [P.res() for _ in range(NB)]
            ssq = [carve(4, F32) for _ in range(NB)]; r_ssq = [P.res() for _ in range(NB)]
            gm = [carve(384, F32) for _ in range(NB)]; r_gm = [P.res() for _ in range(NB)]
            hAb = [carve(384, BF16) for _ in range(NB)]; r_hAb = [P.res() for _ in range(NB)]
            phaseA_res = ([r_sqb, r_qTg, r_kTg, r_Cf, r_Ctmp] + r_kt + r_vx + r_og + r_gi + r_nlf + r_es + r_emb + r_ebl + r_tmp4 + r_St
                          + r_Cb + r_den + r_hraw + r_hsq + r_ssq + r_gm + r_hAb)
            if l > 0:
                barrier(prev_phase_res, phaseA_res)

            P.op("dve", MS(Cf[0:96], 0.0), writes=[r_Cf])
            P.op("dve", MS(Cb[0][0:96], 0.0), writes=[r_Cb[0]])
            for b in range(NB):
                P.op("dve", MS(vx[b][:, :, 96:97], 1.0), writes=[r_vx[b]])
            cbi = 0
            LN_SC = float(np.log(96.0 ** -0.5))
            lnsc = sbt("lnsc%d" % l, [128, 1], F32); r_lnsc = P.res("lnsc")
            P.op("dve", MS(lnsc[:], LN_SC), writes=[r_lnsc])

            for g in range(NG):
                hb = g % 2
                rms_stats(l, g, sqb, r_sqb, 7)
                make_h(l, 0, g, hT[hb], r_hT[hb])
                for qk in range(2):
                    for h in range(4):
                        bank = (qk * 4 + h) % 2
                        col0 = qk * 384 + h * 96
                        P.op("pe", [MM(pb[bank][0:96, :], W[:, c, col0:col0 + 96], hT[hb][:, c, :], start=(c == 0), stop=(c == KC - 1)) for c in range(KC)],
                             reads=[r_W, r_hT[hb]], writes=[r_pb[bank]])
                        dst = (qTg if qk == 0 else kTg)
                        P.op("act", ACT(dst[0:96, h, :], pb[bank][0:96, :], AF.Copy), reads=[r_pb[bank]], writes=[r_qTg if qk == 0 else r_kTg])
                for ti in range(4):
                    T = g * 4 + ti
                    b = T % NB
                    tl = slice(ti * 128, (ti + 1) * 128)
                    for (bank, c0, n) in ((2, 1152, 392), (3, 384, 384), (4, 768, 384)):
                        P.op("pe", [MM(pb[bank][:, 0:n], hT[hb][:, c, tl], W[:, c, c0:c0 + n], start=(c == 0), stop=(c == KC - 1)) for c in range(KC)],
                             reads=[r_W, r_hT[hb]], writes=[r_pb[bank]])
                    P.op("dve", TT(gi[b], pb[2][:, 384:392], bgate[:, l, :], ALU.add), reads=[r_pb[2], r_bgate], writes=[r_gi[b]])
                    P.op("act", ACT(nlf[b], gi[b][:, 4:8], AF.Exp, scale=-1.0), reads=[r_gi[b]], writes=[r_nlf[b]])
                    P.op("act", ACT(nlf[b], nlf[b], AF.Ln, bias=1.0), reads=[r_nlf[b]], writes=[r_nlf[b]])
                    P.op("act", ACT(og[b], pb[2][:, 0:384], AF.Sigmoid), reads=[r_pb[2], r_gi[b]], writes=[r_og[b]])
                    P.op("pe", [MM(pb[5][:, 0:4], U2f, nlf[b], True, True),
                                MM(pb[5][0:96, 8:12], Ef[:, 0:96], nlf[b], True, True),
                                MM(pb[5][0:96, 16:20], Ef[:, 96:192], nlf[b], True, True)],
                         reads=[r_nlf[b], r_cst], writes=[r_pb[5]])
                    P.op("dve", TT(tmp4[b], gi[b][:, 0:4], pb[5][:, 0:4], ALU.add), reads=[r_gi[b], r_pb[5]], writes=[r_tmp4[b]])
                    P.op("act", ACT(es[b], tmp4[b], AF.Exp, bias=lnsc[:]), reads=[r_tmp4[b], r_lnsc], writes=[r_es[b]])
                    P.op("act", ACT(emb[b], pb[5][:, 0:4], AF.Exp), reads=[r_pb[5], r_tmp4[b]], writes=[r_emb[b]])
                    P.op("act", ACT(ebl[b][0:96, 0, :], pb[5][0:96, 8:12], AF.Exp, scale=-1.0), reads=[r_pb[5]], writes=[r_ebl[b]])
                    P.op("act", ACT(ebl[b][0:96, 1, :], pb[5][0:96, 16:20], AF.Exp, scale=-1.0), reads=[r_pb[5]], writes=[r_ebl[b]])
                    P.op("dve", TT(kt[b][:], pb[3][:, 0:384].rearrange("p (h d) -> p h d", h=4), es[b].unsqueeze(2).to_broadcast([128, 4, 96]), ALU.mult),
                         reads=[r_pb[3], r_es[b]], writes=[r_kt[b]])
                    P.op("act", ACT(vx[b][:, :, 0:96], pb[4][:, 0:384].rearrange("p (h d) -> p h d", h=4), AF.Copy), reads=[r_pb[4]], writes=[r_vx[b]])
                    P.op("pe", [MM(pb[6][:, h * 128:(h + 1) * 128], kTg[0:96, h, tl], qTg[0:96, h, tl], True, True) for h in range(4)],
                         reads=[r_kTg, r_qTg], writes=[r_pb[6]])
                    for h in range(4):
                        P.op("dve", STT(Stt[b][:, h, :], pb[6][:, h * 128:(h + 1) * 128], es[b][:, h:h + 1], U2f, ALU.mult, ALU.mult),
                             reads=[r_pb[6], r_es[b], r_cst], writes=[r_St[b]])
                    ca = cbi
                    for j in range(2):
                        ps = slice(64 * j, 64 * j + 64)
                        P.op("pe", [MM(pb[7][0:96, h * 97:(h + 1) * 97], kt[b][ps, h, :], vx[b][ps, h, :], True, True) for h in range(4)],
                             reads=[r_kt[b], r_vx[b]], writes=[r_pb[7]])
                        P.op("dve", TT(Ctmp[0:96], pb[7][0:96, 0:388].rearrange("p (h d) -> p h d", h=4), Cf[0:96], ALU.add),
                             reads=[r_pb[7], r_Cf], writes=[r_Ctmp])
                        P.op("dve", TT(Cf[0:96], Ctmp[0:96], ebl[b][0:96, j, :].unsqueeze(2).to_broadcast([96, 4, 97]), ALU.mult),
                             reads=[r_Ctmp, r_ebl[b]], writes=[r_Cf])
                        nxt = (cbi + 1) % NCB
                        P.op("act", ACT(Cb[nxt][0:96], Cf[0:96], AF.Copy), reads=[r_Cf], writes=[r_Cb[nxt]])
                        cbi = nxt
                    c_a = ca
                    c_b = (ca + 1) % NCB
                    accb = 0 if (T % 2 == 0) else 1
                    fns = []
                    for h in range(4):
                        o = pb[accb][:, h * 97:(h + 1) * 97]
                        fns.append(MM(o, Stt[b][:, h, :], vx[b][:, h, :], True, False, skip_group_check=True))
                        fns.append(MM(pb[accb][0:64, h * 97:(h + 1) * 97], qTg[0:96, h, ti * 128:ti * 128 + 64], Cb[c_a][0:96, h, :], False, False, skip_group_check=True))
                        fns.append(MM(pb[accb][64:128, h * 97:(h + 1) * 97], qTg[0:96, h, ti * 128 + 64:ti * 128 + 128], Cb[c_b][0:96, h, :], False, True,
                                      skip_group_check=True, tile_position=(0, 64)))
                    P.op("pe", fns, reads=[r_St[b], r_vx[b], r_qTg, r_Cb[c_a], r_Cb[c_b]], writes=[r_pb[accb]])
                    acc = pb[accb][:, 0:388].rearrange("p (h d) -> p h d", h=4)
                    P.op("act", ACT(den[b], acc[:, :, 96], AF.Abs), reads=[r_pb[accb]], writes=[r_den[b]])
                    P.op("dve", TT(den[b], den[b], emb[b], ALU.max), reads=[r_den[b], r_emb[b]], writes=[r_den[b]])
                    P.op("dve", (lambda e, b=b: e.reciprocal(out=den[b], in_=den[b])), reads=[r_den[b]], writes=[r_den[b]])
                    P.op("dve", TT(hraw[b][:], acc[:, :, 0:96], den[b].unsqueeze(2).to_broadcast([128, 4, 96]), ALU.mult),
                         reads=[r_pb[accb], r_den[b]], writes=[r_hraw[b]])
                    P.op("dve", TT(hsq[b][:], hraw[b][:], hraw[b][:], ALU.mult), reads=[r_hraw[b]], writes=[r_hsq[b]])
                    P.op("dve", (lambda e, b=b: e.tensor_reduce(out=ssq[b], in_=hsq[b][:], axis=AX.X, op=ALU.add)), reads=[r_hsq[b]], writes=[r_ssq[b]])
                    P.op("act", ACT(ssq[b], ssq[b], AF.Ln, scale=1.0 / 96, bias=1e-6), reads=[r_ssq[b]], writes=[r_ssq[b]])
                    P.op("act", ACT(ssq[b], ssq[b], AF.Exp, scale=-0.5), reads=[r_ssq[b]], writes=[r_ssq[b]])
                    P.op("dve", TT(gm[b], og[b], mnorm[:, l, :], ALU.mult), reads=[r_og[b], r_mnorm], writes=[r_gm[b]])
                    P.op("dve", TT(hraw[b][:], hraw[b][:], ssq[b].unsqueeze(2).to_broadcast([128, 4, 96]), ALU.mult),
                         reads=[r_hraw[b], r_ssq[b]], writes=[r_hraw[b]])
                    P.op("dve", TT(hAb[b], hraw[b][:].rearrange("p h d -> p (h d)"), gm[b], ALU.mult), reads=[r_hraw[b], r_gm[b]], writes=[r_hAb[b]])
                    pT = pb[5][:].bitcast(BF16)
                    P.op("pe", [TR(pT[:, 128 * c:128 * (c + 1)], hAb[b][:, 128 * c:128 * (c + 1)], identB[:]) for c in range(3)],
                         reads=[r_hAb[b], r_cb], writes=[r_pb[5]])
                    P.op("act", ACT(concat[:, 0:3, T * 128:(T + 1) * 128], pT[:, 0:384].rearrange("p (c t) -> p c t", c=3), AF.Copy),
                         reads=[r_pb[5]], writes=[r_cat[0][T], r_cat[1][T], r_cat[2][T]])

            P.mark("B")
            apos[0] = pl0
            sqb = carve(KC * TG, BF16).rearrange("p (c t) -> p c t", c=KC); r_sqb = P.res("sqb")
            lxs = [carve(TG + 4, F32) for _ in range(3)]; r_lxs = [P.res() for _ in range(3)]
            NB = 2
            xc = [carve(TG, F32) for _ in range(NB)]; r_xc = [P.res() for _ in range(NB)]
            xcb = [carve(TG, BF16) for _ in range(NB)]; r_xcb = [P.res() for _ in range(NB)]
            rr = [carve(TG, F32) for _ in range(NB)]; r_rr = [P.res() for _ in range(NB)]
            ii = [carve(TG, F32) for _ in range(NB)]; r_ii = [P.res() for _ in range(NB)]
            aa = [carve(TG, F32) for _ in range(NB)]; r_aa = [P.res() for _ in range(NB)]
            a2 = [carve(TG, F32) for _ in range(NB)]; r_a2 = [P.res() for _ in range(NB)]
            uu = [carve(TG, F32) for _ in range(NB)]; r_uu = [P.res() for _ in range(NB)]
            hs = [carve(TG, F32) for _ in range(NB)]; r_hs = [P.res() for _ in range(NB)]
            gl = [carve(TG, F32) for _ in range(NB)]; r_gl = [P.res() for _ in range(NB)]
            hprev = carve(4, F32); r_hprev = [P.res() for _ in range(3)]
            phaseB_res = [r_sqb] + r_lxs + r_xc + r_xcb + r_rr + r_ii + r_aa + r_a2 + r_uu + r_hs + r_gl + r_hprev
            barrier(phaseA_res + [r_W], phaseB_res + [r_W])
            weight_dma(W[:, :, 0:768], w_in_d[l, :, 1544:2312].rearrange("(c p) n -> p c n", p=128), r_W)
            for c in range(3):
                P.op("dve", MS(lxs[c][:, 0:4], 0.0), writes=[r_lxs[c]])
                P.op("dve", MS(hprev[:, c:c + 1], 0.0), writes=[r_hprev[c]])
            k = 0
            for g in range(NG):
                hb = g % 2
                make_h(l, 0, g, hT[hb], r_hT[hb])
                for c in range(3):
                    b = k % NB
                    k += 1
                    blx, blg, bra, brx = (0, 1, 2, 3) if b == 0 else (4, 5, 6, 7)
                    P.op("pe", [MM(pb[blx][:, :], W[:, kc, c * 128:(c + 1) * 128], hT[hb][:, kc, :], start=(kc == 0), stop=(kc == KC - 1)) for kc in range(KC)],
                         reads=[r_W, r_hT[hb]], writes=[r_pb[blx]])
                    P.op("pe", [MM(pb[blg][:, :], W[:, kc, 384 + c * 128:384 + (c + 1) * 128], hT[hb][:, kc, :], start=(kc == 0), stop=(kc == KC - 1)) for kc in range(KC)],
                         reads=[r_W, r_hT[hb]], writes=[r_pb[blg]])
                    P.op("act", ACT(lxs[c][:, 4:4 + TG], pb[blx][:, :], AF.Copy), reads=[r_pb[blx]], writes=[r_lxs[c]])
                    P.op("dve", TS(xc[b], lxs[c][:, 4:4 + TG], lruc[:, l, c, 3:4], lruc[:, l, c, 4:5], ALU.mult, ALU.add),
                         reads=[r_lxs[c], r_lruc], writes=[r_xc[b]])
                    for j in range(3):
                        P.op("dve", STT(xc[b], lxs[c][:, 1 + j:1 + j + TG], lruc[:, l, c, j:j + 1], xc[b], ALU.mult, ALU.add),
                             reads=[r_lxs[c], r_lruc, r_xc[b]], writes=[r_xc[b]])
                    P.op("dve", CP(lxs[c][:, 1:4], lxs[c][:, TG + 1:TG + 4]), reads=[r_lxs[c]], writes=[r_lxs[c]])
                    P.op("act", ACT(xcb[b], xc[b], AF.Copy), reads=[r_xc[b]], writes=[r_xcb[b]])
                    P.op("pe", MM(pb[bra][:, :], lruw[:, l, 0, c, :], xcb[b], True, True), reads=[r_lruw, r_xcb[b]], writes=[r_pb[bra]])
                    P.op("pe", MM(pb[brx][:, :], lruw[:, l, 1, c, :], xcb[b], True, True), reads=[r_lruw, r_xcb[b]], writes=[r_pb[brx]])
                    P.op("act", ACT(rr[b], pb[bra][:, :], AF.Sigmoid, bias=lruc[:, l, c, 5:6]), reads=[r_pb[bra], r_lruc], writes=[r_rr[b]])
                    P.op("act", ACT(ii[b], pb[brx][:, :], AF.Sigmoid, bias=lruc[:, l, c, 6:7]), reads=[r_pb[brx], r_lruc], writes=[r_ii[b]])
                    P.op("act", ACT(aa[b], rr[b], AF.Exp, scale=lrud[:, l, c, 0:1]), reads=[r_rr[b], r_lrud], writes=[r_aa[b]])
                    P.op("act", ACT(a2[b], rr[b], AF.Exp, scale=lrud[:, l, c, 1:2]), reads=[r_rr[b], r_lrud], writes=[r_a2[b]])
                    P.op("act", ACT(a2[b], a2[b], AF.Sqrt, scale=-1.0, bias=1.0), reads=[r_a2[b]], writes=[r_a2[b]])
                    P.op("dve", TT(uu[b], a2[b], ii[b], ALU.mult), reads=[r_a2[b], r_ii[b]], writes=[r_uu[b]])
                    P.op("dve", TT(uu[b], uu[b], xc[b], ALU.mult), reads=[r_uu[b], r_xc[b]], writes=[r_uu[b]])
                    P.mark("scan-next")
                    SN = int(os.environ.get("K_SN", "128"))
                    for q0 in range(0, TG, SN):
                        ini = hprev[:, c:c + 1] if q0 == 0 else hs[b][:, q0 - 1:q0]
                        P.op("dve", (lambda e, b=b, ini=ini, q0=q0: e.tensor_tensor_scan(out=hs[b][:, q0:q0 + SN], data0=aa[b][:, q0:q0 + SN], data1=uu[b][:, q0:q0 + SN],
                                                                                       initial=ini, op0=ALU.mult, op1=ALU.add)),
                             reads=[r_aa[b], r_uu[b], r_hprev[c], r_hs[b]], writes=[r_hs[b]])
                    P.op("dve", CP(hprev[:, c:c + 1], hs[b][:, TG - 1:TG]), reads=[r_hs[b]], writes=[r_hprev[c]])
                    P.op("act", ACT(gl[b], pb[blg][:, :], AF.Gelu_apprx_tanh), reads=[r_pb[blg]], writes=[r_gl[b]])
                    P.op("dve", TT(concat[:, 3 + c, g * TG:(g + 1) * TG], hs[b], gl[b], ALU.mult), reads=[r_hs[b], r_gl[b]],
                         writes=[r_cat[3 + c][4 * g + i] for i in range(4)])

            P.mark("C")
            apos[0] = pl0
            akT = carve(2 * S, BF16).rearrange("p (c t) -> p c t", c=2); r_akT = [P.res() for _ in range(NG)]
            ikT = carve(S, BF16); r_ikT = [P.res() for _ in range(NG)]
            avx = carve(16 * 4 * 65, BF16).rearrange("p (t h d) -> p t h d", t=16, h=4); r_avx = [P.res() for _ in range(16)]
            iwt = carve(16 * 4, F32).rearrange("p (t h) -> p t h", t=16); r_iwt = [P.res() for _ in range(16)]
            aqT = carve(2 * TG, BF16).rearrange("p (c t) -> p c t", c=2); r_aqT = P.res("aqT")
            iqT = carve(2 * TG, BF16).rearrange("p (c t) -> p c t", c=2); r_iqT = P.res("iqT")
            sc = carve(S, F32); r_sc = P.res("sc")
            msk = carve(S, BF16); r_msk = P.res("msk")
            junk = msk; r_junk = r_msk
            mT = carve(S, BF16).rearrange("p (j q) -> p j q", j=16); r_mT = P.res("mT")
            rlb = [carve(TG, F32) for _ in range(2)]; r_rlb = [P.res() for _ in range(2)]
            exb = [carve(TG, BF16) for _ in range(2)]; r_exb = [P.res() for _ in range(2)]
            pTb = [carve(TG, BF16) for _ in range(2)]; r_pTb = [P.res() for _ in range(2)]
            bis = carve(8 + 2 * NBIS, F32); r_bis = P.res("bis")
            rc4 = carve(4, F32); r_rc4 = P.res("rc4")
            hcb = carve(256, BF16); r_hcb = P.res("hcb")
            phaseC_res = (r_akT + r_ikT + r_avx + r_iwt + [r_aqT, r_iqT, r_sc, r_msk, r_mT, r_bis, r_rc4, r_hcb] + r_rlb + r_exb + r_pTb)
            barrier(phaseB_res + [r_W], phaseC_res + [r_W])
            wsrc = w_in_d[l]
            r3 = lambda a, b_: wsrc[:, a:b_].rearrange("(c p) n -> p c n", p=128)
            weight_dma(W[:, :, 0:512], r3(2312, 2824), r_W)
            weight_dma(W[:, :, 512:768], r3(3080, 3336), r_W)
            weight_dma(W[:, :, 768:832], r3(3336, 3400), r_W)
            weight_dma(W[:, :, 832:896], r3(3336, 3400), r_W)
            weight_dma(W[:, :, 896:1152], r3(2824, 3080), r_W)
            weight_dma(W[:, :, 1152:1156], r3(3400, 3404), r_W)
            for T in range(16):
                P.op("dve", MS(avx[:, T, :, 64:65], 1.0), writes=[r_avx[T]])
            for g in range(NG):
                hb = g % 2
                make_h(l, 0, g, hT[hb], r_hT[hb])
                tokg = slice(g * TG, (g + 1) * TG)
                plan = [(0, aqT[:, 0, :], [r_aqT]), (128, aqT[:, 1, :], [r_aqT]), (256, akT[:, 0, tokg], [r_akT[g]]), (384, akT[:, 1, tokg], [r_akT[g]]),
                        (512, iqT[:, 0, :], [r_iqT]), (640, iqT[:, 1, :], [r_iqT]), (768, ikT[:, tokg], [r_ikT[g]])]
                for i, (c0, dst, rw) in enumerate(plan):
                    bank = i % 2
                    P.op("pe", [MM(pb[bank][:, :], W[:, kc, c0:c0 + 128], hT[hb][:, kc, :], start=(kc == 0), stop=(kc == KC - 1)) for kc in range(KC)],
                         reads=[r_W, r_hT[hb]], writes=[r_pb[bank]])
                    P.op("act", ACT(dst, pb[bank][:, :], AF.Copy), reads=[r_pb[bank]], writes=rw)
                for ti in range(4):
                    T = 4 * g + ti
                    bank = 2 + (ti % 2)
                    P.op("pe", [MM(pb[bank][:, 0:260], hT[hb][:, kc, ti * 128:(ti + 1) * 128], W[:, kc, 896:1156], start=(kc == 0), stop=(kc == KC - 1)) for kc in range(KC)],
                         reads=[r_W, r_hT[hb]], writes=[r_pb[bank]])
                    P.op("act", ACT(avx[:, T, :, 0:64], pb[bank][:, 0:256].rearrange("p (h d) -> p h d", h=4), AF.Copy), reads=[r_pb[bank]], writes=[r_avx[T]])
                    P.op("act", ACT(iwt[:, T, :], pb[bank][:, 256:260], AF.Copy), reads=[r_pb[bank]], writes=[r_iwt[T]])
                for ti in range(4):
                    T = 4 * g + ti
                    nk = 128 * (T + 1)
                    ql = slice(ti * 128, (ti + 1) * 128)
                    nkb = (nk + 511) // 512
                    key_res_k = [r_ikT[gg] for gg in range(g + 1)]
                    cnt = 0
                    for h in range(4):
                        hp = slice(64 * (h % 2), 64 * (h % 2) + 64)
                        for kb in range(nkb):
                            k0 = kb * 512
                            w = min(512, nk - k0)
                            bank = 4 + (cnt % 2)
                            rb = cnt % 2
                            cnt += 1
                            P.op("pe", MM(pb[bank][:, 0:w], iqT[hp, h // 2, ql], ikT[hp, k0:k0 + w], True, True),
                                 reads=[r_iqT] + key_res_k, writes=[r_pb[bank]])
                            P.op("act", ACT(rlb[rb][:, 0:w], pb[bank][:, 0:w], AF.Relu), reads=[r_pb[bank]], writes=[r_rlb[rb]])
                            if h == 0:
                                P.op("dve", TS(sc[:, k0:k0 + w], rlb[rb][:, 0:w], iwt[:, T, 0:1]), reads=[r_rlb[rb], r_iwt[T]], writes=[r_sc])
                            else:
                                P.op("dve", STT(sc[:, k0:k0 + w], rlb[rb][:, 0:w], iwt[:, T, h:h + 1], sc[:, k0:k0 + w], ALU.mult, ALU.add),
                                     reads=[r_rlb[rb], r_iwt[T], r_sc], writes=[r_sc])
                    if T >= 2:
                        P.op("dve", (lambda e, nk=nk: e.tensor_reduce(out=bis[:, 0:1], in_=sc[:, 0:nk], axis=AX.X, op=ALU.max)), reads=[r_sc], writes=[r_bis])
                        P.op("dve", (lambda e, nk=nk: e.tensor_reduce(out=bis[:, 1:2], in_=sc[:, 0:nk], axis=AX.X, op=ALU.min)), reads=[r_sc], writes=[r_bis])
                    P.op("dve", MS(sc[0:64, nk - 64:nk], -1e30), reads=[r_sc], writes=[r_sc])
                    if T >= 2:
                        P.op("dve", TT(bis[:, 2:3], bis[:, 0:1], bis[:, 1:2], ALU.subtract), reads=[r_bis], writes=[r_bis])
                        P.op("dve", TS(bis[:, 8:8 + NBIS], pow2, bis[:, 2:3]), reads=[r_bis, r_cst], writes=[r_bis])
                        P.op("dve", TS(bis[:, 8 + NBIS:8 + 2 * NBIS], bis[:, 8:8 + NBIS], -0.5), reads=[r_bis], writes=[r_bis])
                        P.op("dve", TT(bis[:, 3:4], bis[:, 1:2], bis[:, 8:9], ALU.add), reads=[r_bis], writes=[r_bis])
                        for kk in range(NBIS):
                            P.op("dve", (lambda e, nk=nk: e.tensor_scalar(out=junk[:, 0:nk], in0=sc[:, 0:nk], scalar1=bis[:, 3:4], scalar2=0.0,
                                                                            op0=ALU.is_ge, op1=ALU.add, accum_out=bis[:, 4:5])),
                                 reads=[r_sc, r_bis], writes=[r_msk, r_bis])
                            P.op("dve", STT(bis[:, 5:6], bis[:, 4:5], 255.5, bis[:, 8 + kk:9 + kk], ALU.is_ge, ALU.mult), reads=[r_bis], writes=[r_bis])
                            P.op("dve", STT(bis[:, 3:4], bis[:, 5:6], bis[:, 8 + NBIS + kk:9 + NBIS + kk], bis[:, 3:4], ALU.add, ALU.add), reads=[r_bis], writes=[r_bis])
                        P.op("dve", TT(bis[:, 3:4], bis[:, 3:4], bis[:, 8 + 2 * NBIS - 1:8 + 2 * NBIS], ALU.add), reads=[r_bis], writes=[r_bis])
                        P.op("dve", TS(msk[:, 0:nk], sc[:, 0:nk], bis[:, 3:4], None, ALU.is_ge), reads=[r_sc, r_bis], writes=[r_msk])
                    else:
                        P.op("dve", TS(msk[:, 0:nk], sc[:, 0:nk], -1e29, None, ALU.is_ge), reads=[r_sc], writes=[r_msk])
                    nb_ = T + 1
                    for j0 in range(0, nb_, 8):
                        bank = 6 + ((j0 // 8) % 2)
                        pT = pb[bank][:].bitcast(BF16)
                        n = min(8, nb_ - j0)
                        P.op("pe", [TR(pT[:, 128 * i:128 * (i + 1)], msk[:, (j0 + i) * 128:(j0 + i + 1) * 128], identB[:]) for i in range(n)],
                             reads=[r_msk, r_cb], writes=[r_pb[bank]])
                        P.op("act", ACT(mT[:, j0:j0 + n, :], pT[:, 0:128 * n].rearrange("p (j q) -> p j q", j=n), AF.Copy), reads=[r_pb[bank]], writes=[r_mT])
                    key_res_a = [r_akT[gg] for gg in range(g + 1)]
                    cnt = 0
                    for h in range(4):
                        hp = slice(64 * (h % 2), 64 * (h % 2) + 64)
                        groups = [(j0, min(4, nb_ - j0)) for j0 in range(0, nb_, 4)]
                        for gi_, (j0, n) in enumerate(groups):
                            bank = 4 + (cnt % 2)
                            eb = cnt % 2
                            cnt += 1
                            P.op("pe", [MM(pb[bank][:, 128 * i:128 * (i + 1)], akT[hp, h // 2, (j0 + i) * 128:(j0 + i + 1) * 128], aqT[hp, h // 2, ql], True, True) for i in range(n)],
                                 reads=[r_aqT] + key_res_a, writes=[r_pb[bank]])
                            P.op("act", ACT(exb[eb][:, 0:128 * n], pb[bank][:, 0:128 * n], AF.Exp, scale=0.125), reads=[r_pb[bank]], writes=[r_exb[eb]])
                            P.op("dve", TT(pTb[eb][:, 0:128 * n], exb[eb][:, 0:128 * n], mT[:, j0:j0 + n, :].rearrange("p j q -> p (j q)"), ALU.mult),
                                 reads=[r_exb[eb], r_mT], writes=[r_pTb[eb]])
                            P.op("pe", [MM(pb[3][:, h * 65:(h + 1) * 65], pTb[eb][:, 128 * i:128 * (i + 1)], avx[:, j0 + i, h, :],
                                           start=(j0 + i == 0), stop=(j0 + i == nb_ - 1), skip_group_check=True) for i in range(n)],
                                 reads=[r_pTb[eb]] + [r_avx[j0 + i] for i in range(n)], writes=[r_pb[3]])
                    oacc = pb[3][:, 0:260].rearrange("p (h d) -> p h d", h=4)
                    P.op("dve", (lambda e: e.reciprocal(out=rc4, in_=oacc[:, :, 64])), reads=[r_pb[3]], writes=[r_rc4])
                    P.op("dve", TT(hcb.rearrange("p (h d) -> p h d", h=4), oacc[:, :, 0:64], rc4.unsqueeze(2).to_broadcast([128, 4, 64]), ALU.mult),
                         reads=[r_pb[3], r_rc4], writes=[r_hcb])
                    pT = pb[2][:].bitcast(BF16)
                    P.op("pe", [TR(pT[:, 128 * c:128 * (c + 1)], hcb[:, 128 * c:128 * (c + 1)], identB[:]) for c in range(2)],
                         reads=[r_hcb, r_cb], writes=[r_pb[2]])
                    P.op("act", ACT(concat[:, 6:8, T * 128:(T + 1) * 128], pT[:, 0:256].rearrange("p (c t) -> p c t", c=2), AF.Copy),
                         reads=[r_pb[2]], writes=[r_cat[6][T], r_cat[7][T]])

            P.mark("Cend")
            if dbg:
                P.force = True
            apos[0] = pl0
            if dbg and l == dbg - 1:
                dtmp = carve(S, F32); r_dtmp = P.res("dtmp")
                barrier(phaseC_res, [r_dtmp])
                for c in range(KC):
                    P.op("dve", CP(dtmp[:], concat[:, c, :]), reads=[r_cat[c][t] for t in range(16)], writes=[r_dtmp])
                    P.dma("sp", (lambda e, c=c: e.dma_start(out=dbg_d[c * 128:(c + 1) * 128, :], in_=dtmp[:])), reads=[r_dtmp])

            if stop_after == "C" and l == depth - 1:
                break
            NTD = 256
            sqd = carve(KC * NTD, BF16).rearrange("p (c t) -> p c t", c=KC); r_sqd = P.res("sqd")
            rsd = carve(NTD, F32); r_rsd = P.res("rsd")
            tmpd = carve(NTD, F32); r_tmpd = P.res("tmpd")
            phaseD_res = [r_sqd, r_rsd, r_tmpd]
            barrier(phaseC_res + [r_W], phaseD_res + [r_W])
            weight_dma(W[:, :, 0:1024], w_out_d[l].rearrange("(c p) n -> p c n", p=128), r_W)
            for gd in range(S // NTD):
                t0 = gd * NTD
                tiles = [t0 // 128, t0 // 128 + 1]
                for c in range(KC):
                    bank = c // 2
                    half = (c % 2) * NTD
                    P.op("pe", [MM(pb[bank][:, half:half + NTD], W[:, kc, c * 128:(c + 1) * 128], concat[:, kc, t0:t0 + NTD], start=(kc == 0), stop=(kc == KC - 1)) for kc in range(KC)],
                         reads=[r_W] + [r_cat[kc][t] for kc in range(KC) for t in tiles], writes=[r_pb[bank]])
                srcs = [pb[c // 2][:, (c % 2) * NTD:(c % 2) * NTD + NTD] for c in range(KC)]
                post_norm_residual(l, 1, t0, NTD, srcs, [r_pb[c // 2] for c in range(KC)], sqd, r_sqd, 4, rsd, r_rsd, tmpd, r_tmpd)

            if stop_after == "D" and l == depth - 1:
                break

            P.skip = False
            P.mark("E")
            apos[0] = 0
            hTf = [carve(KC * TG, BF16).rearrange("p (c t) -> p c t", c=KC) for _ in range(2)]; r_hTf = [P.res() for _ in range(2)]
            sqf = carve(KC * TG, BF16).rearrange("p (c t) -> p c t", c=KC); r_sqf = P.res("sqf")
            actg = carve(NFC * TG, BF16).rearrange("p (k t) -> p k t", k=NFC); r_actg = [P.res() for _ in range(NFC)]
            ost = carve(KC * TG, F32).rearrange("p (c t) -> p c t", c=KC); r_ost = [P.res() for _ in range(KC)]
            NWU = 2
            wu = [carve(KC * 2 * 256, BF16).rearrange("p (c s n) -> p c s n", c=KC, s=2) for _ in range(NWU)]; r_wu = [P.res() for _ in range(NWU)]
            NWD = 2
            wd = [carve(NFC * 256, BF16).rearrange("p (k n) -> p k n", k=NFC) for _ in range(NWD)]; r_wd = [P.res() for _ in range(NWD)]
            NU = 2
            ub = [[carve(TG + 2, F32) for _ in range(2)] for _ in range(NU)]; r_ub = [[P.res() for _ in range(2)] for _ in range(NU)]
            cv = [[carve(TG, F32) for _ in range(2)] for _ in range(NU)]; r_cv = [[P.res() for _ in range(2)] for _ in range(NU)]
            uh = carve(44 * 2, F32).rearrange("p (f t) -> p f t", f=44); r_uh = [P.res() for _ in range(44)]
            rsf = carve(TG, F32); r_rsf = P.res("rsf")
            tmpf = carve(TG, F32); r_tmpf = P.res("tmpf")
            phaseE_res = (r_hTf + [r_sqf, r_rsf, r_tmpf] + r_actg + r_ost + r_wu + r_wd + [x for y in r_ub for x in y] + [x for y in r_cv for x in y] + r_uh)
            barrier(all_mixer_res + phaseD_res, phaseE_res)
            P.op("dve", MS(uh[:].rearrange("p f t -> p (f t)"), 0.0), writes=r_uh)
            wu_i = 0
            wd_i = 0
            up_d = ffn_up_d[l]
            dn_d = ffn_down_d[l]
            pair_k = 0
            for g in range(NG):
                hb = g % 2
                if g == 1:
                    P.wait_all_dma("sp", r_wu + r_wd)
                rms_stats(l, g, sqf, r_sqf, 7)
                make_h(l, 2, g, hTf[hb], r_hTf[hb])
                for j2 in range(NFC // 2):
                    ws = wu_i % NWU
                    wu_i += 1
                    wu_flat = wu[ws][:].rearrange("p c s n -> p (c s n)")
                    if g == 0:
                        weight_dma(wu[ws][:, :, 0, :], up_d[:, j2 * 256:(j2 + 1) * 256].rearrange("(c p) n -> p c n", p=128), r_wu[ws])
                        weight_dma(wu[ws][:, :, 1, :], up_d[:, DFF + j2 * 256:DFF + (j2 + 1) * 256].rearrange("(c p) n -> p c n", p=128), r_wu[ws])
                        P.dma("sp", (lambda e, wu_flat=wu_flat, j2=j2, l=l: e.dma_start(out=upbf_d[l, j2], in_=wu_flat)), reads=[r_wu[ws]])
                    else:
                        P.dma("sp", (lambda e, wu_flat=wu_flat, j2=j2, l=l: e.dma_start(out=wu_flat, in_=upbf_d[l, j2])), writes=[r_wu[ws]])
                    for jj in range(2):
                        j = 2 * j2 + jj
                        u = pair_k % NU
                        pair_k += 1
                        bg, bu = (0, 1) if u == 0 else (2, 3)
                        for s_, bank in ((0, bg), (1, bu)):
                            P.op("pe", [MM(pb[bank][:, :], wu[ws][:, kc, s_, jj * 128:(jj + 1) * 128], hTf[hb][:, kc, :], start=(kc == 0), stop=(kc == KC - 1)) for kc in range(KC)],
                                 reads=[r_wu[ws], r_hTf[hb]], writes=[r_pb[bank]])
                        for s_, bank in ((0, bg), (1, bu)):
                            f = j + s_ * NFC
                            ubx = ub[u][s_]; rub = r_ub[u][s_]
                            cvx = cv[u][s_]; rcv = r_cv[u][s_]
                            P.op("act", ACT(ubx[:, 2:2 + TG], pb[bank][:, :], AF.Copy), reads=[r_pb[bank]], writes=[rub])
                            P.op("act", ACT(ubx[:, 0:2], uh[:, f, :], AF.Copy), reads=[r_uh[f]], writes=[rub])
                            P.op("act", ACT(cvx, pb[bank][:, :], AF.Identity, scale=fconv[:, l, f, 2:3], bias=fconv[:, l, f, 3:4]),
                                 reads=[r_pb[bank], r_fconv], writes=[rcv])
                            P.op("dve", STT(cvx, ubx[:, 1:1 + TG], fconv[:, l, f, 1:2], cvx, ALU.mult, ALU.add), reads=[rub, r_fconv, rcv], writes=[rcv])
                            P.op("dve", STT(cvx, ubx[:, 0:TG], fconv[:, l, f, 0:1], cvx, ALU.mult, ALU.add), reads=[rub, r_fconv, rcv], writes=[rcv])
                            P.op("act", ACT(uh[:, f, :], ubx[:, TG:TG + 2], AF.Copy), reads=[rub], writes=[r_uh[f]])
                        P.op("act", ACT(cv[u][0], cv[u][0], AF.Gelu_apprx_tanh), reads=[r_cv[u][0]], writes=[r_cv[u][0]])
                        P.op("dve", TT(actg[:, j, :], cv[u][0], cv[u][1], ALU.mult), reads=[r_cv[u][0], r_cv[u][1]], writes=[r_actg[j]])
                for c2 in range(4):
                    ws = wd_i % NWD
                    wd_i += 1
                    wd_flat = wd[ws][:].rearrange("p k n -> p (k n)")
                    if g == 0:
                        for k0_ in (0, 11):
                            weight_dma(wd[ws][:, k0_:k0_ + 11, :], dn_d[k0_ * 128:(k0_ + 11) * 128, c2 * 256:(c2 + 1) * 256].rearrange("(k p) n -> p k n", p=128), r_wd[ws])
                        P.dma("sp", (lambda e, wd_flat=wd_flat, c2=c2, l=l: e.dma_start(out=dnbf_d[l, c2], in_=wd_flat)), reads=[r_wd[ws]])
                    else:
                        P.dma("sp", (lambda e, wd_flat=wd_flat, c2=c2, l=l: e.dma_start(out=wd_flat, in_=dnbf_d[l, c2])), writes=[r_wd[ws]])
                    for cc in range(2):
                        c = 2 * c2 + cc
                        bank = 4 + (c % 2)
                        P.op("pe", [MM(pb[bank][:, :], wd[ws][:, k_, cc * 128:(cc + 1) * 128], actg[:, k_, :], start=(k_ == 0), stop=(k_ == NFC - 1)) for k_ in range(NFC)],
                             reads=[r_wd[ws]] + r_actg, writes=[r_pb[bank]])
                        P.op("act", ACT(ost[:, c, :], pb[bank][:, :], AF.Copy), reads=[r_pb[bank]], writes=[r_ost[c]])
                post_norm_residual(l, 3, g * TG, TG, [ost[:, c, :] for c in range(KC)], r_ost, sqf, r_sqf, 6, rsf, r_rsf, tmpf, r_tmpf)
            prev_phase_res = phaseE_res

        P.force = True
        for c in range(KC):
            for g in range(NG):
                P.dma("sp", (lambda e, c=c, g=g: e.dma_start(out=yT_d[c * 128:(c + 1) * 128, g * TG:(g + 1) * TG], in_=xT[:, c, g * TG:(g + 1) * TG])),
                      reads=[r_x[c][g]])
        P.wait_all_dma("sp", [r_x[c][g] for c in range(KC) for g in range(NG)] + ([r_dtmp] if dbg else []))
        P.emit()
    return nc


def host_params(inputs):
    f = np.float32
    L = DEPTH
    gains = np.zeros((128, L, 4, 8), f)
    for l in range(L):
        for i, nm in enumerate(("norm_mix_pre", "norm_mix_post", "norm_ffn_pre", "norm_ffn_post")):
            gains[:, l, i, :] = np.asarray(inputs[nm][l], f).reshape(8, 128).T
    bgate = np.zeros((128, L, 8), f)
    bgate[:, :, 0:4] = np.asarray(inputs["b_igate"], f)[None]
    bgate[:, :, 4:8] = np.asarray(inputs["b_fgate"], f)[None]
    mnorm = np.broadcast_to(np.asarray(inputs["mlstm_norm"], f)[None], (128, L, 384)).copy()
    lruc = np.zeros((128, L, 3, 8), f)
    for l in range(L):
        for j in range(4):
            lruc[:, l, :, j] = np.asarray(inputs["lru_conv_w"][l, j], f).reshape(3, 128).T
        lruc[:, l, :, 4] = np.asarray(inputs["lru_conv_b"][l], f).reshape(3, 128).T
        lruc[:, l, :, 5] = np.asarray(inputs["lru_b_a"][l], f).reshape(3, 128).T
        lruc[:, l, :, 6] = np.asarray(inputs["lru_b_x"][l], f).reshape(3, 128).T
        lruc[:, l, :, 7] = np.asarray(inputs["lru_lambda"][l], f).reshape(3, 128).T
    lruw = np.zeros((128, L, 2, 3, 128), f)
    for l in range(L):
        for i, nm in enumerate(("lru_w_a", "lru_w_x")):
            w = np.asarray(inputs[nm][l], f)
            for c in range(3):
                for a in range(2):
                    lruw[64 * a:64 * a + 64, l, i, c, 64 * a:64 * a + 64] = w[2 * c + a]
    fconv = np.zeros((128, L, 44, 4), f)
    for l in range(L):
        for j in range(3):
            fconv[:, l, :, j] = np.asarray(inputs["ffn_conv_w"][l, j], f).reshape(44, 128).T
        fconv[:, l, :, 3] = np.asarray(inputs["ffn_conv_b"][l], f).reshape(44, 128).T
    cst = np.zeros((128, 128 + 128 + 192 + NBIS), f)
    cst[:, 0:128] = np.eye(128, dtype=f)
    for s in range(128):
        for t in range(128):
            if s // 64 == t // 64 and s <= t:
                cst[s, 128 + t] = 1.0
    cst[0:64, 256:352] = 1.0
    cst[64:128, 352:448] = 1.0
    cst[:, 448:448 + NBIS] = (2.0 ** -(np.arange(NBIS) + 1.0))[None, :]
    return {"gains": gains.reshape(128, -1), "bgate": bgate.reshape(128, -1), "mnorm": mnorm.reshape(128, -1), "lruc": lruc.reshape(128, -1),
            "lruw": lruw.reshape(128, -1), "fconv": fconv.reshape(128, -1), "cst": cst}


_NC_CACHE = {}


def kernel(**inputs):
    x = np.asarray(inputs["x"], np.float32)
    B = x.shape[0]
    hp = host_params(inputs)
    shared = {"w_in": np.ascontiguousarray(inputs["w_in"], np.float32), "w_out": np.ascontiguousarray(inputs["w_out"], np.float32),
              "ffn_up": np.ascontiguousarray(inputs["ffn_up"], np.float32), "ffn_down": np.ascontiguousarray(inputs["ffn_down"], np.float32)}
    shared.update(hp)
    if "nc" not in _NC_CACHE:
        _NC_CACHE["nc"] = build()
    nc = _NC_CACHE["nc"]
    in_maps = []
    for b in range(B):
        m = dict(shared)
        m["xT"] = np.ascontiguousarray(x[b].T)
        in_maps.append(m)
    res = run_bass_kernel_spmd(nc, in_maps, core_ids=list(range(B)))
    out = np.stack([np.ascontiguousarray(res.results[b]["yT"].T) for b in range(B)], axis=0)
    return out.astype(np.float32)
```
